# Optimizing a Trainium2 kernel written in Bass

```python
import math
import numpy as np
import jax
import jax.numpy as jnp
from jax import lax


D_MODEL = 1024
BATCH = 8
SEQ = 2048
DEPTH = 2

SB_HEADS = 8
SB_HEAD_DIM = 64
SB_WIDTH = SB_HEADS * SB_HEAD_DIM
SSM_WIDTH = D_MODEL - SB_WIDTH
SSM_GROUP = 16
SSM_GROUPS = SSM_WIDTH // SSM_GROUP
SSM_STATE = 64
Q_BLOCK = 128

NSA_HEADS = 16
NSA_KV_HEADS = 4
NSA_HEAD_DIM = 64
CMP_LEN = 32
CMP_STRIDE = 16
CMP_HIDDEN = 256
SEL_LEN = 64
SEL_TOP = 8
WINDOW = 512
SEL_Q_CHUNK = 32
ROPE_THETA = 10000.0
FORCE_BONUS = 1e4
NEG = -1e30

N_EXPERTS = 16
N_GROUPS = 4
EXPERTS_PER_GROUP = N_EXPERTS // N_GROUPS
TOP_K = 2
D_EXPERT = 512

ALPHA = (2 * DEPTH) ** 0.25
BETA = (8 * DEPTH) ** -0.25
LN_EPS = 1e-5

kernel_name = 'hybrid_stickbreak_s5_nsa_grouped_moe_deepnorm'


def layer_norm(x, g, b):
    xf = x.astype(jnp.float32)
    mu = jnp.mean(xf, axis=-1, keepdims=True)
    var = jnp.mean(jnp.square(xf - mu), axis=-1, keepdims=True)
    return ((xf - mu) * lax.rsqrt(var + LN_EPS) * g + b).astype(x.dtype)


def rope_tables(pos, dim):
    inv = ROPE_THETA ** (-jnp.arange(0, dim, 2, dtype=jnp.float32) / dim)
    ang = pos.astype(jnp.float32)[:, None] * inv[None, :]
    return jnp.cos(ang), jnp.sin(ang)


def apply_rope(x, cos, sin):
    x1, x2 = jnp.split(x.astype(jnp.float32), 2, axis=-1)
    c = cos[None, :, None, :]
    s = sin[None, :, None, :]
    return jnp.concatenate([x1 * c - x2 * s, x1 * s + x2 * c], axis=-1).astype(x.dtype)


def stick_breaking_attention(q, k, v):
    B, L, H, d = q.shape
    scale = d ** -0.5
    outs = []
    for i in range(L // Q_BLOCK):
        q0 = i * Q_BLOCK
        kend = q0 + Q_BLOCK
        z = jnp.einsum('bqhd,bkhd->bhqk', q[:, q0:kend], k[:, :kend]).astype(jnp.float32) * scale
        t = q0 + jnp.arange(Q_BLOCK)
        s = jnp.arange(kend)
        mask = s[None, :] < t[:, None]
        log_1mb = jnp.where(mask, -jax.nn.softplus(z), 0.0)
        after = lax.cumsum(log_1mb, axis=3, reverse=True) - log_1mb
        w = jnp.where(mask, jnp.exp(jax.nn.log_sigmoid(z) + after), 0.0)
        outs.append(jnp.einsum('bhqk,bkhd->bqhd', w.astype(v.dtype), v[:, :kend]))
    return jnp.concatenate(outs, axis=1)


def _ssm_combine(e1, e2):
    a1r, a1i, b1r, b1i = e1
    a2r, a2i, b2r, b2i = e2
    return (a2r * a1r - a2i * a1i,
            a2r * a1i + a2i * a1r,
            a2r * b1r - a2i * b1i + b2r,
            a2r * b1i + a2i * b1r + b2i)


def s5_ssm(u, lam_re, lam_im, log_dt, b_re, b_im, c_re, c_im, d_skip):
    Bsz, L, W = u.shape
    uf = u.astype(jnp.float32).reshape(Bsz, L, SSM_GROUPS, SSM_GROUP)
    dt = jnp.exp(log_dt.astype(jnp.float32))[:, None]
    lr = lam_re.astype(jnp.float32)
    li = lam_im.astype(jnp.float32)
    mag = jnp.exp(lr * dt)
    a_re = mag * jnp.cos(li * dt)
    a_im = mag * jnp.sin(li * dt)
    den = lr * lr + li * li
    nr = a_re - 1.0
    f_re = (nr * lr + a_im * li) / den
    f_im = (a_im * lr - nr * li) / den
    br = b_re.astype(jnp.float32)
    bi = b_im.astype(jnp.float32)
    bb_re = f_re[..., None] * br - f_im[..., None] * bi
    bb_im = f_re[..., None] * bi + f_im[..., None] * br
    bu_re = jnp.einsum('blgh,gph->lbgp', uf, bb_re)
    bu_im = jnp.einsum('blgh,gph->lbgp', uf, bb_im)
    shp = (L, 1, SSM_GROUPS, SSM_STATE)
    elems = (jnp.broadcast_to(a_re[None, None], shp), jnp.broadcast_to(a_im[None, None], shp), bu_re, bu_im)
    _, _, xr, xi = lax.associative_scan(_ssm_combine, elems, axis=0)
    y = (jnp.einsum('ghp,lbgp->blgh', c_re.astype(jnp.float32), xr)
         - jnp.einsum('ghp,lbgp->blgh', c_im.astype(jnp.float32), xi))
    y = y.reshape(Bsz, L, W) + d_skip.astype(jnp.float32) * u.astype(jnp.float32)
    return y.astype(u.dtype)


def even_mixer(x, w_in, lam_re, lam_im, log_dt, b_re, b_im, c_re, c_im, d_skip, w_glu, w_out):
    B, L, _ = x.shape
    proj = x @ w_in
    q, k, v, u = jnp.split(proj, [SB_WIDTH, 2 * SB_WIDTH, 3 * SB_WIDTH], axis=-1)
    shp = (B, L, SB_HEADS, SB_HEAD_DIM)
    o_a = stick_breaking_attention(q.reshape(shp), k.reshape(shp), v.reshape(shp)).reshape(B, L, SB_WIDTH)
    y = s5_ssm(u, lam_re, lam_im, log_dt, b_re, b_im, c_re, c_im, d_skip)
    h = jax.nn.gelu(y)
    o_b = h * jax.nn.sigmoid(h @ w_glu)
    return jnp.concatenate([o_a, o_b], axis=-1) @ w_out


def compress_blocks(t, pos_emb, w1, w2, starts):
    B, L, G, d = t.shape
    idx = jnp.asarray((starts[:, None] + np.arange(CMP_LEN)[None, :]).astype(np.int32))
    blk = t[:, idx] + pos_emb[:, None, :]
    flat = jnp.moveaxis(blk, 3, 2).reshape(B, idx.shape[0], G, CMP_LEN * d)
    return jax.nn.gelu(flat @ w1) @ w2


def nsa_mixer(x, w_in, cmp_pos_k, cmp_w1_k, cmp_w2_k, cmp_pos_v, cmp_w1_v, cmp_w2_v, w_out):
    B, L, _ = x.shape
    H, G, d = NSA_HEADS, NSA_KV_HEADS, NSA_HEAD_DIM
    R = H // G
    scale = d ** -0.5
    kvw = G * d
    proj = x @ w_in
    cuts = [H * d + i * kvw for i in range(7)]
    q, kc, vc, ks, vs, kw, vw, gate = jnp.split(proj, cuts, axis=-1)
    pos = jnp.arange(L)
    cos, sin = rope_tables(pos, d)
    q = apply_rope(q.reshape(B, L, H, d), cos, sin).reshape(B, L, G, R, d)
    kc, vc, ks, vs, kw, vw = [a.reshape(B, L, G, d) for a in (kc, vc, ks, vs, kw, vw)]
    ks = apply_rope(ks, cos, sin)
    kw = apply_rope(kw, cos, sin)

    M = (L - CMP_LEN) // CMP_STRIDE + 1
    cmp_start = np.arange(M) * CMP_STRIDE
    cmp_end = cmp_start + CMP_LEN - 1
    k_cmp = compress_blocks(kc, cmp_pos_k, cmp_w1_k, cmp_w2_k, cmp_start)
    c_cos, c_sin = rope_tables(jnp.asarray(cmp_end.astype(np.int32)), d)
    k_cmp = apply_rope(k_cmp, c_cos, c_sin)
    v_cmp = compress_blocks(vc, cmp_pos_v, cmp_w1_v, cmp_w2_v, cmp_start)
    s_c = jnp.einsum('blgrd,bmgd->blgrm', q, k_cmp).astype(jnp.float32) * scale
    valid_c = (jnp.asarray(cmp_end.astype(np.int32))[None, :] <= pos[:, None])[None, :, None, None, :]
    p_c = jnp.where(valid_c, jax.nn.softmax(jnp.where(valid_c, s_c, NEG), axis=-1), 0.0)
    o_c = jnp.einsum('blgrm,bmgd->blgrd', p_c.astype(v_cmp.dtype), v_cmp)

    NB = L // SEL_LEN
    n_sel = min(SEL_TOP, NB)
    sel_start = np.arange(NB) * SEL_LEN
    ov = ((cmp_start[:, None] < sel_start[None, :] + SEL_LEN)
          & (cmp_start[:, None] + CMP_LEN > sel_start[None, :])).astype(np.float32)
    imp = jnp.einsum('blgrm,mn->blgn', p_c, jnp.asarray(ov))
    blk = jnp.arange(NB)
    cur = pos // SEL_LEN
    valid_s = blk[None, :] * SEL_LEN <= pos[:, None]
    forced = (blk[None, :] == 0) | (blk[None, :] == cur[:, None]) | (blk[None, :] == cur[:, None] - 1)
    score = jnp.where(valid_s[None, :, None, :],
                      imp + jnp.where(forced, FORCE_BONUS, 0.0)[None, :, None, :], NEG)
    _, sel_idx = lax.top_k(score, n_sel)

    ks_t = ks.reshape(B, NB, SEL_LEN, G, d).transpose(0, 3, 1, 2, 4)
    vs_t = vs.reshape(B, NB, SEL_LEN, G, d).transpose(0, 3, 1, 2, 4)
    b_ix = jnp.arange(B)[:, None, None, None]
    g_ix = jnp.arange(G)[None, None, :, None]

    def sel_chunk(c):
        q0 = c * SEL_Q_CHUNK
        qc = lax.dynamic_slice_in_dim(q, q0, SEL_Q_CHUNK, axis=1)
        ic = lax.dynamic_slice_in_dim(sel_idx, q0, SEL_Q_CHUNK, axis=1)
        kg = ks_t[b_ix, g_ix, ic]
        vg = vs_t[b_ix, g_ix, ic]
        s = jnp.einsum('bcgrd,bcgnsd->bcgrns', qc, kg).astype(jnp.float32) * scale
        kpos = ic[..., None] * SEL_LEN + jnp.arange(SEL_LEN)
        tq = q0 + jnp.arange(SEL_Q_CHUNK)
        mask = (kpos <= tq[None, :, None, None, None])[:, :, :, None]
        s = jnp.where(mask, s, NEG).reshape(B, SEL_Q_CHUNK, G, R, n_sel * SEL_LEN)
        p = jax.nn.softmax(s, axis=-1).reshape(B, SEL_Q_CHUNK, G, R, n_sel, SEL_LEN)
        return jnp.einsum('bcgrns,bcgnsd->bcgrd', p.astype(vg.dtype), vg)

    o_s = lax.map(sel_chunk, jnp.arange(L // SEL_Q_CHUNK))
    o_s = jnp.moveaxis(o_s, 0, 1).reshape(B, L, G, R, d)

    span = WINDOW + Q_BLOCK
    kw_pad = jnp.pad(kw, ((0, 0), (WINDOW, 0), (0, 0), (0, 0)))
    vw_pad = jnp.pad(vw, ((0, 0), (WINDOW, 0), (0, 0), (0, 0)))

    def win_block(i):
        q0 = i * Q_BLOCK
        qb = lax.dynamic_slice_in_dim(q, q0, Q_BLOCK, axis=1)
        kb = lax.dynamic_slice_in_dim(kw_pad, q0, span, axis=1)
        vb = lax.dynamic_slice_in_dim(vw_pad, q0, span, axis=1)
        s = jnp.einsum('bqgrd,bkgd->bqgrk', qb, kb).astype(jnp.float32) * scale
        kpos = q0 - WINDOW + jnp.arange(span)
        tq = q0 + jnp.arange(Q_BLOCK)
        mask = ((kpos[None, :] <= tq[:, None]) & (kpos[None, :] > tq[:, None] - WINDOW)
                & (kpos[None, :] >= 0))
        s = jnp.where(mask[None, :, None, None, :], s, NEG)
        p = jax.nn.softmax(s, axis=-1)
        return jnp.einsum('bqgrk,bkgd->bqgrd', p.astype(vb.dtype), vb)

    o_w = lax.map(win_block, jnp.arange(L // Q_BLOCK))
    o_w = jnp.moveaxis(o_w, 0, 1).reshape(B, L, G, R, d)

    g = jax.nn.sigmoid(gate.astype(jnp.float32)).reshape(B, L, G, R, 3).astype(x.dtype)
    o = g[..., 0:1] * o_c + g[..., 1:2] * o_s + g[..., 2:3] * o_w
    return o.reshape(B, L, H * d) @ w_out


def moe_ffn(x, w_router, b_router, w1, w3, w2):
    B, L, D = x.shape
    xt = x.reshape(B * L, D)
    n_tok = xt.shape[0]
    aff = jax.nn.sigmoid((xt @ w_router).astype(jnp.float32))
    sel = aff + b_router.astype(jnp.float32)
    grp = sel.reshape(n_tok, N_GROUPS, EXPERTS_PER_GROUP)
    grp_score = lax.top_k(grp, 2)[0].sum(-1)
    g_idx = jnp.argmax(grp_score, axis=-1)
    in_grp = jnp.take_along_axis(grp, g_idx[:, None, None], axis=1)[:, 0]
    _, loc = lax.top_k(in_grp, TOP_K)
    e_idx = g_idx[:, None] * EXPERTS_PER_GROUP + loc
    w_sel = jnp.take_along_axis(aff, e_idx, axis=1)
    w_sel = w_sel / jnp.sum(w_sel, axis=-1, keepdims=True)
    gates = jnp.sum(jax.nn.one_hot(e_idx, N_EXPERTS, dtype=jnp.float32) * w_sel[..., None], axis=1).astype(x.dtype)
    y = jnp.zeros_like(xt)
    for e in range(N_EXPERTS):
        h = jax.nn.silu(xt @ w1[e]) * (xt @ w3[e])
        y = y + gates[:, e:e + 1] * (h @ w2[e])
    return y.reshape(B, L, D)


def setup_inputs(seed: int = 0) -> dict:
    key = jax.random.key(seed)
    keys = iter(jax.random.split(key, 48))

    def nrm(shape, scale):
        return jax.random.normal(next(keys), shape, jnp.float32) * scale

    def gain():
        return 1.0 + nrm((D_MODEL,), 0.02)

    def bias():
        return nrm((D_MODEL,), 0.02)

    D = D_MODEL
    nsa_in = NSA_HEADS * NSA_HEAD_DIM + 6 * NSA_KV_HEADS * NSA_HEAD_DIM + 3 * NSA_HEADS
    inp = {}
    inp['x'] = nrm((BATCH, SEQ, D), 1.0)
    inp['w_in_0'] = nrm((D, 3 * SB_WIDTH + SSM_WIDTH), D ** -0.5)
    inp['ssm_lam_re'] = -0.5 + nrm((SSM_GROUPS, SSM_STATE), 0.01)
    inp['ssm_lam_im'] = math.pi * jnp.arange(SSM_STATE, dtype=jnp.float32)[None, :] + nrm((SSM_GROUPS, SSM_STATE), 0.01)
    inp['ssm_log_dt'] = jax.random.uniform(next(keys), (SSM_GROUPS,), jnp.float32, math.log(1e-3), math.log(1e-1))
    inp['ssm_b_re'] = nrm((SSM_GROUPS, SSM_STATE, SSM_GROUP), (2 * SSM_GROUP) ** -0.5)
    inp['ssm_b_im'] = nrm((SSM_GROUPS, SSM_STATE, SSM_GROUP), (2 * SSM_GROUP) ** -0.5)
    inp['ssm_c_re'] = nrm((SSM_GROUPS, SSM_GROUP, SSM_STATE), SSM_STATE ** -0.5)
    inp['ssm_c_im'] = nrm((SSM_GROUPS, SSM_GROUP, SSM_STATE), SSM_STATE ** -0.5)
    inp['ssm_d'] = 1.0 + nrm((SSM_WIDTH,), 0.1)
    inp['w_glu'] = nrm((SSM_WIDTH, SSM_WIDTH), SSM_WIDTH ** -0.5)
    inp['w_out_0'] = nrm((D, D), BETA * D ** -0.5)
    inp['ln_mix_g_0'] = gain()
    inp['ln_mix_b_0'] = bias()
    inp['ln_ffn_g_0'] = gain()
    inp['ln_ffn_b_0'] = bias()
    inp['w1_0'] = nrm((N_EXPERTS, D, D_EXPERT), D ** -0.5)
    inp['w3_0'] = nrm((N_EXPERTS, D, D_EXPERT), D ** -0.5)
    inp['w2_0'] = nrm((N_EXPERTS, D_EXPERT, D), BETA * D_EXPERT ** -0.5)
    inp['w_in_1'] = nrm((D, nsa_in), D ** -0.5)
    inp['cmp_pos_k'] = nrm((CMP_LEN, NSA_HEAD_DIM), 0.1)
    inp['cmp_w1_k'] = nrm((CMP_LEN * NSA_HEAD_DIM, CMP_HIDDEN), (CMP_LEN * NSA_HEAD_DIM) ** -0.5)
    inp['cmp_w2_k'] = nrm((CMP_HIDDEN, NSA_HEAD_DIM), CMP_HIDDEN ** -0.5)
    inp['cmp_pos_v'] = nrm((CMP_LEN, NSA_HEAD_DIM), 0.1)
    inp['cmp_w1_v'] = nrm((CMP_LEN * NSA_HEAD_DIM, CMP_HIDDEN), (CMP_LEN * NSA_HEAD_DIM) ** -0.5)
    inp['cmp_w2_v'] = nrm((CMP_HIDDEN, NSA_HEAD_DIM), CMP_HIDDEN ** -0.5)
    inp['w_out_1'] = nrm((NSA_HEADS * NSA_HEAD_DIM, D), BETA * (NSA_HEADS * NSA_HEAD_DIM) ** -0.5)
    inp['ln_mix_g_1'] = gain()
    inp['ln_mix_b_1'] = bias()
    inp['ln_ffn_g_1'] = gain()
    inp['ln_ffn_b_1'] = bias()
    inp['w1_1'] = nrm((N_EXPERTS, D, D_EXPERT), D ** -0.5)
    inp['w3_1'] = nrm((N_EXPERTS, D, D_EXPERT), D ** -0.5)
    inp['w2_1'] = nrm((N_EXPERTS, D_EXPERT, D), BETA * D_EXPERT ** -0.5)
    inp['w_router'] = nrm((D, N_EXPERTS), D ** -0.5)
    inp['b_router'] = nrm((N_EXPERTS,), 0.01)
    return inp


def reference(x, w_in_0, ssm_lam_re, ssm_lam_im, ssm_log_dt, ssm_b_re, ssm_b_im, ssm_c_re, ssm_c_im, ssm_d,
              w_glu, w_out_0, ln_mix_g_0, ln_mix_b_0, ln_ffn_g_0, ln_ffn_b_0, w1_0, w3_0, w2_0,
              w_in_1, cmp_pos_k, cmp_w1_k, cmp_w2_k, cmp_pos_v, cmp_w1_v, cmp_w2_v, w_out_1,
              ln_mix_g_1, ln_mix_b_1, ln_ffn_g_1, ln_ffn_b_1, w1_1, w3_1, w2_1,
              w_router, b_router):
    even_params = (w_in_0, ssm_lam_re, ssm_lam_im, ssm_log_dt, ssm_b_re, ssm_b_im, ssm_c_re, ssm_c_im,
                   ssm_d, w_glu, w_out_0)
    odd_params = (w_in_1, cmp_pos_k, cmp_w1_k, cmp_w2_k, cmp_pos_v, cmp_w1_v, cmp_w2_v, w_out_1)
    ln_params = ((ln_mix_g_0, ln_mix_b_0, ln_ffn_g_0, ln_ffn_b_0),
                 (ln_mix_g_1, ln_mix_b_1, ln_ffn_g_1, ln_ffn_b_1))
    experts = ((w1_0, w3_0, w2_0), (w1_1, w3_1, w2_1))
    h = x
    for layer in range(DEPTH):
        if layer % 2 == 0:
            m = even_mixer(h, *even_params)
        else:
            m = nsa_mixer(h, *odd_params)
        g_m, b_m, g_f, b_f = ln_params[layer]
        h = layer_norm(ALPHA * h + m, g_m, b_m)
        h = layer_norm(ALPHA * h + moe_ffn(h, w_router, b_router, *experts[layer]), g_f, b_f)
    return h
```

```python
import math
import numpy as np
from contextlib import ExitStack
import concourse.bass as bass
import concourse.mybir as mybir
from concourse.bass_utils import run_bass_kernel_spmd

F32 = mybir.dt.float32
BF16 = mybir.dt.bfloat16
I32 = mybir.dt.int32
AF = mybir.ActivationFunctionType
ALU = mybir.AluOpType
AX = mybir.AxisListType

L = 2048
D = 1024
NT = 16
KT = 8
ALPHA = (2 * 2) ** 0.25
LN_EPS = 1e-5
PI = math.pi
BIG = 30000.0
OFF = {}
_c = 0
for _n, _w in [("q", 1024), ("q_sw", 1024), ("ks", 256), ("ks_sw", 256), ("kw", 256), ("kw_sw", 256),
               ("kc", 256), ("vc", 256), ("vs", 256), ("vw", 256), ("gate", 48)]:
    OFF[_n] = _c
    _c += _w
W1ALL_COLS = _c
import os
NEXP = int(os.environ.get('DBG_NEXP', '16'))
MSTOP = int(os.environ.get('DBG_MSTOP', '9'))


class Ctx:
    NDSEM = 24

    def __init__(self, nc, stack):
        self.nc = nc
        self.eng = dict(pe=nc.tensor, act=nc.scalar, dve=nc.vector, pool=nc.gpsimd, sp=nc.sync)
        self.esem = {}
        self.ecnt = {}
        for e in self.eng:
            self.esem[e] = stack.enter_context(nc.semaphore("prog_" + e))
            self.ecnt[e] = 0
        self.dsem = [stack.enter_context(nc.semaphore("dma_%d" % i)) for i in range(self.NDSEM)]
        self.dcnt = [0] * self.NDSEM
        self.dnext = 0
        self.dnext_sw = 0
        self.seen = {}
        self.state = {}
        self.semobj = {}
        for e in self.eng:
            self.semobj[id(self.esem[e])] = self.esem[e]
        for s in self.dsem:
            self.semobj[id(s)] = s
        self.n_wait = 0
        self.n_ins = 0

    def _deps(self, reads, writes):
        deps = {}

        def add(tok):
            if tok is None:
                return
            sid, val = tok
            if deps.get(sid, 0) < val:
                deps[sid] = val
        for k in reads:
            st = self.state.get(k)
            if st:
                add(st[0])
        for k in writes:
            st = self.state.get(k)
            if st:
                add(st[0])
                for t in st[1]:
                    add(t)
        return deps

    def _wait(self, e, deps, skip_self_pe=False):
        engobj = self.eng[e]
        for sid, val in deps.items():
            if skip_self_pe and e == 'pe' and sid == id(self.esem['pe']):
                continue
            if self.seen.get((e, sid), 0) >= val:
                continue
            engobj.wait_ge(self.semobj[sid], val)
            self.seen[(e, sid)] = val
            self.n_wait += 1

    def _update(self, tok, reads, writes):
        for k in reads:
            st = self.state.setdefault(k, [None, []])
            st[1] = [t for t in st[1] if t[0] != tok[0]] + [tok]
        for k in writes:
            self.state[k] = [tok, []]

    def op(self, e, fn, reads=(), writes=(), chain=False):
        deps = self._deps(reads, writes)
        self._wait(e, deps, skip_self_pe=chain)
        ins = fn(self.eng[e])
        self.ecnt[e] += 1
        ins.then_inc(self.esem[e], 1)
        tok = (id(self.esem[e]), self.ecnt[e])
        self._update(tok, reads, writes)
        self.n_ins += 1
        return ins

    def dma(self, q, out, in_, reads=(), writes=(), **kw):
        deps = self._deps(reads, writes)
        half = self.NDSEM // 2
        if q == 'pool':
            i = half + self.dnext_sw
            self.dnext_sw = (self.dnext_sw + 1) % (self.NDSEM - half)
        else:
            i = self.dnext
            self.dnext = (self.dnext + 1) % half
        if self.dcnt[i] > 0:
            deps[id(self.dsem[i])] = max(deps.get(id(self.dsem[i]), 0), self.dcnt[i])
        self._wait(q, deps)
        ins = self.eng[q].dma_start(out=out, in_=in_, **kw)
        self.dcnt[i] += 16
        ins.then_inc(self.dsem[i], 16)
        tok = (id(self.dsem[i]), self.dcnt[i])
        self._update(tok, reads, writes)
        self.n_ins += 1
        return ins

    def barrier(self):
        deps = {}
        for e in self.eng:
            if self.ecnt[e]:
                deps[id(self.esem[e])] = self.ecnt[e]
        for i in range(self.NDSEM):
            if self.dcnt[i]:
                deps[id(self.dsem[i])] = self.dcnt[i]
        for e in self.eng:
            self._wait(e, dict(deps))
        self.state = {}


class K:
    pass


def _mm(C, out, lhsT, rhs, start, stop, reads, writes, chain=True):
    return C.op('pe', lambda e: e.matmul(out, lhsT=lhsT, rhs=rhs, start=start, stop=stop),
                reads=reads, writes=writes, chain=chain)


def build(upto="all", debug=()):
    nc = bass.Bass("TRN2", target_bir_lowering=False)
    din = {}

    def dram_in(name, shape, dt=F32):
        din[name] = nc.dram_tensor(name, list(shape), dt, kind="ExternalInput").ap()
        return din[name]

    x_d = dram_in("x", [L, D])
    w_in_0 = dram_in("w_in_0", [D, 2048])
    lam_re_d = dram_in("lam_re_l", [128, 16])
    lam_im_d = dram_in("lam_im_l", [128, 16])
    logdt_d = dram_in("logdt_l", [128, 16])
    bre_d = dram_in("bre_l", [128, 16, 128])
    bim_d = dram_in("bim_l", [128, 16, 128])
    cre_d = dram_in("cre_l", [128, 16, 128])
    cim_d = dram_in("cim_l", [128, 16, 128])
    ssmd_d = dram_in("ssmd_l", [128, 4])
    w_glu_d = dram_in("w_glu", [512, 512])
    w_out_0 = dram_in("w_out_0", [D, D])
    ln_d = dram_in("ln_all", [8, D])
    w1_d = [dram_in("w1_0", [16, D, 512]), dram_in("w1_1", [16, D, 512])]
    w3_d = [dram_in("w3_0", [16, D, 512]), dram_in("w3_1", [16, D, 512])]
    w2_d = [dram_in("w2_0", [16, 512, D]), dram_in("w2_1", [16, 512, D])]
    w_router_d = dram_in("w_router_l", [128, KT * 16])
    b_router_d = dram_in("b_router", [1, 16])
    ident_d = dram_in("ident", [128, 128])
    triu_d = dram_in("triu_neg", [128, 128])
    w1all_d = dram_in("w1all", [D, W1ALL_COLS])
    cw1_d = [dram_in("cw1k_l", [128, 32, 256]), dram_in("cw1v_l", [128, 32, 256])]
    cpos_d = [dram_in("cposk_T", [128, 32]), dram_in("cposv_T", [128, 32])]
    cw2k_d = dram_in("cw2k_dup", [256, 128])
    cw2ksw_d = dram_in("cw2k_dup_sw", [256, 128])
    cw2v_d = dram_in("cw2v", [256, 64])
    w_out_1 = dram_in("w_out_1", [D, D])
    cosF_d = dram_in("cosF", [128, L])
    sinF_d = dram_in("sinF", [128, L])
    cosC_d = dram_in("cosC", [128, 128])
    sinC_d = dram_in("sinC", [128, 128])
    cmask_d = dram_in("cmask", [128, L])
    ovaug_d = dram_in("ovaug", [128, 33])
    forceb_d = dram_in("forceb", [128, NT, 32])
    esel_d = dram_in("esel", [128, NT, 128])
    caus_d = dram_in("caus", [128, 128])
    anti_d = dram_in("anti", [128, 128])
    rowmask_d = dram_in("rowmask", [128, 4])
    causs_d = dram_in("caus_s", [128, 128])
    hspill_d = nc.dram_tensor("hspill", [L, D], F32, kind="Internal").ap()
    out_d = nc.dram_tensor("out", [L, D], F32, kind="ExternalOutput").ap()
    dbg = {}
    for name, shape in debug:
        dbg[name] = nc.dram_tensor(name, list(shape), F32, kind="ExternalOutput").ap()

    with ExitStack() as st:
        C = Ctx(nc, st)

        sbn = [0]

        def sb(name, shape, dt=F32, stack=st):
            sbn[0] += 1
            return stack.enter_context(nc.sbuf_tensor("s%d_%s" % (sbn[0], name), list(shape), dt))

        h = sb("h", [128, NT, D])
        ident = sb("ident", [128, 128])
        identb = sb("identb", [128, 128], BF16)
        PS = [st.enter_context(nc.psum_tensor("ps%d" % i, [128, 512], F32)) for i in range(8)]
        psrr = [0]

        bank_range = [0, 8]

        def bank(lo=None, hi=None):
            if lo is None:
                lo, hi = bank_range
            i = lo + psrr[0] % (hi - lo)
            psrr[0] += 1
            return PS[i], "ps%d" % i

        evac_rr = [0]

        def evac(out, in_, reads, writes, eng=None, scale=None):
            if eng is None:
                eng = 'act' if evac_rr[0] % 2 == 0 else 'dve'
                evac_rr[0] += 1
            if eng == 'act':
                if scale is None:
                    C.op('act', lambda e: e.activation(out=out, in_=in_, func=AF.Copy), reads=reads, writes=writes)
                else:
                    C.op('act', lambda e: e.activation(out=out, in_=in_, func=AF.Copy, scale=scale), reads=reads, writes=writes)
            else:
                if scale is None:
                    C.op('dve', lambda e: e.tensor_copy(out=out, in_=in_), reads=reads, writes=writes)
                else:
                    C.op('dve', lambda e: e.tensor_scalar(out=out, in0=in_, scalar1=scale, scalar2=None, op0=ALU.mult), reads=reads, writes=writes)

        hb = h[:, :, :].rearrange("p t d -> p (t d)").bitcast(BF16)

        def reload_h(src):
            for t in range(NT):
                C.dma('sp', h[:, t, :], src[t * 128:(t + 1) * 128, :], writes=[('h', t)])

        C.dma('sp', ident[:], ident_d[:, :], writes=['ident'])
        C.op('dve', lambda e: e.tensor_copy(out=identb[:], in_=ident[:]), reads=['ident'], writes=['identb'])
        for t in range(NT):
            C.dma('sp', h[:, t, :], x_d[t * 128:(t + 1) * 128, :], writes=[('h', t)])

        def to_feature_major(xT, tag, router=None):
            pend = []
            for t in range(NT):
                for half in range(2):
                    ps, pk = bank()
                    for kk in range(4):
                        k = half * 4 + kk
                        C.op('pe', lambda e: e.transpose(out=ps[:, kk * 128:(kk + 1) * 128], in_=h[:, t, k * 128:(k + 1) * 128], identity=ident[:]),
                             reads=[('h', t), 'ident'], writes=[pk], chain=True)
                    evac(xT[:, half * 4:half * 4 + 4, t * 128:(t + 1) * 128],
                         ps[:, :].rearrange("p (k n) -> p k n", k=4), reads=[pk], writes=[(tag, t)])
                    if router is not None:
                        pend.append(router(t, half, ps, pk))
                        if len(pend) > 2:
                            pend.pop(0)()
            for th_ in pend:
                th_()

        def layer_norm_tile(t, lnidx, lnp):
            i = t % 2
            stats, mv, rstd, nmr = K.stats[i], K.mv[i], K.rstd[i], K.nmr[i]
            sk = 'ln%d_' % i
            C.op('dve', lambda e: e.bn_stats(out=stats[:, 0, :], in_=h[:, t, 0:512]), reads=[('h', t)], writes=[sk + 'stats'])
            C.op('dve', lambda e: e.bn_stats(out=stats[:, 1, :], in_=h[:, t, 512:1024]), reads=[('h', t)], writes=[sk + 'stats2'])
            C.op('dve', lambda e: e.bn_aggr(out=mv[:, :], in_=stats[:, :, :].rearrange("p a b -> p (a b)")), reads=[sk + 'stats', sk + 'stats2'], writes=[sk + 'mv'])
            C.op('dve', lambda e: e.tensor_scalar(out=rstd[:, :], in0=mv[:, 1:2], scalar1=LN_EPS, scalar2=None, op0=ALU.add), reads=[sk + 'mv'], writes=[sk + 'rstd'])
            C.op('act', lambda e: e.activation(out=rstd[:, :], in_=rstd[:, :], func=AF.Sqrt), reads=[sk + 'rstd'], writes=[sk + 'rstd'])
            C.op('dve', lambda e: e.reciprocal(out=rstd[:, :], in_=rstd[:, :]), reads=[sk + 'rstd'], writes=[sk + 'rstd'])
            C.op('dve', lambda e: e.tensor_scalar(out=nmr[:, :], in0=mv[:, 0:1], scalar1=rstd[:, 0:1], scalar2=-1.0, op0=ALU.mult, op1=ALU.mult),
                 reads=[sk + 'mv', sk + 'rstd'], writes=[sk + 'nmr'])
            C.op('act', lambda e: e.activation(out=h[:, t, :], in_=h[:, t, :], func=AF.Identity, scale=rstd[:, 0:1], bias=nmr[:, 0:1]),
                 reads=[('h', t), sk + 'rstd', sk + 'nmr'], writes=[('h', t)])
            C.op('dve', lambda e: e.tensor_tensor(out=h[:, t, :], in0=h[:, t, :], in1=lnp[:, 0, :], op=ALU.mult), reads=[('h', t), ('lnp', lnidx)], writes=[('h', t)])
            C.op('dve', lambda e: e.tensor_tensor(out=h[:, t, :], in0=h[:, t, :], in1=lnp[:, 1, :], op=ALU.add), reads=[('h', t), ('lnp', lnidx)], writes=[('h', t)])

        def load_ln(lnp, lnidx):
            C.dma('sp', lnp[:, 0, :], ln_d[2 * lnidx:2 * lnidx + 1, :].broadcast_to([128, D]), writes=[('lnp', lnidx)])
            C.dma('sp', lnp[:, 1, :], ln_d[2 * lnidx + 1:2 * lnidx + 2, :].broadcast_to([128, D]), writes=[('lnp', lnidx)])

        K.stats = [sb("stats%d" % i, [128, 2, 6]) for i in range(2)]
        K.mv = [sb("mv%d" % i, [128, 2]) for i in range(2)]
        K.rstd = [sb("rstd%d" % i, [128, 1]) for i in range(2)]
        K.nmr = [sb("nmr%d" % i, [128, 1]) for i in range(2)]

        def dump(name, src_ap, reads):
            if name in dbg:
                if len(src_ap.shape) == 3 and src_ap.shape[2] > 1024:
                    for i in range(src_ap.shape[1]):
                        for c in range(0, src_ap.shape[2], 1024):
                            C.dma('pool', dbg[name][:, i, c:c + 1024], src_ap[:, i, c:c + 1024], reads=reads)
                else:
                    C.dma('pool', dbg[name], src_ap, reads=reads)

        def load_w_chunk(wbuf, key, src, c0, ncols):
            C.dma('pool', wbuf[:, :, 0:ncols], src[:, c0:c0 + ncols].rearrange("(kt p) n -> p kt n", p=128), writes=[key])

        def proj_fm(xT, wbuf, wkey, ncols, dst, dst_tile0, dkey, scale=None, xkey='xT', evac_fn=None):
            for f in range(ncols // 128):
                for tb in range(4):
                    ps, pk = bank()
                    for k in range(KT):
                        _mm(C, ps[:, :], wbuf[:, k, f * 128:(f + 1) * 128], xT[:, k, tb * 512:(tb + 1) * 512], k == 0, k == KT - 1,
                            reads=[wkey] + [(xkey, t) for t in range(tb * 4, tb * 4 + 4)], writes=[pk])
                    if evac_fn is not None:
                        evac_fn(f, tb, ps, pk)
                    else:
                        evac(dst[:, dst_tile0 + f, tb * 512:(tb + 1) * 512], ps[:, :], reads=[pk], writes=[(dkey, dst_tile0 + f, tb)], scale=scale)

        def proj_tm(xT, wbuf, wkey, ncols, dst, dkey, xkey='xT', col0=0):
            for t in range(NT):
                ps, pk = bank()
                for k in range(KT):
                    _mm(C, ps[:, 0:ncols], xT[:, k, t * 128:(t + 1) * 128], wbuf[:, k, 0:ncols], k == 0, k == KT - 1,
                        reads=[wkey, (xkey, t)], writes=[pk])
                evac(dst[:, t, col0:col0 + ncols], ps[:, 0:ncols], reads=[pk], writes=[(dkey, t)])

        def layer0_mixer():
            with ExitStack() as s0:
                ocat = sb("ocat", [128, 8, L], BF16, s0)
                qT = hb[:, 0:8192].rearrange("p (a n) -> p a n", a=4)
                kTz = hb[:, 8192:24576].rearrange("p (a n) -> p a n", a=8)
                v = hb[:, 24576:32768].rearrange("p (t n) -> p t n", t=NT)
                s2 = s0
                sba_gen, sba_n = sb_attention(s2, qT, kTz, v, ocat)
                with ExitStack() as s1:
                    uT = sb("uT", [128, 4, L], BF16, s1)
                    with ExitStack() as s1a:
                        xT = sb("xT", [128, KT, L], BF16, s1a)
                        wbuf = [sb("w0b%d" % i, [128, KT, 512], BF16, s1a) for i in range(2)]
                        load_w_chunk(wbuf[0], 'w0b0', w_in_0, 1536, 512)
                        load_w_chunk(wbuf[1], 'w0b1', w_in_0, 0, 512)
                        to_feature_major(xT, 'xT')
                        proj_fm(xT, wbuf[0], 'w0b0', 512, uT, 0, 'uT')
                        C.barrier()
                        load_w_chunk(wbuf[0], 'w0b0', w_in_0, 512, 512)
                        for c4 in range(4):
                            C.op('pool', lambda e: e.memset(hb[:, 8192 + c4 * 4096:8192 + (c4 + 1) * 4096], 0.0), writes=[('kTz0', c4)])
                        proj_fm(xT, wbuf[1], 'w0b1', 512, qT, 0, 'qT', scale=0.125)
                        load_w_chunk(wbuf[1], 'w0b1', w_in_0, 1024, 512)

                        def k_evac(f, tb, ps, pk):
                            cols = slice(tb * 512, (tb + 1) * 512)
                            zk = [('kTz0', c4) for c4 in range(4)]
                            evac(kTz[0:64, 2 * f, cols], ps[0:64, :], reads=[pk] + zk, writes=[('kTz', 2 * f, tb)], eng='act')
                            evac(kTz[64:128, 2 * f + 1, cols], ps[64:128, :], reads=[pk] + zk, writes=[('kTz', 2 * f + 1, tb)], eng='dve')
                        proj_fm(xT, wbuf[0], 'w0b0', 512, kTz, 0, 'kTz', evac_fn=k_evac)
                        proj_tm(xT, wbuf[1], 'w0b1', 512, v, 'v')
                        C.barrier()
                    ssm(s1, uT, ocat, co=(None if upto == "ssm" else sba_gen), co_per_iter=5)
                    C.barrier()
                bank_range[:] = [0, 3]
                dump("dbg_ocat", ocat[:, :, :], [])
                if upto == "ssm":
                    bank_range[:] = [0, 8]
                    return
                with ExitStack() as s3:
                    wo = sb("wo", [128, KT, D], BF16, s3)
                    C.dma('pool', wo[:, :, 0:512], w_out_0[:, 0:512].rearrange("(kt p) n -> p kt n", p=128), writes=['wo'])
                    C.dma('pool', wo[:, :, 512:1024], w_out_0[:, 512:1024].rearrange("(kt p) n -> p kt n", p=128), writes=['wo2'])
                    lnp = sb("lnp", [128, 2, D], F32, s3)
                    load_ln(lnp, 0)
                    for _ in sba_gen:
                        pass
                    C.barrier()
                    bank_range[:] = [0, 8]
                    dump("dbg_ocat", ocat[:, :, :], [])
                    if upto == "attn":
                        return
                    reload_h(x_d)
                    out_proj_ln(ocat, wo, ['wo', 'wo2'], 0, lnp)
                    C.barrier()

        def out_proj_ln(catT, wo, wkeys, lnidx, lnp):
            for t in range(NT):
                for half in range(2):
                    ps, pk = bank()
                    for k in range(KT):
                        _mm(C, ps[:, :], catT[:, k, t * 128:(t + 1) * 128], wo[:, k, half * 512:(half + 1) * 512], k == 0, k == KT - 1,
                            reads=wkeys, writes=[pk])
                    C.op('dve', lambda e: e.scalar_tensor_tensor(out=h[:, t, half * 512:(half + 1) * 512], in0=h[:, t, half * 512:(half + 1) * 512],
                                                                 scalar=ALPHA, in1=ps[:, :], op0=ALU.mult, op1=ALU.add),
                         reads=[pk, ('h', t)], writes=[('h', t)])
                layer_norm_tile(t, lnidx, lnp)

        def sincos(s1, ang, n, sin_out, cos_out, tag):
            tmp = sb("sc_tmp_" + tag, [128, n], F32, s1)
            ki = sb("sc_ki_" + tag, [128, n], I32, s1)
            kf = sb("sc_kf_" + tag, [128, n], F32, s1)
            for which, outp in ((0, sin_out), (1, cos_out)):
                off = 0.0 if which == 0 else PI / 2
                C.op('dve', lambda e: e.tensor_scalar(out=tmp[:, :], in0=ang, scalar1=off, scalar2=1.0 / (2 * PI), op0=ALU.add, op1=ALU.mult),
                     reads=['ang_' + tag], writes=['sc_tmp'])
                C.op('dve', lambda e: e.tensor_copy(out=ki[:, :], in_=tmp[:, :]), reads=['sc_tmp'], writes=['sc_ki'])
                C.op('dve', lambda e: e.tensor_copy(out=kf[:, :], in_=ki[:, :]), reads=['sc_ki'], writes=['sc_kf'])
                C.op('dve', lambda e: e.tensor_scalar(out=kf[:, :], in0=kf[:, :], scalar1=-2 * PI, scalar2=off, op0=ALU.mult, op1=ALU.add),
                     reads=['sc_kf'], writes=['sc_kf'])
                C.op('dve', lambda e: e.tensor_tensor(out=tmp[:, :], in0=ang, in1=kf[:, :], op=ALU.add), reads=['sc_kf', 'ang_' + tag], writes=['sc_tmp'])
                C.op('dve', lambda e: e.tensor_scalar(out=kf[:, :], in0=tmp[:, :], scalar1=PI, scalar2=-2 * PI, op0=ALU.is_gt, op1=ALU.mult), reads=['sc_tmp'], writes=['sc_kf'])
                C.op('dve', lambda e: e.tensor_tensor(out=tmp[:, :], in0=tmp[:, :], in1=kf[:, :], op=ALU.add), reads=['sc_kf', 'sc_tmp'], writes=['sc_tmp'])
                C.op('dve', lambda e: e.tensor_scalar(out=kf[:, :], in0=tmp[:, :], scalar1=-PI, scalar2=2 * PI, op0=ALU.is_lt, op1=ALU.mult), reads=['sc_tmp'], writes=['sc_kf'])
                C.op('dve', lambda e: e.tensor_tensor(out=tmp[:, :], in0=tmp[:, :], in1=kf[:, :], op=ALU.add), reads=['sc_kf', 'sc_tmp'], writes=['sc_tmp'])
                C.op('dve', lambda e: e.tensor_scalar(out=tmp[:, :], in0=tmp[:, :], scalar1=-PI, scalar2=PI, op0=ALU.max, op1=ALU.min), reads=['sc_tmp'], writes=['sc_tmp'])
                C.op('act', lambda e: e.activation(out=outp, in_=tmp[:, :], func=AF.Sin), reads=['sc_tmp'], writes=['sc_out_%s_%d' % (tag, which)])

        def ssm(s1, uT, ocat, co=None, co_per_iter=0):
            T = 128
            NCH = L // T
            lam_re = sb("lam_re", [128, 16], F32, s1)
            lam_im = sb("lam_im", [128, 16], F32, s1)
            dt = sb("dt", [128, 16], F32, s1)
            mag = sb("mag", [128, 16], F32, s1)
            th = sb("th", [128, 16], F32, s1)
            C.dma('sp', lam_re[:, :], lam_re_d[:, :], writes=['lam_re'])
            C.dma('sp', lam_im[:, :], lam_im_d[:, :], writes=['lam_im'])
            C.dma('sp', dt[:, :], logdt_d[:, :], writes=['dt'])
            C.op('act', lambda e: e.activation(out=dt[:, :], in_=dt[:, :], func=AF.Exp), reads=['dt'], writes=['dt'])
            C.op('dve', lambda e: e.tensor_tensor(out=mag[:, :], in0=lam_re[:, :], in1=dt[:, :], op=ALU.mult), reads=['lam_re', 'dt'], writes=['mag'])
            C.op('act', lambda e: e.activation(out=mag[:, :], in_=mag[:, :], func=AF.Exp), reads=['mag'], writes=['mag'])
            C.op('dve', lambda e: e.tensor_tensor(out=th[:, :], in0=lam_im[:, :], in1=dt[:, :], op=ALU.mult), reads=['lam_im', 'dt'], writes=['ang_th'])
            sn = sb("sn", [128, 16], F32, s1)
            cs = sb("cs", [128, 16], F32, s1)
            with ExitStack() as sx:
                sincos(sx, th[:, :], 16, sn[:, :], cs[:, :], 'th')
                C.barrier()
            a_re = sb("a_re", [128, 16], F32, s1)
            a_im = sb("a_im", [128, 16], F32, s1)
            C.op('dve', lambda e: e.tensor_tensor(out=a_re[:, :], in0=mag[:, :], in1=cs[:, :], op=ALU.mult), reads=['mag', 'sc_out_th_1'], writes=['a_re'])
            C.op('dve', lambda e: e.tensor_tensor(out=a_im[:, :], in0=mag[:, :], in1=sn[:, :], op=ALU.mult), reads=['mag', 'sc_out_th_0'], writes=['a_im'])
            den = sb("den", [128, 16], F32, s1)
            t1 = sb("t1", [128, 16], F32, s1)
            t2 = sb("t2", [128, 16], F32, s1)
            f_re = sb("f_re", [128, 16], F32, s1)
            f_im = sb("f_im", [128, 16], F32, s1)
            nr = sb("nr", [128, 16], F32, s1)
            V = lambda fn, r, w: C.op('dve', fn, reads=r, writes=w)
            V(lambda e: e.tensor_tensor(out=den[:, :], in0=lam_re[:, :], in1=lam_re[:, :], op=ALU.mult), ['lam_re'], ['den'])
            V(lambda e: e.tensor_tensor(out=t1[:, :], in0=lam_im[:, :], in1=lam_im[:, :], op=ALU.mult), ['lam_im'], ['t1'])
            V(lambda e: e.tensor_tensor(out=den[:, :], in0=den[:, :], in1=t1[:, :], op=ALU.add), ['den', 't1'], ['den'])
            V(lambda e: e.reciprocal(out=den[:, :], in_=den[:, :]), ['den'], ['den'])
            V(lambda e: e.tensor_scalar(out=nr[:, :], in0=a_re[:, :], scalar1=-1.0, scalar2=None, op0=ALU.add), ['a_re'], ['nr'])
            V(lambda e: e.tensor_tensor(out=t1[:, :], in0=nr[:, :], in1=lam_re[:, :], op=ALU.mult), ['nr', 'lam_re', 't1'], ['t1'])
            V(lambda e: e.tensor_tensor(out=t2[:, :], in0=a_im[:, :], in1=lam_im[:, :], op=ALU.mult), ['a_im', 'lam_im'], ['t2'])
            V(lambda e: e.tensor_tensor(out=t1[:, :], in0=t1[:, :], in1=t2[:, :], op=ALU.add), ['t1', 't2'], ['t1'])
            V(lambda e: e.tensor_tensor(out=f_re[:, :], in0=t1[:, :], in1=den[:, :], op=ALU.mult), ['t1', 'den'], ['f_re'])
            V(lambda e: e.tensor_tensor(out=t1[:, :], in0=a_im[:, :], in1=lam_re[:, :], op=ALU.mult), ['a_im', 'lam_re', 't1'], ['t1'])
            V(lambda e: e.tensor_tensor(out=t2[:, :], in0=nr[:, :], in1=lam_im[:, :], op=ALU.mult), ['nr', 'lam_im', 't2'], ['t2'])
            V(lambda e: e.tensor_tensor(out=t1[:, :], in0=t1[:, :], in1=t2[:, :], op=ALU.subtract), ['t1', 't2'], ['t1'])
            V(lambda e: e.tensor_tensor(out=f_im[:, :], in0=t1[:, :], in1=den[:, :], op=ALU.mult), ['t1', 'den'], ['f_im'])
            iot = sb("iot", [128, T], F32, s1)
            C.op('pool', lambda e: e.iota(iot[:, :], pattern=[[1, T]], base=0, channel_multiplier=0, allow_small_or_imprecise_dtypes=True), writes=['iot'])
            cosT = sb("cosT", [128, 16, T], F32, s1)
            sinT = sb("sinT", [128, 16, T], F32, s1)
            with ExitStack() as sx:
                ang = sb("ang", [128, 16, T], F32, sx)
                for j in range(16):
                    V(lambda e: e.tensor_scalar(out=ang[:, j, :], in0=iot[:, :], scalar1=th[:, j:j + 1], scalar2=None, op0=ALU.mult), ['iot', 'ang_th'], ['ang_tab'])
                sincos(sx, ang[:, :, :].rearrange("p a b -> p (a b)"), 16 * T, sinT[:, :, :].rearrange("p a b -> p (a b)"),
                       cosT[:, :, :].rearrange("p a b -> p (a b)"), 'tab')
                C.barrier()
            Rre = sb("Rre", [128, 16, T], F32, s1)
            Rim = sb("Rim", [128, 16, T], F32, s1)
            tmpT = sb("tmpT", [128, T], F32, s1)
            for j in range(16):
                V(lambda e: e.tensor_scalar(out=tmpT[:, :], in0=sinT[:, j, :], scalar1=f_im[:, j:j + 1], scalar2=None, op0=ALU.mult), ['sc_out_tab_0', 'f_im', 'tmpT'], ['tmpT'])
                V(lambda e: e.scalar_tensor_tensor(out=Rre[:, j, :], in0=cosT[:, j, :], scalar=f_re[:, j:j + 1], in1=tmpT[:, :], op0=ALU.mult, op1=ALU.add),
                  ['sc_out_tab_1', 'f_re', 'tmpT'], ['Rre'])
                V(lambda e: e.tensor_scalar(out=tmpT[:, :], in0=sinT[:, j, :], scalar1=f_re[:, j:j + 1], scalar2=None, op0=ALU.mult), ['sc_out_tab_0', 'f_re', 'tmpT'], ['tmpT'])
                V(lambda e: e.scalar_tensor_tensor(out=Rim[:, j, :], in0=cosT[:, j, :], scalar=f_im[:, j:j + 1], in1=tmpT[:, :], op0=ALU.mult, op1=ALU.subtract),
                  ['sc_out_tab_1', 'f_im', 'tmpT'], ['Rim'])
            angE = sb("angE", [128, 16], F32, s1)
            Ere = sb("Ere", [128, 16], F32, s1)
            Eim = sb("Eim", [128, 16], F32, s1)
            V(lambda e: e.tensor_scalar(out=angE[:, :], in0=th[:, :], scalar1=float(T), scalar2=None, op0=ALU.mult), ['ang_th'], ['ang_E'])
            with ExitStack() as sx:
                sincos(sx, angE[:, :], 16, Eim[:, :], Ere[:, :], 'E')
                C.barrier()
            MAGB = bool(int(os.environ.get('DBG_MAGB', '1')))
            if not MAGB:
                magT = sb("magT", [128, 16, T], F32, s1)
                for j in range(16):
                    V(lambda e: e.tensor_scalar(out=magT[:, j, :], in0=iot[:, :], scalar1=0.0, scalar2=mag[:, j:j + 1], op0=ALU.mult, op1=ALU.add), ['iot', 'mag'], ['magT'])
            breT = sb("breT", [128, 16, 128], BF16, s1)
            bimT = sb("bimT", [128, 16, 128], BF16, s1)
            creT = sb("creT", [128, 16, 128], BF16, s1)
            cimT = sb("cimT", [128, 16, 128], BF16, s1)
            C.dma('pool', breT[:, :, :], bre_d[:, :, :], writes=['breT'])
            C.dma('pool', bimT[:, :, :], bim_d[:, :, :], writes=['bimT'])
            C.dma('pool', creT[:, :, :], cre_d[:, :, :], writes=['creT'])
            C.dma('pool', cimT[:, :, :], cim_d[:, :, :], writes=['cimT'])
            C.op('pool', lambda e: e.tensor_scalar(out=cimT[:, :, :], in0=cimT[:, :, :], scalar1=-1.0, scalar2=None, op0=ALU.mult), reads=['cimT'], writes=['cimT'])
            ssmd = sb("ssmd", [128, 4], F32, s1)
            C.dma('sp', ssmd[:, :], ssmd_d[:, :], writes=['ssmd'])
            sw = ExitStack()
            NB = 2
            bt_re = [sb("bt_re%d" % i, [128, 4, T], F32, sw) for i in range(NB)]
            bt_im = [sb("bt_im%d" % i, [128, 4, T], F32, sw) for i in range(NB)]
            m1 = [sb("m1_%d" % i, [128, 4, T], F32, sw) for i in range(NB)]
            m2 = [sb("m2_%d" % i, [128, 4, T], F32, sw) for i in range(NB)]
            xt_re = [sb("xt_re%d" % i, [128, 4, T], F32, sw) for i in range(NB)]
            xt_im = [sb("xt_im%d" % i, [128, 4, T], F32, sw) for i in range(NB)]
            NBX = 3
            x_re = [sb("x_re%d" % i, [128, 4, T], BF16, sw) for i in range(NBX)]
            x_im = [sb("x_im%d" % i, [128, 4, T], BF16, sw) for i in range(NBX)]
            p1, p2 = m1, m2
            carry_re = sb("carry_re", [128, 16], F32, sw)
            carry_im = sb("carry_im", [128, 16], F32, sw)
            c1 = sb("c1", [128, 4], F32, sw)
            c2 = sb("c2", [128, 4], F32, sw)
            c3 = sb("c3", [128, 4], F32, sw)
            c4 = sb("c4", [128, 4], F32, sw)
            ytmp = [sb("ytmp%d" % i, [128, T], F32, sw) for i in range(3)]
            C.op('dve', lambda e: e.memset(carry_re[:, :], 0.0), writes=[('carry', g) for g in range(4)])
            C.op('dve', lambda e: e.memset(carry_im[:, :], 0.0), writes=[('carryi', g) for g in range(4)])
            fl = lambda tns: tns[:, :, :].rearrange("p a b -> p (a b)")
            G = lambda fn, r, w: C.op('pool', fn, reads=r, writes=w)
            gA = iot
            pend_g2 = []
            if os.environ.get('DBG_MEM'):
                print("SBUF remaining at SSM peak:", nc.sbuf_bytes_remaining)

            def make_iter(c, gq, it):
                b = it % NB
                bx = it % NBX
                yb = it % 3
                js = slice(4 * gq, 4 * gq + 4)
                kb = 'ssmbuf%d' % b
                kx = 'ssmx%d' % bx
                R_re = Rre[:, js, :].rearrange("p a b -> p (a b)")
                R_im = Rim[:, js, :].rearrange("p a b -> p (a b)")
                cT = cosT[:, js, :].rearrange("p a b -> p (a b)")
                sT = sinT[:, js, :].rearrange("p a b -> p (a b)")
                ucol = uT[:, gq, c * T:(c + 1) * T]
                ukey = ('uT', gq, c // 4)
                st_ = {}
                P1, P2 = [], []

                def t_mm():
                    st_['psr'], st_['pkr'] = PS[0], 'ps0'
                    st_['psi'], st_['pki'] = PS[1], 'ps1'
                    for jj in range(4):
                        j = 4 * gq + jj
                        _mm(C, st_['psr'][:, jj * T:(jj + 1) * T], breT[:, j, :], ucol, True, True, reads=['breT', ukey], writes=[st_['pkr']])
                        _mm(C, st_['psi'][:, jj * T:(jj + 1) * T], bimT[:, j, :], ucol, True, True, reads=['bimT', ukey], writes=[st_['pki']])
                P1.append(t_mm)
                P1.append(lambda: V(lambda e: e.tensor_tensor(out=fl(m1[b]), in0=st_['psr'][:, :], in1=R_re, op=ALU.mult), [st_['pkr'], 'Rre'], [kb + 'm1']))
                P1.append(lambda: V(lambda e: e.tensor_tensor(out=fl(m2[b]), in0=st_['psi'][:, :], in1=R_im, op=ALU.mult), [st_['pki'], 'Rim'], [kb + 'm2']))
                P1.append(lambda: V(lambda e: e.tensor_tensor(out=fl(bt_re[b]), in0=fl(m1[b]), in1=fl(m2[b]), op=ALU.subtract), [kb + 'm1', kb + 'm2'], [kb + 'btre']))
                P1.append(lambda: V(lambda e: e.tensor_tensor(out=fl(m1[b]), in0=st_['psi'][:, :], in1=R_re, op=ALU.mult), [st_['pki'], 'Rre', kb + 'm1'], [kb + 'm1']))
                P1.append(lambda: V(lambda e: e.tensor_tensor(out=fl(m2[b]), in0=st_['psr'][:, :], in1=R_im, op=ALU.mult), [st_['pkr'], 'Rim', kb + 'm2'], [kb + 'm2']))
                P1.append(lambda: V(lambda e: e.tensor_tensor(out=fl(bt_im[b]), in0=fl(m1[b]), in1=fl(m2[b]), op=ALU.add), [kb + 'm1', kb + 'm2'], [kb + 'btim']))
                for jj in range(4):
                    def t_scan(jj=jj):
                        j = 4 * gq + jj
                        V(lambda e: e.tensor_tensor_scan(out=xt_re[b][:, jj, :], data0=(mag[:, j:j + 1].to_broadcast([128, T]) if MAGB else magT[:, j, :]), data1=bt_re[b][:, jj, :], initial=carry_re[:, j:j + 1], op0=ALU.mult, op1=ALU.add),
                          ['mag', kb + 'btre', ('carry', gq)], [kb + 'xtre'])
                        V(lambda e: e.tensor_tensor_scan(out=xt_im[b][:, jj, :], data0=(mag[:, j:j + 1].to_broadcast([128, T]) if MAGB else magT[:, j, :]), data1=bt_im[b][:, jj, :], initial=carry_im[:, j:j + 1], op0=ALU.mult, op1=ALU.add),
                          ['mag', kb + 'btim', ('carryi', gq)], [kb + 'xtim'])
                    P1.append(t_scan)
                if c < NCH - 1:
                    lr = xt_re[b][:, :, T - 1]
                    li = xt_im[b][:, :, T - 1]
                    P1.append(lambda: V(lambda e: e.tensor_tensor(out=c1[:, :], in0=lr, in1=Ere[:, js], op=ALU.mult), [kb + 'xtre', 'sc_out_E_1', 'c1'], ['c1']))
                    P1.append(lambda: V(lambda e: e.tensor_tensor(out=c2[:, :], in0=li, in1=Eim[:, js], op=ALU.mult), [kb + 'xtim', 'sc_out_E_0', 'c2'], ['c2']))
                    P1.append(lambda: V(lambda e: e.tensor_tensor(out=c3[:, :], in0=li, in1=Ere[:, js], op=ALU.mult), [kb + 'xtim', 'sc_out_E_1', 'c3'], ['c3']))
                    P1.append(lambda: V(lambda e: e.tensor_tensor(out=c4[:, :], in0=lr, in1=Eim[:, js], op=ALU.mult), [kb + 'xtre', 'sc_out_E_0', 'c4'], ['c4']))
                    P1.append(lambda: V(lambda e: e.tensor_tensor(out=carry_re[:, js], in0=c1[:, :], in1=c2[:, :], op=ALU.subtract), ['c1', 'c2', ('carry', gq)], [('carry', gq)]))
                    P1.append(lambda: V(lambda e: e.tensor_tensor(out=carry_im[:, js], in0=c3[:, :], in1=c4[:, :], op=ALU.add), ['c3', 'c4', ('carryi', gq)], [('carryi', gq)]))
                P2.append(lambda: G(lambda e: e.tensor_tensor(out=fl(p1[b]), in0=fl(xt_re[b]), in1=cT, op=ALU.mult), [kb + 'xtre', 'sc_out_tab_1'], [kb + 'm1']))
                P2.append(lambda: G(lambda e: e.tensor_tensor(out=fl(p2[b]), in0=fl(xt_im[b]), in1=sT, op=ALU.mult), [kb + 'xtim', 'sc_out_tab_0'], [kb + 'm2']))
                P2.append(lambda: V(lambda e: e.tensor_tensor(out=fl(x_re[bx]), in0=fl(p1[b]), in1=fl(p2[b]), op=ALU.subtract), [kb + 'm1', kb + 'm2'], [kx + 'xre']))
                P2.append(lambda: G(lambda e: e.tensor_tensor(out=fl(p1[b]), in0=fl(xt_re[b]), in1=sT, op=ALU.mult), [kb + 'xtre', 'sc_out_tab_0', kb + 'm1'], [kb + 'm1']))
                P2.append(lambda: G(lambda e: e.tensor_tensor(out=fl(p2[b]), in0=fl(xt_im[b]), in1=cT, op=ALU.mult), [kb + 'xtim', 'sc_out_tab_1', kb + 'm2'], [kb + 'm2']))
                P2.append(lambda: V(lambda e: e.tensor_tensor(out=fl(x_im[bx]), in0=fl(p1[b]), in1=fl(p2[b]), op=ALU.add), [kb + 'm1', kb + 'm2'], [kx + 'xim']))

                def t_y():
                    psy, pky = PS[2], 'ps2'
                    for jj in range(4):
                        j = 4 * gq + jj
                        _mm(C, psy[:, 0:T], creT[:, j, :], x_re[bx][:, jj, :], jj == 0, False, reads=['creT', kx + 'xre'], writes=[pky])
                        _mm(C, psy[:, 0:T], cimT[:, j, :], x_im[bx][:, jj, :], False, jj == 3, reads=['cimT', kx + 'xim'], writes=[pky])
                    V(lambda e: e.scalar_tensor_tensor(out=ytmp[yb][:, :], in0=ucol, scalar=ssmd[:, gq:gq + 1], in1=psy[:, 0:T],
                                                       op0=ALU.mult, op1=ALU.add), [pky, 'ssmd', ukey], ['ytmp%d' % yb])
                    if "dbg_ssm_y" in dbg:
                        C.dma('sp', dbg["dbg_ssm_y"][:, gq, c * T:(c + 1) * T], ytmp[yb][:, :], reads=['ytmp%d' % yb])

                def t_g():
                    yk = 'ytmp%d' % yb
                    yv = ytmp[yb][:, :]
                    V(lambda e: e.tensor_tensor(out=gA[:, :], in0=yv, in1=yv, op=ALU.mult), [yk, 'gA'], ['gA'])
                    V(lambda e: e.tensor_scalar(out=gA[:, :], in0=gA[:, :], scalar1=0.044715, scalar2=1.0, op0=ALU.mult, op1=ALU.add), ['gA'], ['gA'])
                    V(lambda e: e.tensor_tensor(out=gA[:, :], in0=gA[:, :], in1=yv, op=ALU.mult), [yk, 'gA'], ['gA'])
                    C.op('act', lambda e: e.activation(out=gA[:, :], in_=gA[:, :], func=AF.Exp, scale=-1.5957691216057308), reads=['gA'], writes=['gA'])

                    def t_g2():
                        V(lambda e: e.tensor_scalar(out=gA[:, :], in0=gA[:, :], scalar1=1.0, scalar2=None, op0=ALU.add), ['gA'], ['gA'])
                        V(lambda e: e.reciprocal(out=gA[:, :], in_=gA[:, :]), ['gA'], ['gA'])
                        V(lambda e: e.tensor_tensor(out=ocat[:, 4 + gq, c * T:(c + 1) * T], in0=yv, in1=gA[:, :], op=ALU.mult), [yk, 'gA'], [('ocat', 4 + gq, c // 4)])
                    pend_g2.append(t_g2)
                return P1, P2, t_y, t_g

            iters = [(c, gq) for c in range(NCH) for gq in range(4)]
            prev2 = None
            pend_y = []
            pend_g = []
            for it, (c, gq) in enumerate(iters):
                P1, P2, t_y, t_g = make_iter(c, gq, it)
                while pend_g2:
                    pend_g2.pop(0)()
                if len(pend_g) >= 2:
                    pend_g.pop(0)()
                if len(pend_y) >= 1:
                    ty_ = pend_y.pop(0)
                    ty_[0]()
                    pend_g.append(ty_[1])
                A_, B_ = P1, (prev2[0] if prev2 else [])
                nco = 0
                for k in range(max(len(A_), len(B_))):
                    if k < len(B_):
                        B_[k]()
                    if k < len(A_):
                        A_[k]()
                    if co is not None and k % 3 == 2 and nco < co_per_iter:
                        next(co, None)
                        nco += 1
                if prev2:
                    pend_y.append((prev2[1], prev2[2]))
                prev2 = (P2, t_y, t_g)
                if co is not None:
                    for _ in range(co_per_iter - nco):
                        next(co, None)
            for t_ in prev2[0]:
                t_()
            pend_y.append((prev2[1], prev2[2]))
            for ty_ in pend_y:
                while len(pend_g) >= 2:
                    while pend_g2:
                        pend_g2.pop(0)()
                    pend_g.pop(0)()
                ty_[0]()
                pend_g.append(ty_[1])
            for tg_ in pend_g:
                while pend_g2:
                    pend_g2.pop(0)()
                tg_()
            while pend_g2:
                pend_g2.pop(0)()
            C.barrier()
            sw.close()
            if co is not None:
                bank_range[:] = [0, 3]
            wg = sb("wg", [128, 4, 512], BF16, s1)
            C.dma('pool', wg[:, :, :], w_glu_d.rearrange("(kt p) n -> p kt n", p=128), writes=['wg'])
            sg = [sb("sg%d" % i, [128, 512], BF16, s1) for i in range(4)]
            for tb in range(4):
                for f in range(4):
                    ps, pk = bank()
                    for k in range(4):
                        _mm(C, ps[:, :], wg[:, k, f * 128:(f + 1) * 128], ocat[:, 4 + k, tb * 512:(tb + 1) * 512], k == 0, k == 3,
                            reads=['wg'] + [('ocat', 4 + g, tb) for g in range(4)], writes=[pk])
                    C.op('act', lambda e: e.activation(out=sg[f][:, :], in_=ps[:, :], func=AF.Sigmoid), reads=[pk], writes=['sg%d' % f])
                for f in range(4):
                    V(lambda e: e.tensor_tensor(out=ocat[:, 4 + f, tb * 512:(tb + 1) * 512], in0=ocat[:, 4 + f, tb * 512:(tb + 1) * 512], in1=sg[f][:, :], op=ALU.mult),
                      ['sg%d' % f, ('ocat', 4 + f, tb)], [('ocat', 4 + f, tb)])

        def sb_attention(s2, qT, kT, v, ocat):
            Uneg = sb("Uneg", [128, 128], BF16, s2)
            onesneg = sb("onesneg", [128, 128], BF16, s2)
            C.dma('pool', Uneg[:, :], triu_d[:, :], writes=['Uneg'])
            causs = sb("causs", [128, 128], BF16, s2)
            C.dma('pool', causs[:, :], causs_d[:, :], writes=['causs'])
            C.op('dve', lambda e: e.memset(onesneg[:, :], -1.0), writes=['onesneg'])
            SK1, SK2 = 2, 2
            NBE, NBS, NBW = 2, SK1 + 1, SK2 + 1
            ee = [sb("ee%d" % i, [128, 512], BF16, s2) for i in range(NBE)]
            sp = [sb("sp%d" % i, [128, 512], BF16, s2) for i in range(NBS)]
            ww = [sb("ww%d" % i, [128, 512], BF16, s2) for i in range(NBW)]
            Sb = [[sb("S%d_%d" % (i, j), [128, 512], BF16, s2) for j in range(2)] for i in range(2)]
            steps = []
            blk = 0
            for hd in range(8):
                for qb in range(4):
                    nkt = 4 * qb + 4
                    for i, kt in enumerate(range(nkt - 1, -1, -1)):
                        steps.append(dict(hd=hd, qb=qb, kt=kt, i=i, n=nkt, blk=blk))
                    blk += 1
            pso_of = {}

            def stageA(n):
                st_ = steps[n]
                hd, qb, kt = st_['hd'], st_['qb'], st_['kt']
                hp, ho = hd // 2, (hd % 2) * 64
                c0 = 128 * max(0, kt - 4 * qb)
                be, bs = n % NBE, n % NBS
                psa, pka = PS[3 + n % 3], 'ps%d' % (3 + n % 3)
                st_['psa'], st_['pka'] = psa, pka
                if st_['i'] == 0:
                    for j in range(2):
                        C.op('act', lambda e: e.memzero(Sb[st_['blk'] % 2][j][:, :]), reads=[], writes=[('S', st_['blk'] % 2, j)])
                qcols = slice(qb * 512 + c0, (qb + 1) * 512)
                diag = kt >= 4 * qb
                C.op('pe', lambda e: e.matmul(psa[:, c0:512], lhsT=kT[:, hd, kt * 128:(kt + 1) * 128], rhs=qT[:, hp, qcols], start=True, stop=(not diag), skip_group_check=True),
                     reads=[('kTz', hd, kt // 4), ('qT', hp, qb)], writes=[pka], chain=True)
                if diag:
                    C.op('pe', lambda e: e.matmul(psa[:, c0:c0 + 128], lhsT=identb[:, :], rhs=causs[:, :], start=False, stop=True, skip_group_check=True),
                         reads=['identb', 'causs'], writes=[pka], chain=True)
                C.op('act', lambda e: e.activation(out=ee[be][:, c0:512], in_=psa[:, c0:512], func=AF.Exp), reads=[pka], writes=['ee%d' % be])
                C.op('act', lambda e: e.activation(out=sp[bs][:, c0:512], in_=ee[be][:, c0:512], func=AF.Ln, bias=1.0), reads=['ee%d' % be], writes=['sp%d' % bs])

            def stageB1(n):
                st_ = steps[n]
                hd, qb, kt, i = st_['hd'], st_['qb'], st_['kt'], st_['i']
                c0 = 128 * max(0, kt - 4 * qb)
                bs, bw = n % NBS, n % NBW
                psa, pka = st_['psa'], st_['pka']
                Sprev = Sb[st_['blk'] % 2][(i + 1) % 2]
                Snew = Sb[st_['blk'] % 2][i % 2]
                kprev = ('S', st_['blk'] % 2, (i + 1) % 2)
                knew = ('S', st_['blk'] % 2, i % 2)
                C.op('pe', lambda e: e.matmul(psa[:, c0:512], lhsT=Uneg[:, :], rhs=sp[bs][:, c0:512], start=False, stop=(i == 0), skip_group_check=True),
                     reads=['Uneg', 'sp%d' % bs], writes=[pka], chain=True)
                if i > 0:
                    C.op('pe', lambda e: e.matmul(psa[:, c0:512], lhsT=onesneg[:, :], rhs=Sprev[:, c0:512], start=False, stop=True, skip_group_check=True),
                         reads=['onesneg', kprev], writes=[pka], chain=True)
                C.op('act', lambda e: e.activation(out=ww[bw][:, c0:512], in_=psa[:, c0:512], func=AF.Exp), reads=[pka], writes=['ww%d' % bw])
                if kt > 0:
                    C.op('pe', lambda e: e.matmul(PS[7][:, c0:512], lhsT=identb[:, :], rhs=sp[bs][:, c0:512], start=(i == 0), stop=False, skip_group_check=True),
                         reads=['identb', 'sp%d' % bs], writes=['ps7'], chain=True)
                    C.op('act', lambda e: e.activation(out=Snew[:, c0:512], in_=PS[7][:, c0:512], func=AF.Copy), reads=['ps7'], writes=[knew])

            def stageB2(n):
                st_ = steps[n]
                hd, qb, kt, i = st_['hd'], st_['qb'], st_['kt'], st_['i']
                hp, ho = hd // 2, (hd % 2) * 64
                c0 = 128 * max(0, kt - 4 * qb)
                bw = n % NBW
                if i == 0:
                    pso_of[st_['blk']] = (PS[6], 'ps6')
                pso, pko = pso_of[st_['blk']]
                C.op('pe', lambda e: e.matmul(pso[ho:ho + 64, c0:512], lhsT=v[:, kt, hd * 64:(hd + 1) * 64], rhs=ww[bw][:, c0:512], start=(i == 0), stop=(kt == 0), skip_group_check=True),
                     reads=[('v', kt), 'ww%d' % bw], writes=[pko], chain=True)
                if kt == 0:
                    evac(ocat[ho:ho + 64, hp, qb * 512:(qb + 1) * 512], pso[ho:ho + 64, :], reads=[pko], writes=[('ocat', hp, qb, ho)], eng='act')

            def gen():
                for n in range(len(steps) + SK1 + SK2):
                    if n < len(steps):
                        stageA(n)
                    if 0 <= n - SK1 < len(steps):
                        stageB1(n - SK1)
                    if 0 <= n - SK1 - SK2 < len(steps):
                        stageB2(n - SK1 - SK2)
                    yield n
            return gen(), len(steps) + SK1 + SK2

        def moe(layer, lnidx, final=False):
            with ExitStack() as s0:
                xT = sb("xTm", [128, KT, L], BF16, s0)
                wr = sb("wr", [128, KT, 16], F32, s0)
                br = sb("br", [128, 16], F32, s0)
                if not os.environ.get('DBG_NOWR'):
                    C.dma('sp', wr[:, :, :].rearrange("p a b -> p (a b)"), w_router_d[:, :], writes=['wr'])
                if not os.environ.get('DBG_NOBR'):
                    C.dma('sp', br[:, :], b_router_d[0:1, :].broadcast_to([128, 16]), writes=['br'])
                xf = [sb("xf%d" % i, [128, 4, 128], F32, s0) for i in range(4)]
                logit = sb("logit", [128, NT, 16], F32, s0)
                psl = PS[7]
                rr = [0]

                def router(t, half, ps, pk):
                    b = rr[0] % 4
                    rr[0] += 1
                    C.op('dve', lambda e: e.tensor_copy(out=xf[b][:, :, :], in_=ps[:, :].rearrange("p (k n) -> p k n", k=4)), reads=[], writes=[pk, 'xf%d' % b])

                    def mm():
                        for kk in range(4):
                            k = half * 4 + kk
                            C.op('pe', lambda e: e.matmul(psl[:, t * 16:(t + 1) * 16], lhsT=xf[b][:, kk, :], rhs=wr[:, k, :], start=(t == 0 and k == 0), stop=(k == 7), skip_group_check=True),
                                 reads=['xf%d' % b, 'wr'], writes=['psl'], chain=True)
                    return mm

                w1b = [sb("w1b%d" % i, [128, KT, 512], BF16, s0) for i in range(2)]
                w3b = [sb("w3b%d" % i, [128, KT, 512], BF16, s0) for i in range(2)]
                w2b = [sb("w2b%d" % i, [128, 4, D], BF16, s0) for i in range(2)]
                hT = sb("hTm", [128, 4, L], BF16, s0)
                sl = [sb("sl%d" % i, [128, 512], F32, s0) for i in range(2)]

                def load_expert(e_):
                    b = e_ % 2
                    C.dma('pool', w1b[b][:, :, :], w1_d[layer][e_].rearrange("(kt p) n -> p kt n", p=128), writes=['w1b%d' % b])
                    C.dma('pool', w3b[b][:, :, :], w3_d[layer][e_].rearrange("(kt p) n -> p kt n", p=128), writes=['w3b%d' % b])
                    C.dma('pool', w2b[b][:, :, :], w2_d[layer][e_].rearrange("(kt p) n -> p kt n", p=128), writes=['w2b%d' % b])

                lnp = sb("lnp", [128, 2, D], F32, s0)
                load_ln(lnp, lnidx)
                load_expert(0)
                bank_range[:] = [0, 7]
                to_feature_major(xT, 'xTm', router=(None if os.environ.get('DBG_NOROUTER') else router))
                V = lambda fn, r, w: C.op('dve', fn, reads=r, writes=w)
                if MSTOP <= 1:
                    C.barrier()
                    return
                aff = sb("aff", [128, NT, 16], F32, s0)
                sel = sb("sel", [128, NT, 16], F32, s0)
                C.op('act', lambda e: e.activation(out=aff[:, :, :].rearrange("p a b -> p (a b)"), in_=psl[:, 0:NT * 16], func=AF.Sigmoid), reads=['psl'], writes=['aff'])
                V(lambda e: e.tensor_tensor(out=sel[:, :, :], in0=aff[:, :, :], in1=br[:, :].unsqueeze(1).to_broadcast([128, NT, 16]), op=ALU.add), ['aff', 'br'], ['sel'])
                sel4 = sel[:, :, :].rearrange("p t (g e) -> p (t g) e", g=4)
                mx1 = sb("mx1", [128, NT * 4], F32, s0)
                mx2 = sb("mx2", [128, NT * 4], F32, s0)
                eq = sb("eq", [128, NT * 4, 4], F32, s0)
                V(lambda e: e.tensor_reduce(out=mx1[:, :], in_=sel4, axis=AX.X, op=ALU.max), ['sel'], ['mx1'])
                V(lambda e: e.tensor_tensor(out=eq[:, :, :], in0=sel4, in1=mx1[:, :].unsqueeze(2).to_broadcast([128, NT * 4, 4]), op=ALU.is_ge), ['sel', 'mx1'], ['eq'])
                V(lambda e: e.scalar_tensor_tensor(out=eq[:, :, :], in0=eq[:, :, :], scalar=-1000.0, in1=sel4, op0=ALU.mult, op1=ALU.add), ['eq', 'sel'], ['eq'])
                V(lambda e: e.tensor_reduce(out=mx2[:, :], in_=eq[:, :, :], axis=AX.X, op=ALU.max), ['eq'], ['mx2'])
                gs = sb("gs", [128, NT, 4], F32, s0)
                V(lambda e: e.tensor_tensor(out=gs[:, :, :].rearrange("p a b -> p (a b)"), in0=mx1[:, :], in1=mx2[:, :], op=ALU.add), ['mx1', 'mx2'], ['gs'])
                gmax = sb("gmax", [128, NT], F32, s0)
                V(lambda e: e.tensor_reduce(out=gmax[:, :], in_=gs[:, :, :], axis=AX.X, op=ALU.max), ['gs'], ['gmax'])
                gsel = sb("gsel", [128, NT, 4], F32, s0)
                V(lambda e: e.tensor_tensor(out=gsel[:, :, :], in0=gs[:, :, :], in1=gmax[:, :].unsqueeze(2).to_broadcast([128, NT, 4]), op=ALU.is_ge), ['gs', 'gmax'], ['gsel'])
                m2 = sb("m2", [128, NT * 4, 4], F32, s0)
                V(lambda e: e.tensor_tensor(out=m2[:, :, :], in0=sel4, in1=mx2[:, :].unsqueeze(2).to_broadcast([128, NT * 4, 4]), op=ALU.is_ge), ['sel', 'mx2'], ['m2'])
                V(lambda e: e.tensor_tensor(out=m2[:, :, :], in0=m2[:, :, :], in1=gsel[:, :, :].rearrange("p a b -> p (a b)").unsqueeze(2).to_broadcast([128, NT * 4, 4]), op=ALU.mult),
                  ['m2', 'gsel'], ['m2'])
                gates = sb("gates", [128, NT, 16], F32, s0)
                V(lambda e: e.tensor_tensor(out=gates[:, :, :].rearrange("p a b -> p (a b)"), in0=aff[:, :, :].rearrange("p a b -> p (a b)"),
                                            in1=m2[:, :, :].rearrange("p a b -> p (a b)"), op=ALU.mult), ['aff', 'm2'], ['gates'])
                gsum = sb("gsum", [128, NT], F32, s0)
                V(lambda e: e.tensor_reduce(out=gsum[:, :], in_=gates[:, :, :], axis=AX.X, op=ALU.add), ['gates'], ['gsum'])
                V(lambda e: e.reciprocal(out=gsum[:, :], in_=gsum[:, :]), ['gsum'], ['gsum'])
                V(lambda e: e.tensor_tensor(out=gates[:, :, :], in0=gates[:, :, :], in1=gsum[:, :].unsqueeze(2).to_broadcast([128, NT, 16]), op=ALU.mult), ['gates', 'gsum'], ['gates'])
                dump("dbg_gates%d" % layer, gates[:, :, :], ['gates'])
                if MSTOP <= 2:
                    C.barrier()
                    return
                for t in range(NT):
                    C.op('act', lambda e: e.activation(out=h[:, t, :], in_=h[:, t, :], func=AF.Copy, scale=ALPHA),
                         reads=[('h', t)], writes=[('h', t)])
                si = 0
                xk = [('xTm', t) for t in range(NT)]
                def up(e_, tb):
                    nonlocal si
                    b = e_ % 2
                    for f in range(4):
                        ps1, pk1 = bank()
                        ps3, pk3 = bank()
                        for k in range(KT):
                            _mm(C, ps1[:, :], w1b[b][:, k, f * 128:(f + 1) * 128], xT[:, k, tb * 512:(tb + 1) * 512], k == 0, k == KT - 1,
                                reads=['w1b%d' % b] + xk[tb * 4:tb * 4 + 4], writes=[pk1])
                        for k in range(KT):
                            _mm(C, ps3[:, :], w3b[b][:, k, f * 128:(f + 1) * 128], xT[:, k, tb * 512:(tb + 1) * 512], k == 0, k == KT - 1,
                                reads=['w3b%d' % b] + xk[tb * 4:tb * 4 + 4], writes=[pk3])
                        sbi = si % 2
                        si += 1
                        C.op('act', lambda e: e.activation(out=sl[sbi][:, :], in_=ps1[:, :], func=AF.Silu), reads=[pk1], writes=['sl%d' % sbi])
                        V(lambda e: e.tensor_tensor(out=hT[:, f, tb * 512:(tb + 1) * 512], in0=sl[sbi][:, :], in1=ps3[:, :], op=ALU.mult),
                          ['sl%d' % sbi, pk3], [('hTm', f, tb)])

                def down(e_, tb):
                    b = e_ % 2
                    for tt in range(4):
                        t = tb * 4 + tt
                        for half in range(2):
                            ps, pk = bank()
                            for f in range(4):
                                _mm(C, ps[:, :], hT[:, f, t * 128:(t + 1) * 128], w2b[b][:, f, half * 512:(half + 1) * 512], f == 0, f == 3,
                                    reads=['w2b%d' % b, ('hTm', f, tb)], writes=[pk])
                            V(lambda e: e.scalar_tensor_tensor(out=h[:, t, half * 512:(half + 1) * 512], in0=ps[:, :], scalar=gates[:, t, e_:e_ + 1],
                                                               in1=h[:, t, half * 512:(half + 1) * 512], op0=ALU.mult, op1=ALU.add),
                              [pk, 'gates', ('h', t)], [('h', t)])

                pairs = [(e_, tb) for e_ in range(NEXP) for tb in range(4)]
                for idx, (e_, tb) in enumerate(pairs):
                    if idx == 0 and NEXP > 1:
                        load_expert(1)
                    up(e_, tb)
                    if idx >= 1:
                        down(*pairs[idx - 1])
                    if tb == 0 and e_ >= 1 and e_ + 1 < NEXP:
                        load_expert(e_ + 1)
                down(*pairs[-1])
                for t in range(NT):
                    layer_norm_tile(t, lnidx, lnp)
                    if final:
                        C.dma('sp', out_d[t * 128:(t + 1) * 128, :], h[:, t, :], reads=[('h', t)])
                C.barrier()
            bank_range[:] = [0, 8]


        def nsa_mixer():
            V = lambda fn, r, w: C.op('dve', fn, reads=r, writes=w)
            G = lambda fn, r, w: C.op('pool', fn, reads=r, writes=w)
            A = lambda fn, r, w: C.op('act', fn, reads=r, writes=w)
            with ExitStack() as s0:
                xo = sb("xo", [128, NT * D], BF16, s0)
                obuf = xo[:, :].rearrange("p (t d) -> p t d", t=NT)
                wo = sb("wo1", [128, KT, D], BF16, s0)
                C.dma('pool', wo[:, :, 0:512], w_out_1[:, 0:512].rearrange("(kt p) n -> p kt n", p=128), writes=['wo'])
                C.dma('pool', wo[:, :, 512:1024], w_out_1[:, 512:1024].rearrange("(kt p) n -> p kt n", p=128), writes=['wo2'])
                lnp = sb("lnp", [128, 2, D], F32, s0)
                load_ln(lnp, 2)
                with ExitStack() as sp_:
                    qT = hb[:, 0:16384].rearrange("p (a n) -> p a n", a=8)
                    ksd = hb[:, 16384:24576].rearrange("p (a n) -> p a n", a=4)
                    kwd = hb[:, 24576:32768].rearrange("p (a n) -> p a n", a=4)
                    kvTz = hb[:, 0:16384].rearrange("p (a n) -> p a n", a=8)
                    vsa = sb("vsa", [128, NT, 4, 65], BF16, sp_)
                    vwa = sb("vwa", [128, NT, 4, 65], BF16, sp_)
                    gate = sb("gate", [128, NT, 48], F32, sp_)
                    kcmp = sb("kcmp", [128, 4, 128], BF16, sp_)
                    V(lambda e: e.memset(kcmp[:, :, :], 0.0), [], ['kcmp0'])
                    vca = sb("vca", [128, 4, 97], BF16, sp_)
                    cmask = sb("cmask", [128, L], BF16, sp_)
                    esel = sb("esel", [128, NT, 128], BF16, sp_)
                    caus = sb("caus", [128, 128], BF16, sp_)
                    anti = sb("anti", [128, 128], BF16, sp_)
                    forceb = sb("forceb", [128, NT, 32], F32, sp_)
                    for c in range(2):
                        C.dma('pool', cmask[:, c * 1024:(c + 1) * 1024], cmask_d[:, c * 1024:(c + 1) * 1024], writes=[('cmask', c)])
                        C.dma('pool', esel[:, c * 8:(c + 1) * 8, :], esel_d[:, c * 8:(c + 1) * 8, :], writes=[('esel', c)])
                    C.dma('pool', caus[:, :], caus_d[:, :], writes=['caus'])
                    C.dma('pool', anti[:, :], anti_d[:, :], writes=['anti'])
                    C.dma('sp', forceb[:, :, :], forceb_d[:, :, :], writes=['forceb'])
                    rowmask = sb("rowmask", [128, 4], F32, sp_)
                    C.dma('sp', rowmask[:, :], rowmask_d[:, :], writes=['rowmask'])
                    ovaug = sb("ovaug", [128, 33], F32, sp_)
                    C.dma('sp', ovaug[:, :], ovaug_d[:, :], writes=['ovaug'])
                    for g in range(4):
                        V(lambda e: e.tensor_copy(out=vca[:, g, 64:97], in_=ovaug[:, :]), ['ovaug'], [('vca1', g)])
                    V(lambda e: e.memset(vsa[:, :, :, 64:65], 1.0), [], ['vsa1'])
                    V(lambda e: e.memset(vwa[:, :, :, 64:65], 1.0), [], ['vwa1'])
                    with ExitStack() as sx:
                        xT = xo[:, :].rearrange("p (k n) -> p k n", k=KT)
                        to_feature_major(xT, 'xT')
                        for t in range(NT):
                            C.dma('sp', hspill_d[t * 128:(t + 1) * 128, :], h[:, t, :], reads=[('h', t)])
                        C.barrier()
                        xk = lambda tb: [('xT', t) for t in range(tb * 4, tb * 4 + 4)]

                        def loadw(wb, key, col, ncols):
                            C.dma('pool', wb[:, :, 0:ncols], w1all_d[:, col:col + ncols].rearrange("(kt p) n -> p kt n", p=128), writes=[key])

                        with ExitStack() as sc:
                            with ExitStack() as swb:
                                wb = sb("nwb", [128, KT, 512], BF16, swb)
                                loadw(wb, 'nwb', OFF["kc"], 512)
                                for c4 in range(4):
                                    C.op('pool', lambda e: e.memset(hb[:, c4 * 4096:(c4 + 1) * 4096], 0.0), writes=[('kvz0', c4)])

                                def kv_evac(f, tb, ps, pk):
                                    cols = slice(tb * 512, (tb + 1) * 512)
                                    zk = [('kvz0', c4) for c4 in range(4)]
                                    evac(kvTz[0:64, 2 * f, cols], ps[0:64, :], reads=[pk] + zk, writes=[('kvTz', 2 * f, tb)], eng='act')
                                    evac(kvTz[64:128, 2 * f + 1, cols], ps[64:128, :], reads=[pk] + zk, writes=[('kvTz', 2 * f + 1, tb)], eng='dve')
                                proj_fm(xT, wb, 'nwb', 512, kvTz, 0, 'kvTz', evac_fn=kv_evac)
                                C.barrier()
                            w1b_single = sb("cw1b", [128, 32, 256], BF16, sc)
                            w1b = [w1b_single, w1b_single]
                            posb = [sb("cposb%d" % i, [128, 32], BF16, sc) for i in range(2)]
                            w2k = sb("cw2k", [128, 2, 128], BF16, sc)
                            w2ks = sb("cw2ks", [128, 2, 128], BF16, sc)
                            w2v = sb("cw2v", [128, 2, 64], BF16, sc)
                            cosC = sb("cosC", [128, 128], F32, sc)
                            sinC = sb("sinC", [128, 128], F32, sc)
                            for i in range(2):
                                C.dma('pool', posb[i][:, :], cpos_d[i][:, :], writes=[('cposb', i)])
                            C.dma('pool', w2k[:, :, :], cw2k_d.rearrange("(kt p) n -> p kt n", p=128), writes=['cw2k'])
                            C.dma('pool', w2ks[:, :, :], cw2ksw_d.rearrange("(kt p) n -> p kt n", p=128), writes=['cw2ks'])
                            C.dma('pool', w2v[:, :, :], cw2v_d.rearrange("(kt p) n -> p kt n", p=128), writes=['cw2v'])
                            C.dma('sp', cosC[:, :], cosC_d[:, :], writes=['cosC'])
                            C.dma('sp', sinC[:, :], sinC_d[:, :], writes=['sinC'])
                            bias = sb("cbias", [128, 2, 2], F32, sc)
                            gh = sb("cgh", [128, 2, 4, 2, 128], BF16, sc)
                            t1 = sb("ct1", [128, 128], F32, sc)
                            t2 = sb("ct2", [128, 128], F32, sc)
                            w1keys = lambda i: [('cw1b', lh) for lh in range(4)]
                            for i in range(2):
                                for lh in range(4):
                                    C.dma('pool', w1b[i][:, lh * 8:(lh + 1) * 8, :], cw1_d[i][:, lh * 8:(lh + 1) * 8, :], writes=[('cw1b', lh)])
                                for ht in range(2):
                                    ps, pk = bank()
                                    for l in range(32):
                                        _mm(C, ps[:, 0:1], w1b[i][0:64, l, ht * 128:(ht + 1) * 128], posb[i][0:64, l:l + 1], l == 0, l == 31,
                                            reads=w1keys(i) + [('cposb', i)], writes=[pk])
                                    V(lambda e: e.tensor_copy(out=bias[:, i, ht:ht + 1], in_=ps[:, 0:1]), [pk], [('cbias', i, ht)])
                                for g in range(4):
                                    for ht in range(2):
                                        ps, pk = bank()
                                        for l in range(32):
                                            _mm(C, ps[:, 0:127], w1b[i][:, l, ht * 128:(ht + 1) * 128],
                                                kvTz[:, 4 * i + g, l:l + 16 * 126 + 1:16], l == 0, l == 31,
                                                reads=w1keys(i) + [('kvTz', 4 * i + g, tb) for tb in range(4)], writes=[pk])
                                        A(lambda e: e.activation(out=gh[:, i, g, ht, 0:127], in_=ps[:, 0:127], func=AF.Gelu_apprx_tanh, bias=bias[:, i, ht:ht + 1]),
                                          [pk, ('cbias', i, ht)], [('cgh', i, g, ht)])
                            for g in range(4):
                                ps, pk = bank()
                                ps2, pk2 = bank()
                                for ht in range(2):
                                    _mm(C, ps[:, 0:127], w2k[:, ht, :], gh[:, 0, g, ht, 0:127], ht == 0, ht == 1, reads=['cw2k', ('cgh', 0, g, ht)], writes=[pk])
                                for ht in range(2):
                                    _mm(C, ps2[:, 0:127], w2ks[:, ht, :], gh[:, 0, g, ht, 0:127], ht == 0, ht == 1, reads=['cw2ks', ('cgh', 0, g, ht)], writes=[pk2])
                                V(lambda e: e.tensor_tensor(out=t1[:, 0:127], in0=ps[:, 0:127], in1=cosC[:, 0:127], op=ALU.mult), [pk, 'cosC', 'ct1'], ['ct1'])
                                V(lambda e: e.tensor_tensor(out=t2[:, 0:127], in0=ps2[:, 0:127], in1=sinC[:, 0:127], op=ALU.mult), [pk2, 'sinC', 'ct2'], ['ct2'])
                                b0 = (g % 2) * 64
                                V(lambda e: e.tensor_tensor(out=kcmp[b0:b0 + 64, g, 0:127], in0=t1[b0:b0 + 64, 0:127], in1=t2[b0:b0 + 64, 0:127], op=ALU.add),
                                  ['ct1', 'ct2', 'kcmp0'], [('kcmp', g)])
                                ps3, pk3 = bank()
                                for ht in range(2):
                                    _mm(C, ps3[0:127, 0:64], gh[:, 1, g, ht, 0:127], w2v[:, ht, :], ht == 0, ht == 1, reads=['cw2v', ('cgh', 1, g, ht)], writes=[pk3])
                                evac(vca[0:127, g, 0:64], ps3[0:127, 0:64], reads=[pk3], writes=[('vca0', g)])
                            C.barrier()
                        with ExitStack() as sr:
                            cosF = sb("cosF", [128, L], F32, sr)
                            sinF = sb("sinF", [128, L], F32, sr)
                            for c in range(4):
                                C.dma('sp', cosF[:, c * 512:(c + 1) * 512], cosF_d[:, c * 512:(c + 1) * 512], writes=[('cosF', c)])
                                C.dma('sp', sinF[:, c * 512:(c + 1) * 512], sinF_d[:, c * 512:(c + 1) * 512], writes=[('sinF', c)])
                            NWB = 2
                            wA = [sb("nwA%d" % i, [128, KT, 128], BF16, sr) for i in range(NWB)]
                            wS = [sb("nwS%d" % i, [128, KT, 128], BF16, sr) for i in range(NWB)]
                            r1 = [sb("r1_%d" % i, [128, 512], F32, sr) for i in range(2)]
                            r2 = [sb("r2_%d" % i, [128, 512], F32, sr) for i in range(2)]
                            ri = [0]
                            for c4 in range(4):
                                C.op('pool', lambda e: e.memset(hb[:, 16384 + c4 * 4096:16384 + (c4 + 1) * 4096], 0.0), writes=[('kz0', c4)])
                            jobs = [('rope', 'q', ft, qT, 'nqT') for ft in range(8)] + [('rope', 'ks', ft, ksd, 'ksd') for ft in range(2)] + \
                                   [('rope', 'kw', ft, kwd, 'kwd') for ft in range(2)] + \
                                   [('tm', 'vs', hf, vsa, 'vs') for hf in range(2)] + [('tm', 'vw', hf, vwa, 'vw') for hf in range(2)] + [('gate', 'gate', 0, gate, 'gate')]

                            def load_job(ji):
                                kind, name, idx, _, _ = jobs[ji]
                                b_ = ji % NWB
                                ncols = 48 if kind == 'gate' else 128
                                loadw(wA[b_], 'nwA%d' % b_, OFF[name] + idx * 128, ncols)
                                if kind == 'rope':
                                    loadw(wS[b_], 'nwS%d' % b_, OFF[name + "_sw"] + idx * 128, 128)

                            load_job(0)
                            for ji, (kind, name, idx, dst, dkey) in enumerate(jobs):
                                b_ = ji % NWB
                                if ji + 1 < len(jobs):
                                    load_job(ji + 1)
                                wa, ws = wA[b_], wS[b_]
                                ka, ks_ = 'nwA%d' % b_, 'nwS%d' % b_
                                if kind == 'rope':
                                    for tb in range(4):
                                        ps, pk = bank()
                                        ps2, pk2 = bank()
                                        for k in range(KT):
                                            _mm(C, ps[:, :], wa[:, k, :], xT[:, k, tb * 512:(tb + 1) * 512], k == 0, k == KT - 1, reads=[ka] + xk(tb), writes=[pk])
                                        for k in range(KT):
                                            _mm(C, ps2[:, :], ws[:, k, :], xT[:, k, tb * 512:(tb + 1) * 512], k == 0, k == KT - 1, reads=[ks_] + xk(tb), writes=[pk2])
                                        rb = ri[0] % 2
                                        ri[0] += 1
                                        V(lambda e: e.tensor_tensor(out=r1[rb][:, :], in0=ps[:, :], in1=cosF[:, tb * 512:(tb + 1) * 512], op=ALU.mult),
                                          [pk, ('cosF', tb)], ['r1_%d' % rb])
                                        V(lambda e: e.tensor_tensor(out=r2[rb][:, :], in0=ps2[:, :], in1=sinF[:, tb * 512:(tb + 1) * 512], op=ALU.mult),
                                          [pk2, ('sinF', tb)], ['r2_%d' % rb])
                                        if name == 'q':
                                            V(lambda e: e.tensor_tensor(out=dst[:, idx, tb * 512:(tb + 1) * 512], in0=r1[rb][:, :], in1=r2[rb][:, :], op=ALU.add),
                                              ['r1_%d' % rb, 'r2_%d' % rb], [(dkey, idx, tb)])
                                        else:
                                            zk = [('kz0', c4) for c4 in range(4)]
                                            for hf in range(2):
                                                V(lambda e: e.tensor_tensor(out=dst[hf * 64:(hf + 1) * 64, 2 * idx + hf, tb * 512:(tb + 1) * 512], in0=r1[rb][hf * 64:(hf + 1) * 64, :],
                                                                            in1=r2[rb][hf * 64:(hf + 1) * 64, :], op=ALU.add),
                                                  ['r1_%d' % rb, 'r2_%d' % rb] + zk, [(dkey, 2 * idx + hf, tb)])
                                elif kind == 'tm':
                                    for t in range(NT):
                                        ps, pk = bank()
                                        for k in range(KT):
                                            _mm(C, ps[:, 0:128], xT[:, k, t * 128:(t + 1) * 128], wa[:, k, 0:128], k == 0, k == KT - 1, reads=[ka, ('xT', t)], writes=[pk])
                                        evac(dst[:, t, 2 * idx:2 * idx + 2, 0:64], ps[:, 0:128].rearrange("p (g d) -> p g d", g=2), reads=[pk], writes=[(dkey, t, idx)])
                                else:
                                    for t in range(NT):
                                        ps, pk = bank()
                                        for k in range(KT):
                                            _mm(C, ps[:, 0:48], xT[:, k, t * 128:(t + 1) * 128], wa[:, k, 0:48], k == 0, k == KT - 1, reads=[ka, ('xT', t)], writes=[pk])
                                        A(lambda e: e.activation(out=gate[:, t, :], in_=ps[:, 0:48], func=AF.Sigmoid), [pk], [('gate', t)])
                            C.barrier()
                        C.barrier()
                    with ExitStack() as sa:
                        imp = sb("imp", [128, NT, 128], F32, sa)
                        selbT = sb("selbTz", [128, 4, L], BF16, sa)
                        NE = 6
                        E = [sb("E%d" % i, [128, 512], BF16, sa) for i in range(NE)]
                        ei = [0]
                        rden2 = [sb("rden%d" % i, [128, 4], F32, sa) for i in range(2)]
                        coef2 = [sb("coef%d" % i, [128, 4], F32, sa) for i in range(2)]
                        tmpO = [sb("tmpO%d" % i, [128, 4, 64], F32, sa) for i in range(2)]
                        tmpI = [sb("tmpI%d" % i, [128, 4, 32], F32, sa) for i in range(2)]
                        C.barrier()

                        cmb_i = [0]

                        def combine(psO, pkO, W, h_, tb, bi, first_branch):
                            ci = cmb_i[0] % 2
                            cmb_i[0] += 1
                            rd, cf, tO, tI = rden2[ci], coef2[ci], tmpO[ci], tmpI[ci]
                            kr, kc_, ko, ki = 'rden%d' % ci, 'coef%d' % ci, 'tmpO%d' % ci, 'tmpI%d' % ci
                            view = psO[:, 0:4 * W].rearrange("p (t w) -> p t w", w=W)
                            V(lambda e: e.tensor_scalar(out=rd[:, :], in0=view[:, :, 64], scalar1=1e-30, scalar2=None, op0=ALU.max), [pkO, kr], [kr])
                            V(lambda e: e.reciprocal(out=rd[:, :], in_=rd[:, :]), [kr], [kr])
                            V(lambda e: e.tensor_tensor(out=cf[:, :], in0=rd[:, :], in1=gate[:, 4 * tb:4 * tb + 4, 3 * h_ + bi], op=ALU.mult),
                              [kr, kc_] + [('gate', t) for t in range(4 * tb, 4 * tb + 4)], [kc_])
                            okeys = [('obuf', t, h_) for t in range(4 * tb, 4 * tb + 4)]
                            odst = obuf[:, 4 * tb:4 * tb + 4, h_ * 64:(h_ + 1) * 64]
                            cfb = cf[:, :].unsqueeze(2).to_broadcast([128, 4, 64])
                            if first_branch:
                                V(lambda e: e.tensor_tensor(out=odst, in0=view[:, :, 0:64], in1=cfb, op=ALU.mult), [pkO, kc_], okeys)
                            else:
                                V(lambda e: e.tensor_tensor(out=tO[:, :, :], in0=view[:, :, 0:64], in1=cfb, op=ALU.mult), [pkO, kc_, ko], [ko])
                                V(lambda e: e.tensor_tensor(out=odst, in0=odst, in1=tO[:, :, :], op=ALU.add), [ko] + okeys, okeys)
                            if W == 97:
                                g_ = h_ // 4
                                ikeys = [('imp', t, g_) for t in range(4 * tb, 4 * tb + 4)]
                                idst = imp[:, 4 * tb:4 * tb + 4, g_ * 32:(g_ + 1) * 32]
                                rdb = rd[:, :].unsqueeze(2).to_broadcast([128, 4, 32])
                                if h_ % 4 == 0:
                                    V(lambda e: e.tensor_tensor(out=idst, in0=view[:, :, 65:97], in1=rdb, op=ALU.mult), [pkO, kr], ikeys)
                                else:
                                    V(lambda e: e.tensor_tensor(out=tI[:, :, :], in0=view[:, :, 65:97], in1=rdb, op=ALU.mult), [pkO, kr, ki], [ki])
                                    V(lambda e: e.tensor_tensor(out=idst, in0=idst, in1=tI[:, :, :], op=ALU.add), [ki] + ikeys, ikeys)

                        SK = 4
                        psO_of = {}
                        psO_cnt = [0]

                        def run_pipeline(steps):
                            def stA(n):
                                st_ = steps[n]
                                psS, pkS = PS[n % 5], 'ps%d' % (n % 5)
                                st_['psS'], st_['pkS'] = psS, pkS
                                mms = st_['mms']
                                R = st_['rows']
                                for i_, (lt, rh, (a0, a1), rd) in enumerate(mms):
                                    C.op('pe', lambda e: e.matmul(psS[0:R, a0:a1], lhsT=lt, rhs=rh, start=(i_ == 0), stop=(i_ == len(mms) - 1), skip_group_check=True),
                                         reads=rd, writes=[pkS], chain=True)
                                eb = n % NE
                                c0, c1 = st_['cols']
                                A(lambda e: e.activation(out=E[eb][0:R, c0:c1], in_=psS[0:R, c0:c1], func=AF.Exp, scale=0.125), [pkS], ['E%d' % eb])

                            def stB(n):
                                st_ = steps[n]
                                eb = n % NE
                                R = st_['rows']
                                W = st_['W']
                                if st_['first']:
                                    bo = 5 + psO_cnt[0] % 3
                                    psO_cnt[0] += 1
                                    psO_of[st_['blk']] = (PS[bo], 'ps%d' % bo)
                                psO, pkO = psO_of[st_['blk']]
                                first = st_['first']
                                for tt in range(st_['tt_lo'], st_['tt_hi'] + 1):
                                    C.op('pe', lambda e: e.matmul(psO[:, tt * W:(tt + 1) * W], lhsT=E[eb][0:R, tt * 128:(tt + 1) * 128], rhs=st_['vrhs'],
                                                                  start=first, stop=False, skip_group_check=True),
                                         reads=['E%d' % eb] + st_['vreads'], writes=[pkO], chain=True)
                                    first = False
                                if st_['last']:
                                    combine(psO, pkO, W, st_['h'], st_['tb'], st_['which'], st_['which'] == 0)

                            for n in range(len(steps) + SK):
                                if n < len(steps):
                                    stA(n)
                                if n >= SK:
                                    stB(n - SK)

                        def compressed_steps(h_, tb, blkid):
                            g_ = h_ // 4
                            qt_ = (g_ // 2) * 4 + h_ % 4
                            mms = [(kcmp[:, g_, 0:127], qT[:, qt_, tb * 512:(tb + 1) * 512], (0, 512), [('kcmp', g_), 'kcmp0', ('nqT', qt_, tb)]),
                                   (identb[0:127, 0:127], cmask[0:127, tb * 512:(tb + 1) * 512], (0, 512), ['identb', ('cmask', tb // 2)])]
                            return [dict(blk=blkid, which=0, h=h_, tb=tb, rows=127, W=97, cols=(0, 512), mms=mms, tt_lo=0, tt_hi=3,
                                         first=True, last=True, vrhs=vca[0:127, g_, :], vreads=[('vca0', g_), ('vca1', g_)])]

                        def branch_steps(h_, tb, which, blkid):
                            g_ = h_ // 4
                            qt_ = (g_ // 2) * 4 + h_ % 4
                            kT_ = ksd if which == 1 else kwd
                            va = vsa if which == 1 else vwa
                            kkey = 'ksd' if which == 1 else 'kwd'
                            vkey = 'vs' if which == 1 else 'vw'
                            v1key = 'vsa1' if which == 1 else 'vwa1'
                            kts = list(range(0, 4 * tb + 4)) if which == 1 else list(range(max(0, 4 * tb - 4), 4 * tb + 4))
                            out = []
                            for ki, kt in enumerate(kts):
                                rel = kt - 4 * tb
                                tt_lo = max(0, rel)
                                tt_hi = 3 if which == 1 else min(3, rel + 4)
                                c0, c1 = 128 * tt_lo, 128 * (tt_hi + 1)
                                mms = [(kT_[:, g_, kt * 128:(kt + 1) * 128], qT[:, qt_, tb * 512 + c0:tb * 512 + c1], (c0, c1),
                                        [(kkey, g_, kt // 4), ('nqT', qt_, tb)])]
                                if which == 1:
                                    mms.append((esel[:, kt, :], selbT[:, g_, tb * 512 + c0:tb * 512 + c1], (c0, c1),
                                                [('esel', kt // 8)] + [('selbT', t, g_) for t in range(4 * tb, 4 * tb + 4)]))
                                if 0 <= rel <= 3:
                                    mms.append((identb[:, :], caus[:, :], (128 * rel, 128 * rel + 128), ['identb', 'caus']))
                                if which == 2 and 0 <= rel + 4 <= 3:
                                    mms.append((identb[:, :], anti[:, :], (128 * (rel + 4), 128 * (rel + 4) + 128), ['identb', 'anti']))
                                out.append(dict(blk=blkid, which=which, h=h_, tb=tb, rows=128, W=65, cols=(c0, c1), mms=mms, tt_lo=tt_lo, tt_hi=tt_hi,
                                                first=(ki == 0), last=(ki == len(kts) - 1), vrhs=va[:, kt, g_, :], vreads=[(vkey, kt, g_ // 2), v1key]))
                            return out

                        score = [sb("score%d" % i, [128, 128], F32, sa) for i in range(2)]
                        mx8 = [sb("mx8_%d" % i, [128, 4, 8], F32, sa) for i in range(2)]
                        selb = [sb("selb%d" % i, [128, 128], F32, sa) for i in range(2)]

                        selb8 = [sb("selb8_%d" % i, [128, 128], F32, sa) for i in range(8)]

                        def sel_pre(t):
                            i = t % 2
                            ks_, km_, kb_ = 'score%d' % i, 'mx8_%d' % i, 'selb8_%d' % (t % 8)
                            sbt = selb8[t % 8]
                            for g_ in range(4):
                                V(lambda e: e.tensor_tensor(out=score[i][:, g_ * 32:(g_ + 1) * 32], in0=imp[:, t, g_ * 32:(g_ + 1) * 32], in1=forceb[:, t, :], op=ALU.add),
                                  [('imp', t, g_), 'forceb', ks_], [ks_])
                            for g_ in range(4):
                                V(lambda e: e.max(out=mx8[i][:, g_, :], in_=score[i][:, g_ * 32:(g_ + 1) * 32]), [ks_, km_], [km_])
                            for g_ in range(4):
                                V(lambda e: e.tensor_scalar(out=sbt[:, g_ * 32:(g_ + 1) * 32], in0=score[i][:, g_ * 32:(g_ + 1) * 32], scalar1=mx8[i][:, g_, 7:8], scalar2=-1.0,
                                                            op0=ALU.is_ge, op1=ALU.add), [ks_, km_, kb_], [kb_])

                        def sel_post(t):
                            kb_ = 'selb8_%d' % (t % 8)
                            ps, pk = PS[t % 5], 'ps%d' % (t % 5)
                            C.op('pe', lambda e: e.transpose(out=ps[:, 0:128], in_=selb8[t % 8][:, :], identity=ident[:]), reads=[kb_, 'ident'], writes=[pk], chain=True)
                            for g_ in range(4):
                                V(lambda e: e.tensor_scalar(out=selbT[:, g_, t * 128:(t + 1) * 128], in0=ps[:, 0:128], scalar1=rowmask[:, g_:g_ + 1], scalar2=None, op0=ALU.mult),
                                  [pk, 'rowmask'], [('selbT', t, g_)])

                        def cw_steps(tb):
                            out = []
                            for h_ in range(16):
                                out += compressed_steps(h_, tb, ('c', h_, tb))
                                out += branch_steps(h_, tb, 2, ('w', h_, tb))
                            return out

                        def s_steps(tb):
                            out = []
                            for h_ in range(16):
                                out += branch_steps(h_, tb, 1, ('s', h_, tb))
                            return out

                        run_pipeline(cw_steps(0))
                        for t in range(0, 4):
                            sel_pre(t)
                        run_pipeline(cw_steps(1))
                        for t in range(0, 4):
                            sel_post(t)
                        for t in range(4, 8):
                            sel_pre(t)
                        run_pipeline(cw_steps(2))
                        for t in range(4, 8):
                            sel_post(t)
                        for t in range(8, 12):
                            sel_pre(t)
                        run_pipeline(cw_steps(3))
                        for t in range(8, 12):
                            sel_post(t)
                        for t in range(12, 16):
                            sel_pre(t)
                        run_pipeline(s_steps(0))
                        for t in range(12, 16):
                            sel_post(t)
                        for tb in range(1, 4):
                            run_pipeline(s_steps(tb))
                        C.barrier()
                    C.barrier()
                with ExitStack() as s3:
                    oT = sb("oT", [128, KT, L], BF16, s3)
                    reload_h(hspill_d)
                    for t in range(NT):
                        for half in range(2):
                            ps, pk = bank()
                            psb_ = ps[:, :].bitcast(BF16)
                            for kk in range(4):
                                k = half * 4 + kk
                                C.op('pe', lambda e: e.transpose(out=psb_[:, kk * 128:(kk + 1) * 128], in_=obuf[:, t, k * 128:(k + 1) * 128], identity=identb[:]),
                                     reads=['identb'], writes=[pk], chain=True)
                            evac(oT[:, half * 4:half * 4 + 4, t * 128:(t + 1) * 128], psb_[:, 0:512].rearrange("p (k n) -> p k n", k=4), reads=[pk], writes=[('oT', t)])
                    if "dbg_obuf" in dbg:
                        for t in range(NT):
                            C.dma('pool', dbg["dbg_obuf"][:, t, :], obuf[:, t, :], reads=[])
                    out_proj_ln(oT, wo, ['wo', 'wo2'] + [('oT', t) for t in range(NT)], 2, lnp)
                    C.barrier()

        stages = ["ssm", "attn", "mix0", "moe0", "all"]
        if upto.startswith("nsa"):
            nsa_mixer()
        else:
            layer0_mixer()
            if upto not in ("mix0", "ssm", "attn"):
                moe(0, 1)
                if upto == "all":
                    nsa_mixer()
                    moe(1, 3, final=True)
        C.barrier()
        if upto != "all":
            for t in range(NT):
                C.dma('sp' if t % 2 == 0 else 'act', out_d[t * 128:(t + 1) * 128, :], h[:, t, :], reads=[('h', t)])
        C.barrier()
        print("instructions", C.n_ins, "waits", C.n_wait)
    return nc


def host_layouts(inp):
    f32 = np.float32
    o = {}
    lam_re = np.asarray(inp["ssm_lam_re"], f32)
    lam_im = np.asarray(inp["ssm_lam_im"], f32)
    log_dt = np.asarray(inp["ssm_log_dt"], f32)

    def state_layout(a):
        return np.ascontiguousarray(a.reshape(16, 2, 64).transpose(1, 2, 0).reshape(128, 16))
    o["lam_re_l"] = state_layout(lam_re)
    o["lam_im_l"] = state_layout(lam_im)
    o["logdt_l"] = state_layout(np.repeat(log_dt[:, None], 64, axis=1))
    b_re = np.asarray(inp["ssm_b_re"], f32)
    b_im = np.asarray(inp["ssm_b_im"], f32)
    c_re = np.asarray(inp["ssm_c_re"], f32)
    c_im = np.asarray(inp["ssm_c_im"], f32)
    bre = np.zeros((128, 16, 128), f32)
    bim = np.zeros((128, 16, 128), f32)
    cre = np.zeros((128, 16, 128), f32)
    cim = np.zeros((128, 16, 128), f32)
    for g in range(32):
        j, two = g // 2, g % 2
        r0 = 32 * (j % 4) + 16 * two
        bre[r0:r0 + 16, j, two * 64:(two + 1) * 64] = b_re[g].T
        bim[r0:r0 + 16, j, two * 64:(two + 1) * 64] = b_im[g].T
        cre[two * 64:(two + 1) * 64, j, r0:r0 + 16] = c_re[g].T
        cim[two * 64:(two + 1) * 64, j, r0:r0 + 16] = c_im[g].T
    o["bre_l"], o["bim_l"], o["cre_l"], o["cim_l"] = bre, bim, cre, cim
    o["ssmd_l"] = np.ascontiguousarray(np.asarray(inp["ssm_d"], f32).reshape(4, 128).T)
    o["ln_all"] = np.stack([np.asarray(inp[k], f32) for k in
                            ["ln_mix_g_0", "ln_mix_b_0", "ln_ffn_g_0", "ln_ffn_b_0", "ln_mix_g_1", "ln_mix_b_1", "ln_ffn_g_1", "ln_ffn_b_1"]])
    o["b_router"] = np.asarray(inp["b_router"], f32).reshape(1, 16)
    for k in ["w_in_0", "w_glu", "w_out_0", "w1_0", "w3_0", "w2_0", "w1_1", "w3_1", "w2_1"]:
        o[k] = np.ascontiguousarray(np.asarray(inp[k], f32))
    o["w_router_l"] = np.ascontiguousarray(np.asarray(inp["w_router"], f32).reshape(8, 128, 16).transpose(1, 0, 2).reshape(128, 128))
    o["ident"] = np.eye(128, dtype=f32)
    jj, ss = np.meshgrid(np.arange(128), np.arange(128), indexing="ij")
    o["triu_neg"] = -(jj >= ss).astype(f32)

    w1 = np.asarray(inp["w_in_1"], f32)
    perm64 = (np.arange(64) + 32) % 64

    def swap_cols(w):
        nh = w.shape[1] // 64
        idx = (np.arange(nh)[:, None] * 64 + perm64[None, :]).reshape(-1)
        return w[:, idx]

    def dup_heads(w):
        return np.concatenate([np.concatenate([w[:, g * 64:(g + 1) * 64]] * 2, axis=1) for g in range(4)], axis=1)
    q_w = w1[:, 0:1024]
    kc_w, vc_w, ks_w, vs_w, kw_w, vw_w = [w1[:, 1024 + i * 256:1024 + (i + 1) * 256] for i in range(6)]
    gate_w = w1[:, 2560:2608]
    hq = []
    for T in range(8):
        G2, r = T // 4, T % 4
        hq += [(2 * G2) * 4 + r, (2 * G2 + 1) * 4 + r]
    qidx = (np.asarray(hq)[:, None] * 64 + np.arange(64)[None, :]).reshape(-1)
    q_sw_full = swap_cols(q_w)
    pieces = {"q": q_w[:, qidx], "q_sw": q_sw_full[:, qidx], "ks": ks_w, "ks_sw": swap_cols(ks_w),
              "kw": kw_w, "kw_sw": swap_cols(kw_w), "kc": kc_w, "vc": vc_w, "vs": vs_w, "vw": vw_w, "gate": gate_w}
    w1all = np.zeros((1024, W1ALL_COLS), f32)
    for n_, a_ in pieces.items():
        w1all[:, OFF[n_]:OFF[n_] + a_.shape[1]] = a_
    o["w1all"] = w1all
    for nm, key in (("k", "cmp_w1_k"), ("v", "cmp_w1_v")):
        cw1 = np.asarray(inp[key], f32).reshape(32, 64, 256).transpose(1, 0, 2)
        o["cw1%s_l" % nm] = np.ascontiguousarray(np.concatenate([cw1, cw1], axis=0))
    for nm, key in (("k", "cmp_pos_k"), ("v", "cmp_pos_v")):
        pt = np.asarray(inp[key], f32).T
        o["cpos%s_T" % nm] = np.ascontiguousarray(np.concatenate([pt, pt], axis=0))
    w2k = np.asarray(inp["cmp_w2_k"], f32)
    o["cw2k_dup"] = np.ascontiguousarray(np.concatenate([w2k, w2k], axis=1))
    o["cw2k_dup_sw"] = np.ascontiguousarray(np.concatenate([w2k[:, perm64], w2k[:, perm64]], axis=1))
    o["cw2v"] = np.ascontiguousarray(np.asarray(inp["cmp_w2_v"], f32))
    o["w_out_1"] = np.ascontiguousarray(np.asarray(inp["w_out_1"], f32))
    inv = (10000.0 ** (-np.arange(0, 64, 2, dtype=f32) / 64)).astype(f32)

    def rope_tabs(pos):
        ang = pos.astype(f32)[None, :] * inv[:, None]
        c, s_ = np.cos(ang).astype(f32), np.sin(ang).astype(f32)
        cosT = np.concatenate([c, c, c, c], axis=0)
        sinT = np.concatenate([-s_, s_, -s_, s_], axis=0)
        return cosT, sinT
    o["cosF"], o["sinF"] = rope_tabs(np.arange(2048))
    cc, sc_ = rope_tabs(np.arange(127) * 16 + 31)
    o["cosC"] = np.zeros((128, 128), f32); o["cosC"][:, :127] = cc
    o["sinC"] = np.zeros((128, 128), f32); o["sinC"][:, :127] = sc_
    m_ = np.arange(128)[:, None]; t_ = np.arange(2048)[None, :]
    o["cmask"] = np.where(16 * m_ + 31 <= t_, 0.0, -BIG).astype(f32)
    mm_ = np.arange(128)[:, None]; nn_ = np.arange(32)[None, :]
    ov = ((16 * mm_ < 64 * nn_ + 64) & (16 * mm_ + 32 > 64 * nn_)).astype(f32)
    o["ovaug"] = np.concatenate([np.ones((128, 1), f32), ov], axis=1)
    tt_ = np.arange(2048)[:, None]; cur = tt_ // 64
    valid = nn_ * 64 <= tt_
    forced = (nn_ == 0) | (nn_ == cur) | (nn_ == cur - 1)
    fb = np.where(valid, np.where(forced, 1e4, 0.0), -1e30).astype(f32)
    o["forceb"] = np.ascontiguousarray(fb.reshape(16, 128, 32).transpose(1, 0, 2))
    es = np.zeros((128, 16, 128), f32)
    for g in range(4):
        for kt in range(16):
            for j in range(128):
                es[32 * g + 2 * kt + j // 64, kt, j] = BIG
    o["esel"] = es
    ji, ii = np.meshgrid(np.arange(128), np.arange(128), indexing="ij")
    o["caus"] = np.where(ji > ii, -BIG, 0.0).astype(f32)
    o["anti"] = np.where(ji <= ii, -BIG, 0.0).astype(f32)
    o["caus_s"] = np.where(ji >= ii, -BIG, 0.0).astype(f32)
    o["rowmask"] = (np.arange(128)[:, None] // 32 == np.arange(4)[None, :]).astype(f32)
    return o


_NC_CACHE = {}


def kernel(**inputs):
    n = 8
    x = np.asarray(inputs["x"], np.float32)
    shared = host_layouts(inputs)
    if "nc" not in _NC_CACHE:
        _NC_CACHE["nc"] = build("all")
    nc = _NC_CACHE["nc"]
    in_maps = []
    for c in range(n):
        m = dict(shared)
        m["x"] = np.ascontiguousarray(x[c])
        in_maps.append(m)
    res = run_bass_kernel_spmd(nc, in_maps, core_ids=list(range(n)))
    return np.stack([np.asarray(r["out"], np.float32) for r in res.results], axis=0)
```

```python
import math
import numpy as np
from contextlib import ExitStack
import concourse.bass as bass
import concourse.mybir as mybir
from concourse.bass_utils import run_bass_kernel_spmd

F32 = mybir.dt.float32
BF16 = mybir.dt.bfloat16
I32 = mybir.dt.int32
AF = mybir.ActivationFunctionType
ALU = mybir.AluOpType
AX = mybir.AxisListType

L = 2048
D = 1024
NT = 16
KT = 8
ALPHA = (2 * 2) ** 0.25
LN_EPS = 1e-5
PI = math.pi
BIG = 30000.0
OFF = {}
_c = 0
for _n, _w in [("q", 1024), ("q_sw", 1024), ("ks", 256), ("ks_sw", 256), ("kw", 256), ("kw_sw", 256),
               ("kc", 256), ("vc", 256), ("vs", 256), ("vw", 256), ("gate", 48)]:
    OFF[_n] = _c
    _c += _w
W1ALL_COLS = _c
import os
NEXP = int(os.environ.get('DBG_NEXP', '16'))
MSTOP = int(os.environ.get('DBG_MSTOP', '9'))


class Ctx:
    NDSEM = 24

    def __init__(self, nc, stack):
        self.nc = nc
        self.eng = dict(pe=nc.tensor, act=nc.scalar, dve=nc.vector, pool=nc.gpsimd, sp=nc.sync)
        self.esem = {}
        self.ecnt = {}
        for e in self.eng:
            self.esem[e] = stack.enter_context(nc.semaphore("prog_" + e))
            self.ecnt[e] = 0
        self.dsem = [stack.enter_context(nc.semaphore("dma_%d" % i)) for i in range(self.NDSEM)]
        self.dcnt = [0] * self.NDSEM
        self.dnext = 0
        self.dnext_sw = 0
        self.seen = {}
        self.state = {}
        self.semobj = {}
        for e in self.eng:
            self.semobj[id(self.esem[e])] = self.esem[e]
        for s in self.dsem:
            self.semobj[id(s)] = s
        self.n_wait = 0
        self.n_ins = 0

    def _deps(self, reads, writes):
        deps = {}

        def add(tok):
            if tok is None:
                return
            sid, val = tok
            if deps.get(sid, 0) < val:
                deps[sid] = val
        for k in reads:
            st = self.state.get(k)
            if st:
                add(st[0])
        for k in writes:
            st = self.state.get(k)
            if st:
                add(st[0])
                for t in st[1]:
                    add(t)
        return deps

    def _wait(self, e, deps, skip_self_pe=False):
        engobj = self.eng[e]
        for sid, val in deps.items():
            if skip_self_pe and e == 'pe' and sid == id(self.esem['pe']):
                continue
            if self.seen.get((e, sid), 0) >= val:
                continue
            engobj.wait_ge(self.semobj[sid], val)
            self.seen[(e, sid)] = val
            self.n_wait += 1

    def _update(self, tok, reads, writes):
        for k in reads:
            st = self.state.setdefault(k, [None, []])
            st[1] = [t for t in st[1] if t[0] != tok[0]] + [tok]
        for k in writes:
            self.state[k] = [tok, []]

    def op(self, e, fn, reads=(), writes=(), chain=False):
        deps = self._deps(reads, writes)
        self._wait(e, deps, skip_self_pe=chain)
        ins = fn(self.eng[e])
        self.ecnt[e] += 1
        ins.then_inc(self.esem[e], 1)
        tok = (id(self.esem[e]), self.ecnt[e])
        self._update(tok, reads, writes)
        self.n_ins += 1
        return ins

    def dma(self, q, out, in_, reads=(), writes=(), **kw):
        deps = self._deps(reads, writes)
        half = self.NDSEM // 2
        if q == 'pool':
            i = half + self.dnext_sw
            self.dnext_sw = (self.dnext_sw + 1) % (self.NDSEM - half)
        else:
            i = self.dnext
            self.dnext = (self.dnext + 1) % half
        if self.dcnt[i] > 0:
            deps[id(self.dsem[i])] = max(deps.get(id(self.dsem[i]), 0), self.dcnt[i])
        self._wait(q, deps)
        ins = self.eng[q].dma_start(out=out, in_=in_, **kw)
        self.dcnt[i] += 16
        ins.then_inc(self.dsem[i], 16)
        tok = (id(self.dsem[i]), self.dcnt[i])
        self._update(tok, reads, writes)
        self.n_ins += 1
        return ins

    def barrier(self):
        deps = {}
        for e in self.eng:
            if self.ecnt[e]:
                deps[id(self.esem[e])] = self.ecnt[e]
        for i in range(self.NDSEM):
            if self.dcnt[i]:
                deps[id(self.dsem[i])] = self.dcnt[i]
        for e in self.eng:
            self._wait(e, dict(deps))
        self.state = {}


class K:
    pass


def _mm(C, out, lhsT, rhs, start, stop, reads, writes, chain=True):
    return C.op('pe', lambda e: e.matmul(out, lhsT=lhsT, rhs=rhs, start=start, stop=stop),
                reads=reads, writes=writes, chain=chain)


def build(upto="all", debug=()):
    nc = bass.Bass("TRN2", target_bir_lowering=False)
    din = {}

    def dram_in(name, shape, dt=F32):
        din[name] = nc.dram_tensor(name, list(shape), dt, kind="ExternalInput").ap()
        return din[name]

    x_d = dram_in("x", [L, D])
    w_in_0 = dram_in("w_in_0", [D, 2048])
    lam_re_d = dram_in("lam_re_l", [128, 16])
    lam_im_d = dram_in("lam_im_l", [128, 16])
    logdt_d = dram_in("logdt_l", [128, 16])
    bre_d = dram_in("bre_l", [128, 16, 128])
    bim_d = dram_in("bim_l", [128, 16, 128])
    cre_d = dram_in("cre_l", [128, 16, 128])
    cim_d = dram_in("cim_l", [128, 16, 128])
    ssmd_d = dram_in("ssmd_l", [128, 4])
    w_glu_d = dram_in("w_glu", [512, 512])
    w_out_0 = dram_in("w_out_0", [D, D])
    ln_d = dram_in("ln_all", [8, D])
    w1_d = [dram_in("w1_0", [16, D, 512]), dram_in("w1_1", [16, D, 512])]
    w3_d = [dram_in("w3_0", [16, D, 512]), dram_in("w3_1", [16, D, 512])]
    w2_d = [dram_in("w2_0", [16, 512, D]), dram_in("w2_1", [16, 512, D])]
    w_router_d = dram_in("w_router_l", [128, KT * 16])
    b_router_d = dram_in("b_router", [1, 16])
    ident_d = dram_in("ident", [128, 128])
    triu_d = dram_in("triu_neg", [128, 128])
    w1all_d = dram_in("w1all", [D, W1ALL_COLS])
    cw1_d = [dram_in("cw1k_l", [128, 32, 256]), dram_in("cw1v_l", [128, 32, 256])]
    cpos_d = [dram_in("cposk_T", [128, 32]), dram_in("cposv_T", [128, 32])]
    cw2k_d = dram_in("cw2k_dup", [256, 128])
    cw2ksw_d = dram_in("cw2k_dup_sw", [256, 128])
    cw2v_d = dram_in("cw2v", [256, 64])
    w_out_1 = dram_in("w_out_1", [D, D])
    cosF_d = dram_in("cosF", [128, L])
    sinF_d = dram_in("sinF", [128, L])
    cosC_d = dram_in("cosC", [128, 128])
    sinC_d = dram_in("sinC", [128, 128])
    cmask_d = dram_in("cmask", [128, L])
    ovaug_d = dram_in("ovaug", [128, 33])
    forceb_d = dram_in("forceb", [128, NT, 32])
    esel_d = dram_in("esel", [128, NT, 128])
    caus_d = dram_in("caus", [128, 128])
    anti_d = dram_in("anti", [128, 128])
    rowmask_d = dram_in("rowmask", [128, 4])
    causs_d = dram_in("caus_s", [128, 128])
    hspill_d = nc.dram_tensor("hspill", [L, D], F32, kind="Internal").ap()
    out_d = nc.dram_tensor("out", [L, D], F32, kind="ExternalOutput").ap()
    dbg = {}
    for name, shape in debug:
        dbg[name] = nc.dram_tensor(name, list(shape), F32, kind="ExternalOutput").ap()

    with ExitStack() as st:
        C = Ctx(nc, st)

        sbn = [0]

        def sb(name, shape, dt=F32, stack=st):
            sbn[0] += 1
            return stack.enter_context(nc.sbuf_tensor("s%d_%s" % (sbn[0], name), list(shape), dt))

        h = sb("h", [128, NT, D])
        ident = sb("ident", [128, 128])
        identb = sb("identb", [128, 128], BF16)
        PS = [st.enter_context(nc.psum_tensor("ps%d" % i, [128, 512], F32)) for i in range(8)]
        psrr = [0]

        bank_range = [0, 8]

        def bank(lo=None, hi=None):
            if lo is None:
                lo, hi = bank_range
            i = lo + psrr[0] % (hi - lo)
            psrr[0] += 1
            return PS[i], "ps%d" % i

        evac_rr = [0]

        def evac(out, in_, reads, writes, eng=None, scale=None):
            if eng is None:
                eng = 'act' if evac_rr[0] % 2 == 0 else 'dve'
                evac_rr[0] += 1
            if eng == 'act':
                if scale is None:
                    C.op('act', lambda e: e.activation(out=out, in_=in_, func=AF.Copy), reads=reads, writes=writes)
                else:
                    C.op('act', lambda e: e.activation(out=out, in_=in_, func=AF.Copy, scale=scale), reads=reads, writes=writes)
            else:
                if scale is None:
                    C.op('dve', lambda e: e.tensor_copy(out=out, in_=in_), reads=reads, writes=writes)
                else:
                    C.op('dve', lambda e: e.tensor_scalar(out=out, in0=in_, scalar1=scale, scalar2=None, op0=ALU.mult), reads=reads, writes=writes)

        hb = h[:, :, :].rearrange("p t d -> p (t d)").bitcast(BF16)

        def reload_h(src):
            for t in range(NT):
                C.dma('sp', h[:, t, :], src[t * 128:(t + 1) * 128, :], writes=[('h', t)])

        C.dma('sp', ident[:], ident_d[:, :], writes=['ident'])
        C.op('dve', lambda e: e.tensor_copy(out=identb[:], in_=ident[:]), reads=['ident'], writes=['identb'])
        for t in range(NT):
            C.dma('sp', h[:, t, :], x_d[t * 128:(t + 1) * 128, :], writes=[('h', t)])

        def to_feature_major(xT, tag, router=None):
            pend = []
            for t in range(NT):
                for half in range(2):
                    ps, pk = bank()
                    for kk in range(4):
                        k = half * 4 + kk
                        C.op('pe', lambda e: e.transpose(out=ps[:, kk * 128:(kk + 1) * 128], in_=h[:, t, k * 128:(k + 1) * 128], identity=ident[:]),
                             reads=[('h', t), 'ident'], writes=[pk], chain=True)
                    evac(xT[:, half * 4:half * 4 + 4, t * 128:(t + 1) * 128],
                         ps[:, :].rearrange("p (k n) -> p k n", k=4), reads=[pk], writes=[(tag, t)])
                    if router is not None:
                        pend.append(router(t, half, ps, pk))
                        if len(pend) > 2:
                            pend.pop(0)()
            for th_ in pend:
                th_()

        def layer_norm_tile(t, lnidx, lnp):
            i = t % 2
            stats, mv, rstd, nmr = K.stats[i], K.mv[i], K.rstd[i], K.nmr[i]
            sk = 'ln%d_' % i
            C.op('dve', lambda e: e.bn_stats(out=stats[:, 0, :], in_=h[:, t, 0:512]), reads=[('h', t)], writes=[sk + 'stats'])
            C.op('dve', lambda e: e.bn_stats(out=stats[:, 1, :], in_=h[:, t, 512:1024]), reads=[('h', t)], writes=[sk + 'stats2'])
            C.op('dve', lambda e: e.bn_aggr(out=mv[:, :], in_=stats[:, :, :].rearrange("p a b -> p (a b)")), reads=[sk + 'stats', sk + 'stats2'], writes=[sk + 'mv'])
            C.op('dve', lambda e: e.tensor_scalar(out=rstd[:, :], in0=mv[:, 1:2], scalar1=LN_EPS, scalar2=None, op0=ALU.add), reads=[sk + 'mv'], writes=[sk + 'rstd'])
            C.op('act', lambda e: e.activation(out=rstd[:, :], in_=rstd[:, :], func=AF.Sqrt), reads=[sk + 'rstd'], writes=[sk + 'rstd'])
            C.op('dve', lambda e: e.reciprocal(out=rstd[:, :], in_=rstd[:, :]), reads=[sk + 'rstd'], writes=[sk + 'rstd'])
            C.op('dve', lambda e: e.tensor_scalar(out=nmr[:, :], in0=mv[:, 0:1], scalar1=rstd[:, 0:1], scalar2=-1.0, op0=ALU.mult, op1=ALU.mult),
                 reads=[sk + 'mv', sk + 'rstd'], writes=[sk + 'nmr'])
            C.op('act', lambda e: e.activation(out=h[:, t, :], in_=h[:, t, :], func=AF.Identity, scale=rstd[:, 0:1], bias=nmr[:, 0:1]),
                 reads=[('h', t), sk + 'rstd', sk + 'nmr'], writes=[('h', t)])
            C.op('dve', lambda e: e.tensor_tensor(out=h[:, t, :], in0=h[:, t, :], in1=lnp[:, 0, :], op=ALU.mult), reads=[('h', t), ('lnp', lnidx)], writes=[('h', t)])
            C.op('dve', lambda e: e.tensor_tensor(out=h[:, t, :], in0=h[:, t, :], in1=lnp[:, 1, :], op=ALU.add), reads=[('h', t), ('lnp', lnidx)], writes=[('h', t)])

        def load_ln(lnp, lnidx):
            C.dma('sp', lnp[:, 0, :], ln_d[2 * lnidx:2 * lnidx + 1, :].broadcast_to([128, D]), writes=[('lnp', lnidx)])
            C.dma('sp', lnp[:, 1, :], ln_d[2 * lnidx + 1:2 * lnidx + 2, :].broadcast_to([128, D]), writes=[('lnp', lnidx)])

        K.stats = [sb("stats%d" % i, [128, 2, 6]) for i in range(2)]
        K.mv = [sb("mv%d" % i, [128, 2]) for i in range(2)]
        K.rstd = [sb("rstd%d" % i, [128, 1]) for i in range(2)]
        K.nmr = [sb("nmr%d" % i, [128, 1]) for i in range(2)]

        def dump(name, src_ap, reads):
            if name in dbg:
                if len(src_ap.shape) == 3 and src_ap.shape[2] > 1024:
                    for i in range(src_ap.shape[1]):
                        for c in range(0, src_ap.shape[2], 1024):
                            C.dma('pool', dbg[name][:, i, c:c + 1024], src_ap[:, i, c:c + 1024], reads=reads)
                else:
                    C.dma('pool', dbg[name], src_ap, reads=reads)

        def load_w_chunk(wbuf, key, src, c0, ncols):
            C.dma('pool', wbuf[:, :, 0:ncols], src[:, c0:c0 + ncols].rearrange("(kt p) n -> p kt n", p=128), writes=[key])

        def proj_fm(xT, wbuf, wkey, ncols, dst, dst_tile0, dkey, scale=None, xkey='xT', evac_fn=None):
            for f in range(ncols // 128):
                for tb in range(4):
                    ps, pk = bank()
                    for k in range(KT):
                        _mm(C, ps[:, :], wbuf[:, k, f * 128:(f + 1) * 128], xT[:, k, tb * 512:(tb + 1) * 512], k == 0, k == KT - 1,
                            reads=[wkey] + [(xkey, t) for t in range(tb * 4, tb * 4 + 4)], writes=[pk])
                    if evac_fn is not None:
                        evac_fn(f, tb, ps, pk)
                    else:
                        evac(dst[:, dst_tile0 + f, tb * 512:(tb + 1) * 512], ps[:, :], reads=[pk], writes=[(dkey, dst_tile0 + f, tb)], scale=scale)

        def proj_tm(xT, wbuf, wkey, ncols, dst, dkey, xkey='xT', col0=0):
            for t in range(NT):
                ps, pk = bank()
                for k in range(KT):
                    _mm(C, ps[:, 0:ncols], xT[:, k, t * 128:(t + 1) * 128], wbuf[:, k, 0:ncols], k == 0, k == KT - 1,
                        reads=[wkey, (xkey, t)], writes=[pk])
                evac(dst[:, t, col0:col0 + ncols], ps[:, 0:ncols], reads=[pk], writes=[(dkey, t)])

        def layer0_mixer():
            with ExitStack() as s0:
                ocat = sb("ocat", [128, 8, L], BF16, s0)
                qT = hb[:, 0:8192].rearrange("p (a n) -> p a n", a=4)
                kTz = hb[:, 8192:24576].rearrange("p (a n) -> p a n", a=8)
                v = hb[:, 24576:32768].rearrange("p (t n) -> p t n", t=NT)
                s2 = s0
                sba_gen, sba_n = sb_attention(s2, qT, kTz, v, ocat)
                with ExitStack() as s1:
                    uT = sb("uT", [128, 4, L], BF16, s1)
                    with ExitStack() as s1a:
                        xT = sb("xT", [128, KT, L], BF16, s1a)
                        wbuf = [sb("w0b%d" % i, [128, KT, 512], BF16, s1a) for i in range(2)]
                        load_w_chunk(wbuf[0], 'w0b0', w_in_0, 1536, 512)
                        load_w_chunk(wbuf[1], 'w0b1', w_in_0, 0, 512)
                        to_feature_major(xT, 'xT')
                        proj_fm(xT, wbuf[0], 'w0b0', 512, uT, 0, 'uT')
                        C.barrier()
                        load_w_chunk(wbuf[0], 'w0b0', w_in_0, 512, 512)
                        for c4 in range(4):
                            C.op('pool', lambda e: e.memset(hb[:, 8192 + c4 * 4096:8192 + (c4 + 1) * 4096], 0.0), writes=[('kTz0', c4)])
                        proj_fm(xT, wbuf[1], 'w0b1', 512, qT, 0, 'qT', scale=0.125)
                        load_w_chunk(wbuf[1], 'w0b1', w_in_0, 1024, 512)

                        def k_evac(f, tb, ps, pk):
                            cols = slice(tb * 512, (tb + 1) * 512)
                            zk = [('kTz0', c4) for c4 in range(4)]
                            evac(kTz[0:64, 2 * f, cols], ps[0:64, :], reads=[pk] + zk, writes=[('kTz', 2 * f, tb)], eng='act')
                            evac(kTz[64:128, 2 * f + 1, cols], ps[64:128, :], reads=[pk] + zk, writes=[('kTz', 2 * f + 1, tb)], eng='dve')
                        proj_fm(xT, wbuf[0], 'w0b0', 512, kTz, 0, 'kTz', evac_fn=k_evac)
                        proj_tm(xT, wbuf[1], 'w0b1', 512, v, 'v')
                        C.barrier()
                    ssm(s1, uT, ocat, co=(None if upto == "ssm" else sba_gen), co_per_iter=5)
                    C.barrier()
                bank_range[:] = [0, 3]
                dump("dbg_ocat", ocat[:, :, :], [])
                if upto == "ssm":
                    bank_range[:] = [0, 8]
                    return
                with ExitStack() as s3:
                    wo = sb("wo", [128, KT, D], BF16, s3)
                    C.dma('pool', wo[:, :, 0:512], w_out_0[:, 0:512].rearrange("(kt p) n -> p kt n", p=128), writes=['wo'])
                    C.dma('pool', wo[:, :, 512:1024], w_out_0[:, 512:1024].rearrange("(kt p) n -> p kt n", p=128), writes=['wo2'])
                    lnp = sb("lnp", [128, 2, D], F32, s3)
                    load_ln(lnp, 0)
                    for _ in sba_gen:
                        pass
                    C.barrier()
                    bank_range[:] = [0, 8]
                    dump("dbg_ocat", ocat[:, :, :], [])
                    if upto == "attn":
                        return
                    reload_h(x_d)
                    out_proj_ln(ocat, wo, ['wo', 'wo2'], 0, lnp)
                    C.barrier()

        def out_proj_ln(catT, wo, wkeys, lnidx, lnp):
            for t in range(NT):
                for half in range(2):
                    ps, pk = bank()
                    for k in range(KT):
                        _mm(C, ps[:, :], catT[:, k, t * 128:(t + 1) * 128], wo[:, k, half * 512:(half + 1) * 512], k == 0, k == KT - 1,
                            reads=wkeys, writes=[pk])
                    C.op('dve', lambda e: e.scalar_tensor_tensor(out=h[:, t, half * 512:(half + 1) * 512], in0=h[:, t, half * 512:(half + 1) * 512],
                                                                 scalar=ALPHA, in1=ps[:, :], op0=ALU.mult, op1=ALU.add),
                         reads=[pk, ('h', t)], writes=[('h', t)])
                layer_norm_tile(t, lnidx, lnp)

        def sincos(s1, ang, n, sin_out, cos_out, tag):
            tmp = sb("sc_tmp_" + tag, [128, n], F32, s1)
            ki = sb("sc_ki_" + tag, [128, n], I32, s1)
            kf = sb("sc_kf_" + tag, [128, n], F32, s1)
            for which, outp in ((0, sin_out), (1, cos_out)):
                off = 0.0 if which == 0 else PI / 2
                C.op('dve', lambda e: e.tensor_scalar(out=tmp[:, :], in0=ang, scalar1=off, scalar2=1.0 / (2 * PI), op0=ALU.add, op1=ALU.mult),
                     reads=['ang_' + tag], writes=['sc_tmp'])
                C.op('dve', lambda e: e.tensor_copy(out=ki[:, :], in_=tmp[:, :]), reads=['sc_tmp'], writes=['sc_ki'])
                C.op('dve', lambda e: e.tensor_copy(out=kf[:, :], in_=ki[:, :]), reads=['sc_ki'], writes=['sc_kf'])
                C.op('dve', lambda e: e.tensor_scalar(out=kf[:, :], in0=kf[:, :], scalar1=-2 * PI, scalar2=off, op0=ALU.mult, op1=ALU.add),
                     reads=['sc_kf'], writes=['sc_kf'])
                C.op('dve', lambda e: e.tensor_tensor(out=tmp[:, :], in0=ang, in1=kf[:, :], op=ALU.add), reads=['sc_kf', 'ang_' + tag], writes=['sc_tmp'])
                C.op('dve', lambda e: e.tensor_scalar(out=kf[:, :], in0=tmp[:, :], scalar1=PI, scalar2=-2 * PI, op0=ALU.is_gt, op1=ALU.mult), reads=['sc_tmp'], writes=['sc_kf'])
                C.op('dve', lambda e: e.tensor_tensor(out=tmp[:, :], in0=tmp[:, :], in1=kf[:, :], op=ALU.add), reads=['sc_kf', 'sc_tmp'], writes=['sc_tmp'])
                C.op('dve', lambda e: e.tensor_scalar(out=kf[:, :], in0=tmp[:, :], scalar1=-PI, scalar2=2 * PI, op0=ALU.is_lt, op1=ALU.mult), reads=['sc_tmp'], writes=['sc_kf'])
                C.op('dve', lambda e: e.tensor_tensor(out=tmp[:, :], in0=tmp[:, :], in1=kf[:, :], op=ALU.add), reads=['sc_kf', 'sc_tmp'], writes=['sc_tmp'])
                C.op('dve', lambda e: e.tensor_scalar(out=tmp[:, :], in0=tmp[:, :], scalar1=-PI, scalar2=PI, op0=ALU.max, op1=ALU.min), reads=['sc_tmp'], writes=['sc_tmp'])
                C.op('act', lambda e: e.activation(out=outp, in_=tmp[:, :], func=AF.Sin), reads=['sc_tmp'], writes=['sc_out_%s_%d' % (tag, which)])

        def ssm(s1, uT, ocat, co=None, co_per_iter=0):
            T = 128
            NCH = L // T
            lam_re = sb("lam_re", [128, 16], F32, s1)
            lam_im = sb("lam_im", [128, 16], F32, s1)
            dt = sb("dt", [128, 16], F32, s1)
            mag = sb("mag", [128, 16], F32, s1)
            th = sb("th", [128, 16], F32, s1)
            C.dma('sp', lam_re[:, :], lam_re_d[:, :], writes=['lam_re'])
            C.dma('sp', lam_im[:, :], lam_im_d[:, :], writes=['lam_im'])
            C.dma('sp', dt[:, :], logdt_d[:, :], writes=['dt'])
            C.op('act', lambda e: e.activation(out=dt[:, :], in_=dt[:, :], func=AF.Exp), reads=['dt'], writes=['dt'])
            C.op('dve', lambda e: e.tensor_tensor(out=mag[:, :], in0=lam_re[:, :], in1=dt[:, :], op=ALU.mult), reads=['lam_re', 'dt'], writes=['mag'])
            C.op('act', lambda e: e.activation(out=mag[:, :], in_=mag[:, :], func=AF.Exp), reads=['mag'], writes=['mag'])
            C.op('dve', lambda e: e.tensor_tensor(out=th[:, :], in0=lam_im[:, :], in1=dt[:, :], op=ALU.mult), reads=['lam_im', 'dt'], writes=['ang_th'])
            sn = sb("sn", [128, 16], F32, s1)
            cs = sb("cs", [128, 16], F32, s1)
            with ExitStack() as sx:
                sincos(sx, th[:, :], 16, sn[:, :], cs[:, :], 'th')
                C.barrier()
            a_re = sb("a_re", [128, 16], F32, s1)
            a_im = sb("a_im", [128, 16], F32, s1)
            C.op('dve', lambda e: e.tensor_tensor(out=a_re[:, :], in0=mag[:, :], in1=cs[:, :], op=ALU.mult), reads=['mag', 'sc_out_th_1'], writes=['a_re'])
            C.op('dve', lambda e: e.tensor_tensor(out=a_im[:, :], in0=mag[:, :], in1=sn[:, :], op=ALU.mult), reads=['mag', 'sc_out_th_0'], writes=['a_im'])
            den = sb("den", [128, 16], F32, s1)
            t1 = sb("t1", [128, 16], F32, s1)
            t2 = sb("t2", [128, 16], F32, s1)
            f_re = sb("f_re", [128, 16], F32, s1)
            f_im = sb("f_im", [128, 16], F32, s1)
            nr = sb("nr", [128, 16], F32, s1)
            V = lambda fn, r, w: C.op('dve', fn, reads=r, writes=w)
            V(lambda e: e.tensor_tensor(out=den[:, :], in0=lam_re[:, :], in1=lam_re[:, :], op=ALU.mult), ['lam_re'], ['den'])
            V(lambda e: e.tensor_tensor(out=t1[:, :], in0=lam_im[:, :], in1=lam_im[:, :], op=ALU.mult), ['lam_im'], ['t1'])
            V(lambda e: e.tensor_tensor(out=den[:, :], in0=den[:, :], in1=t1[:, :], op=ALU.add), ['den', 't1'], ['den'])
            V(lambda e: e.reciprocal(out=den[:, :], in_=den[:, :]), ['den'], ['den'])
            V(lambda e: e.tensor_scalar(out=nr[:, :], in0=a_re[:, :], scalar1=-1.0, scalar2=None, op0=ALU.add), ['a_re'], ['nr'])
            V(lambda e: e.tensor_tensor(out=t1[:, :], in0=nr[:, :], in1=lam_re[:, :], op=ALU.mult), ['nr', 'lam_re', 't1'], ['t1'])
            V(lambda e: e.tensor_tensor(out=t2[:, :], in0=a_im[:, :], in1=lam_im[:, :], op=ALU.mult), ['a_im', 'lam_im'], ['t2'])
            V(lambda e: e.tensor_tensor(out=t1[:, :], in0=t1[:, :], in1=t2[:, :], op=ALU.add), ['t1', 't2'], ['t1'])
            V(lambda e: e.tensor_tensor(out=f_re[:, :], in0=t1[:, :], in1=den[:, :], op=ALU.mult), ['t1', 'den'], ['f_re'])
            V(lambda e: e.tensor_tensor(out=t1[:, :], in0=a_im[:, :], in1=lam_re[:, :], op=ALU.mult), ['a_im', 'lam_re', 't1'], ['t1'])
            V(lambda e: e.tensor_tensor(out=t2[:, :], in0=nr[:, :], in1=lam_im[:, :], op=ALU.mult), ['nr', 'lam_im', 't2'], ['t2'])
            V(lambda e: e.tensor_tensor(out=t1[:, :], in0=t1[:, :], in1=t2[:, :], op=ALU.subtract), ['t1', 't2'], ['t1'])
            V(lambda e: e.tensor_tensor(out=f_im[:, :], in0=t1[:, :], in1=den[:, :], op=ALU.mult), ['t1', 'den'], ['f_im'])
            iot = sb("iot", [128, T], F32, s1)
            C.op('pool', lambda e: e.iota(iot[:, :], pattern=[[1, T]], base=0, channel_multiplier=0, allow_small_or_imprecise_dtypes=True), writes=['iot'])
            cosT = sb("cosT", [128, 16, T], F32, s1)
            sinT = sb("sinT", [128, 16, T], F32, s1)
            with ExitStack() as sx:
                ang = sb("ang", [128, 16, T], F32, sx)
                for j in range(16):
                    V(lambda e: e.tensor_scalar(out=ang[:, j, :], in0=iot[:, :], scalar1=th[:, j:j + 1], scalar2=None, op0=ALU.mult), ['iot', 'ang_th'], ['ang_tab'])
                sincos(sx, ang[:, :, :].rearrange("p a b -> p (a b)"), 16 * T, sinT[:, :, :].rearrange("p a b -> p (a b)"),
                       cosT[:, :, :].rearrange("p a b -> p (a b)"), 'tab')
                C.barrier()
            Rre = sb("Rre", [128, 16, T], F32, s1)
            Rim = sb("Rim", [128, 16, T], F32, s1)
            tmpT = sb("tmpT", [128, T], F32, s1)
            for j in range(16):
                V(lambda e: e.tensor_scalar(out=tmpT[:, :], in0=sinT[:, j, :], scalar1=f_im[:, j:j + 1], scalar2=None, op0=ALU.mult), ['sc_out_tab_0', 'f_im', 'tmpT'], ['tmpT'])
                V(lambda e: e.scalar_tensor_tensor(out=Rre[:, j, :], in0=cosT[:, j, :], scalar=f_re[:, j:j + 1], in1=tmpT[:, :], op0=ALU.mult, op1=ALU.add),
                  ['sc_out_tab_1', 'f_re', 'tmpT'], ['Rre'])
                V(lambda e: e.tensor_scalar(out=tmpT[:, :], in0=sinT[:, j, :], scalar1=f_re[:, j:j + 1], scalar2=None, op0=ALU.mult), ['sc_out_tab_0', 'f_re', 'tmpT'], ['tmpT'])
                V(lambda e: e.scalar_tensor_tensor(out=Rim[:, j, :], in0=cosT[:, j, :], scalar=f_im[:, j:j + 1], in1=tmpT[:, :], op0=ALU.mult, op1=ALU.subtract),
                  ['sc_out_tab_1', 'f_im', 'tmpT'], ['Rim'])
            angE = sb("angE", [128, 16], F32, s1)
            Ere = sb("Ere", [128, 16], F32, s1)
            Eim = sb("Eim", [128, 16], F32, s1)
            V(lambda e: e.tensor_scalar(out=angE[:, :], in0=th[:, :], scalar1=float(T), scalar2=None, op0=ALU.mult), ['ang_th'], ['ang_E'])
            with ExitStack() as sx:
                sincos(sx, angE[:, :], 16, Eim[:, :], Ere[:, :], 'E')
                C.barrier()
            MAGB = bool(int(os.environ.get('DBG_MAGB', '1')))
            if not MAGB:
                magT = sb("magT", [128, 16, T], F32, s1)
                for j in range(16):
                    V(lambda e: e.tensor_scalar(out=magT[:, j, :], in0=iot[:, :], scalar1=0.0, scalar2=mag[:, j:j + 1], op0=ALU.mult, op1=ALU.add), ['iot', 'mag'], ['magT'])
            breT = sb("breT", [128, 16, 128], BF16, s1)
            bimT = sb("bimT", [128, 16, 128], BF16, s1)
            creT = sb("creT", [128, 16, 128], BF16, s1)
            cimT = sb("cimT", [128, 16, 128], BF16, s1)
            C.dma('pool', breT[:, :, :], bre_d[:, :, :], writes=['breT'])
            C.dma('pool', bimT[:, :, :], bim_d[:, :, :], writes=['bimT'])
            C.dma('pool', creT[:, :, :], cre_d[:, :, :], writes=['creT'])
            C.dma('pool', cimT[:, :, :], cim_d[:, :, :], writes=['cimT'])
            C.op('pool', lambda e: e.tensor_scalar(out=cimT[:, :, :], in0=cimT[:, :, :], scalar1=-1.0, scalar2=None, op0=ALU.mult), reads=['cimT'], writes=['cimT'])
            ssmd = sb("ssmd", [128, 4], F32, s1)
            C.dma('sp', ssmd[:, :], ssmd_d[:, :], writes=['ssmd'])
            sw = ExitStack()
            NB = 2
            bt_re = [sb("bt_re%d" % i, [128, 4, T], F32, sw) for i in range(NB)]
            bt_im = [sb("bt_im%d" % i, [128, 4, T], F32, sw) for i in range(NB)]
            m1 = [sb("m1_%d" % i, [128, 4, T], F32, sw) for i in range(NB)]
            m2 = [sb("m2_%d" % i, [128, 4, T], F32, sw) for i in range(NB)]
            xt_re = [sb("xt_re%d" % i, [128, 4, T], F32, sw) for i in range(NB)]
            xt_im = [sb("xt_im%d" % i, [128, 4, T], F32, sw) for i in range(NB)]
            NBX = 3
            x_re = [sb("x_re%d" % i, [128, 4, T], BF16, sw) for i in range(NBX)]
            x_im = [sb("x_im%d" % i, [128, 4, T], BF16, sw) for i in range(NBX)]
            p1, p2 = m1, m2
            carry_re = sb("carry_re", [128, 16], F32, sw)
            carry_im = sb("carry_im", [128, 16], F32, sw)
            c1 = sb("c1", [128, 4], F32, sw)
            c2 = sb("c2", [128, 4], F32, sw)
            c3 = sb("c3", [128, 4], F32, sw)
            c4 = sb("c4", [128, 4], F32, sw)
            ytmp = [sb("ytmp%d" % i, [128, T], F32, sw) for i in range(3)]
            C.op('dve', lambda e: e.memset(carry_re[:, :], 0.0), writes=[('carry', g) for g in range(4)])
            C.op('dve', lambda e: e.memset(carry_im[:, :], 0.0), writes=[('carryi', g) for g in range(4)])
            fl = lambda tns: tns[:, :, :].rearrange("p a b -> p (a b)")
            G = lambda fn, r, w: C.op('pool', fn, reads=r, writes=w)
            gA = iot
            pend_g2 = []
            if os.environ.get('DBG_MEM'):
                print("SBUF remaining at SSM peak:", nc.sbuf_bytes_remaining)

            def make_iter(c, gq, it):
                b = it % NB
                bx = it % NBX
                yb = it % 3
                js = slice(4 * gq, 4 * gq + 4)
                kb = 'ssmbuf%d' % b
                kx = 'ssmx%d' % bx
                R_re = Rre[:, js, :].rearrange("p a b -> p (a b)")
                R_im = Rim[:, js, :].rearrange("p a b -> p (a b)")
                cT = cosT[:, js, :].rearrange("p a b -> p (a b)")
                sT = sinT[:, js, :].rearrange("p a b -> p (a b)")
                ucol = uT[:, gq, c * T:(c + 1) * T]
                ukey = ('uT', gq, c // 4)
                st_ = {}
                P1, P2 = [], []

                def t_mm():
                    st_['psr'], st_['pkr'] = PS[0], 'ps0'
                    st_['psi'], st_['pki'] = PS[1], 'ps1'
                    for jj in range(4):
                        j = 4 * gq + jj
                        _mm(C, st_['psr'][:, jj * T:(jj + 1) * T], breT[:, j, :], ucol, True, True, reads=['breT', ukey], writes=[st_['pkr']])
                        _mm(C, st_['psi'][:, jj * T:(jj + 1) * T], bimT[:, j, :], ucol, True, True, reads=['bimT', ukey], writes=[st_['pki']])
                P1.append(t_mm)
                P1.append(lambda: V(lambda e: e.tensor_tensor(out=fl(m1[b]), in0=st_['psr'][:, :], in1=R_re, op=ALU.mult), [st_['pkr'], 'Rre'], [kb + 'm1']))
                P1.append(lambda: V(lambda e: e.tensor_tensor(out=fl(m2[b]), in0=st_['psi'][:, :], in1=R_im, op=ALU.mult), [st_['pki'], 'Rim'], [kb + 'm2']))
                P1.append(lambda: V(lambda e: e.tensor_tensor(out=fl(bt_re[b]), in0=fl(m1[b]), in1=fl(m2[b]), op=ALU.subtract), [kb + 'm1', kb + 'm2'], [kb + 'btre']))
                P1.append(lambda: V(lambda e: e.tensor_tensor(out=fl(m1[b]), in0=st_['psi'][:, :], in1=R_re, op=ALU.mult), [st_['pki'], 'Rre', kb + 'm1'], [kb + 'm1']))
                P1.append(lambda: V(lambda e: e.tensor_tensor(out=fl(m2[b]), in0=st_['psr'][:, :], in1=R_im, op=ALU.mult), [st_['pkr'], 'Rim', kb + 'm2'], [kb + 'm2']))
                P1.append(lambda: V(lambda e: e.tensor_tensor(out=fl(bt_im[b]), in0=fl(m1[b]), in1=fl(m2[b]), op=ALU.add), [kb + 'm1', kb + 'm2'], [kb + 'btim']))
                for jj in range(4):
                    def t_scan(jj=jj):
                        j = 4 * gq + jj
                        V(lambda e: e.tensor_tensor_scan(out=xt_re[b][:, jj, :], data0=(mag[:, j:j + 1].to_broadcast([128, T]) if MAGB else magT[:, j, :]), data1=bt_re[b][:, jj, :], initial=carry_re[:, j:j + 1], op0=ALU.mult, op1=ALU.add),
                          ['mag', kb + 'btre', ('carry', gq)], [kb + 'xtre'])
                        V(lambda e: e.tensor_tensor_scan(out=xt_im[b][:, jj, :], data0=(mag[:, j:j + 1].to_broadcast([128, T]) if MAGB else magT[:, j, :]), data1=bt_im[b][:, jj, :], initial=carry_im[:, j:j + 1], op0=ALU.mult, op1=ALU.add),
                          ['mag', kb + 'btim', ('carryi', gq)], [kb + 'xtim'])
                    P1.append(t_scan)
                if c < NCH - 1:
                    lr = xt_re[b][:, :, T - 1]
                    li = xt_im[b][:, :, T - 1]
                    P1.append(lambda: V(lambda e: e.tensor_tensor(out=c1[:, :], in0=lr, in1=Ere[:, js], op=ALU.mult), [kb + 'xtre', 'sc_out_E_1', 'c1'], ['c1']))
                    P1.append(lambda: V(lambda e: e.tensor_tensor(out=c2[:, :], in0=li, in1=Eim[:, js], op=ALU.mult), [kb + 'xtim', 'sc_out_E_0', 'c2'], ['c2']))
                    P1.append(lambda: V(lambda e: e.tensor_tensor(out=c3[:, :], in0=li, in1=Ere[:, js], op=ALU.mult), [kb + 'xtim', 'sc_out_E_1', 'c3'], ['c3']))
                    P1.append(lambda: V(lambda e: e.tensor_tensor(out=c4[:, :], in0=lr, in1=Eim[:, js], op=ALU.mult), [kb + 'xtre', 'sc_out_E_0', 'c4'], ['c4']))
                    P1.append(lambda: V(lambda e: e.tensor_tensor(out=carry_re[:, js], in0=c1[:, :], in1=c2[:, :], op=ALU.subtract), ['c1', 'c2', ('carry', gq)], [('carry', gq)]))
                    P1.append(lambda: V(lambda e: e.tensor_tensor(out=carry_im[:, js], in0=c3[:, :], in1=c4[:, :], op=ALU.add), ['c3', 'c4', ('carryi', gq)], [('carryi', gq)]))
                P2.append(lambda: G(lambda e: e.tensor_tensor(out=fl(p1[b]), in0=fl(xt_re[b]), in1=cT, op=ALU.mult), [kb + 'xtre', 'sc_out_tab_1'], [kb + 'm1']))
                P2.append(lambda: G(lambda e: e.tensor_tensor(out=fl(p2[b]), in0=fl(xt_im[b]), in1=sT, op=ALU.mult), [kb + 'xtim', 'sc_out_tab_0'], [kb + 'm2']))
                P2.append(lambda: V(lambda e: e.tensor_tensor(out=fl(x_re[bx]), in0=fl(p1[b]), in1=fl(p2[b]), op=ALU.subtract), [kb + 'm1', kb + 'm2'], [kx + 'xre']))
                P2.append(lambda: G(lambda e: e.tensor_tensor(out=fl(p1[b]), in0=fl(xt_re[b]), in1=sT, op=ALU.mult), [kb + 'xtre', 'sc_out_tab_0', kb + 'm1'], [kb + 'm1']))
                P2.append(lambda: G(lambda e: e.tensor_tensor(out=fl(p2[b]), in0=fl(xt_im[b]), in1=cT, op=ALU.mult), [kb + 'xtim', 'sc_out_tab_1', kb + 'm2'], [kb + 'm2']))
                P2.append(lambda: V(lambda e: e.tensor_tensor(out=fl(x_im[bx]), in0=fl(p1[b]), in1=fl(p2[b]), op=ALU.add), [kb + 'm1', kb + 'm2'], [kx + 'xim']))

                def t_y():
                    psy, pky = PS[2], 'ps2'
                    for jj in range(4):
                        j = 4 * gq + jj
                        _mm(C, psy[:, 0:T], creT[:, j, :], x_re[bx][:, jj, :], jj == 0, False, reads=['creT', kx + 'xre'], writes=[pky])
                        _mm(C, psy[:, 0:T], cimT[:, j, :], x_im[bx][:, jj, :], False, jj == 3, reads=['cimT', kx + 'xim'], writes=[pky])
                    V(lambda e: e.scalar_tensor_tensor(out=ytmp[yb][:, :], in0=ucol, scalar=ssmd[:, gq:gq + 1], in1=psy[:, 0:T],
                                                       op0=ALU.mult, op1=ALU.add), [pky, 'ssmd', ukey], ['ytmp%d' % yb])
                    if "dbg_ssm_y" in dbg:
                        C.dma('sp', dbg["dbg_ssm_y"][:, gq, c * T:(c + 1) * T], ytmp[yb][:, :], reads=['ytmp%d' % yb])

                def t_g():
                    yk = 'ytmp%d' % yb
                    yv = ytmp[yb][:, :]
                    V(lambda e: e.tensor_tensor(out=gA[:, :], in0=yv, in1=yv, op=ALU.mult), [yk, 'gA'], ['gA'])
                    V(lambda e: e.tensor_scalar(out=gA[:, :], in0=gA[:, :], scalar1=0.044715, scalar2=1.0, op0=ALU.mult, op1=ALU.add), ['gA'], ['gA'])
                    V(lambda e: e.tensor_tensor(out=gA[:, :], in0=gA[:, :], in1=yv, op=ALU.mult), [yk, 'gA'], ['gA'])
                    C.op('act', lambda e: e.activation(out=gA[:, :], in_=gA[:, :], func=AF.Exp, scale=-1.5957691216057308), reads=['gA'], writes=['gA'])

                    C.op('act', lambda e: e.activation(out=gA[:, :], in_=gA[:, :], func=AF.Ln, bias=1.0), reads=['gA'], writes=['gA'])
                    C.op('act', lambda e: e.activation(out=gA[:, :], in_=gA[:, :], func=AF.Exp, scale=-1.0), reads=['gA'], writes=['gA'])

                    def t_g2():
                        V(lambda e: e.tensor_tensor(out=ocat[:, 4 + gq, c * T:(c + 1) * T], in0=yv, in1=gA[:, :], op=ALU.mult), [yk, 'gA'], [('ocat', 4 + gq, c // 4)])
                    pend_g2.append(t_g2)
                return P1, P2, t_y, t_g

            iters = [(c, gq) for c in range(NCH) for gq in range(4)]
            prev2 = None
            pend_y = []
            pend_g = []
            for it, (c, gq) in enumerate(iters):
                P1, P2, t_y, t_g = make_iter(c, gq, it)
                while pend_g2:
                    pend_g2.pop(0)()
                if len(pend_g) >= 2:
                    pend_g.pop(0)()
                if len(pend_y) >= 1:
                    ty_ = pend_y.pop(0)
                    ty_[0]()
                    pend_g.append(ty_[1])
                A_, B_ = P1, (prev2[0] if prev2 else [])
                nco = 0
                for k in range(max(len(A_), len(B_))):
                    if k < len(B_):
                        B_[k]()
                    if k < len(A_):
                        A_[k]()
                    if co is not None and k % 3 == 2 and nco < co_per_iter:
                        next(co, None)
                        nco += 1
                if prev2:
                    pend_y.append((prev2[1], prev2[2]))
                prev2 = (P2, t_y, t_g)
                if co is not None:
                    for _ in range(co_per_iter - nco):
                        next(co, None)
            for t_ in prev2[0]:
                t_()
            pend_y.append((prev2[1], prev2[2]))
            for ty_ in pend_y:
                while len(pend_g) >= 2:
                    while pend_g2:
                        pend_g2.pop(0)()
                    pend_g.pop(0)()
                ty_[0]()
                pend_g.append(ty_[1])
            for tg_ in pend_g:
                while pend_g2:
                    pend_g2.pop(0)()
                tg_()
            while pend_g2:
                pend_g2.pop(0)()
            C.barrier()
            sw.close()
            if co is not None:
                bank_range[:] = [0, 3]
            wg = sb("wg", [128, 4, 512], BF16, s1)
            C.dma('pool', wg[:, :, :], w_glu_d.rearrange("(kt p) n -> p kt n", p=128), writes=['wg'])
            sg = [sb("sg%d" % i, [128, 512], BF16, s1) for i in range(4)]
            for tb in range(4):
                for f in range(4):
                    ps, pk = bank()
                    for k in range(4):
                        _mm(C, ps[:, :], wg[:, k, f * 128:(f + 1) * 128], ocat[:, 4 + k, tb * 512:(tb + 1) * 512], k == 0, k == 3,
                            reads=['wg'] + [('ocat', 4 + g, tb) for g in range(4)], writes=[pk])
                    C.op('act', lambda e: e.activation(out=sg[f][:, :], in_=ps[:, :], func=AF.Sigmoid), reads=[pk], writes=['sg%d' % f])
                for f in range(4):
                    V(lambda e: e.tensor_tensor(out=ocat[:, 4 + f, tb * 512:(tb + 1) * 512], in0=ocat[:, 4 + f, tb * 512:(tb + 1) * 512], in1=sg[f][:, :], op=ALU.mult),
                      ['sg%d' % f, ('ocat', 4 + f, tb)], [('ocat', 4 + f, tb)])

        def sb_attention(s2, qT, kT, v, ocat):
            Uneg = sb("Uneg", [128, 128], BF16, s2)
            onesneg = sb("onesneg", [128, 128], BF16, s2)
            C.dma('pool', Uneg[:, :], triu_d[:, :], writes=['Uneg'])
            causs = sb("causs", [128, 128], BF16, s2)
            C.dma('pool', causs[:, :], causs_d[:, :], writes=['causs'])
            C.op('dve', lambda e: e.memset(onesneg[:, :], -1.0), writes=['onesneg'])
            SK1, SK2 = 2, 2
            NBE, NBS, NBW = 2, SK1 + 1, SK2 + 1
            ee = [sb("ee%d" % i, [128, 512], BF16, s2) for i in range(NBE)]
            sp = [sb("sp%d" % i, [128, 512], BF16, s2) for i in range(NBS)]
            ww = [sb("ww%d" % i, [128, 512], BF16, s2) for i in range(NBW)]
            Sb = [[sb("S%d_%d" % (i, j), [128, 512], BF16, s2) for j in range(2)] for i in range(2)]
            steps = []
            blk = 0
            for hd in range(8):
                for qb in range(4):
                    nkt = 4 * qb + 4
                    for i, kt in enumerate(range(nkt - 1, -1, -1)):
                        steps.append(dict(hd=hd, qb=qb, kt=kt, i=i, n=nkt, blk=blk))
                    blk += 1
            pso_of = {}

            def stageA(n):
                st_ = steps[n]
                hd, qb, kt = st_['hd'], st_['qb'], st_['kt']
                hp, ho = hd // 2, (hd % 2) * 64
                c0 = 128 * max(0, kt - 4 * qb)
                be, bs = n % NBE, n % NBS
                psa, pka = PS[3 + n % 3], 'ps%d' % (3 + n % 3)
                st_['psa'], st_['pka'] = psa, pka
                if st_['i'] == 0:
                    for j in range(2):
                        C.op('act', lambda e: e.memzero(Sb[st_['blk'] % 2][j][:, :]), reads=[], writes=[('S', st_['blk'] % 2, j)])
                qcols = slice(qb * 512 + c0, (qb + 1) * 512)
                diag = kt >= 4 * qb
                C.op('pe', lambda e: e.matmul(psa[:, c0:512], lhsT=kT[:, hd, kt * 128:(kt + 1) * 128], rhs=qT[:, hp, qcols], start=True, stop=(not diag), skip_group_check=True),
                     reads=[('kTz', hd, kt // 4), ('qT', hp, qb)], writes=[pka], chain=True)
                if diag:
                    C.op('pe', lambda e: e.matmul(psa[:, c0:c0 + 128], lhsT=identb[:, :], rhs=causs[:, :], start=False, stop=True, skip_group_check=True),
                         reads=['identb', 'causs'], writes=[pka], chain=True)
                C.op('act', lambda e: e.activation(out=ee[be][:, c0:512], in_=psa[:, c0:512], func=AF.Exp), reads=[pka], writes=['ee%d' % be])
                C.op('act', lambda e: e.activation(out=sp[bs][:, c0:512], in_=ee[be][:, c0:512], func=AF.Ln, bias=1.0), reads=['ee%d' % be], writes=['sp%d' % bs])

            def stageB1(n):
                st_ = steps[n]
                hd, qb, kt, i = st_['hd'], st_['qb'], st_['kt'], st_['i']
                c0 = 128 * max(0, kt - 4 * qb)
                bs, bw = n % NBS, n % NBW
                psa, pka = st_['psa'], st_['pka']
                Sprev = Sb[st_['blk'] % 2][(i + 1) % 2]
                Snew = Sb[st_['blk'] % 2][i % 2]
                kprev = ('S', st_['blk'] % 2, (i + 1) % 2)
                knew = ('S', st_['blk'] % 2, i % 2)
                C.op('pe', lambda e: e.matmul(psa[:, c0:512], lhsT=Uneg[:, :], rhs=sp[bs][:, c0:512], start=False, stop=(i == 0), skip_group_check=True),
                     reads=['Uneg', 'sp%d' % bs], writes=[pka], chain=True)
                if i > 0:
                    C.op('pe', lambda e: e.matmul(psa[:, c0:512], lhsT=onesneg[:, :], rhs=Sprev[:, c0:512], start=False, stop=True, skip_group_check=True),
                         reads=['onesneg', kprev], writes=[pka], chain=True)
                C.op('act', lambda e: e.activation(out=ww[bw][:, c0:512], in_=psa[:, c0:512], func=AF.Exp), reads=[pka], writes=['ww%d' % bw])
                if kt > 0:
                    C.op('pe', lambda e: e.matmul(PS[7][:, c0:512], lhsT=identb[:, :], rhs=sp[bs][:, c0:512], start=(i == 0), stop=False, skip_group_check=True),
                         reads=['identb', 'sp%d' % bs], writes=['ps7'], chain=True)
                    C.op('act', lambda e: e.activation(out=Snew[:, c0:512], in_=PS[7][:, c0:512], func=AF.Copy), reads=['ps7'], writes=[knew])

            def stageB2(n):
                st_ = steps[n]
                hd, qb, kt, i = st_['hd'], st_['qb'], st_['kt'], st_['i']
                hp, ho = hd // 2, (hd % 2) * 64
                c0 = 128 * max(0, kt - 4 * qb)
                bw = n % NBW
                if i == 0:
                    pso_of[st_['blk']] = (PS[6], 'ps6')
                pso, pko = pso_of[st_['blk']]
                C.op('pe', lambda e: e.matmul(pso[ho:ho + 64, c0:512], lhsT=v[:, kt, hd * 64:(hd + 1) * 64], rhs=ww[bw][:, c0:512], start=(i == 0), stop=(kt == 0), skip_group_check=True),
                     reads=[('v', kt), 'ww%d' % bw], writes=[pko], chain=True)
                if kt == 0:
                    evac(ocat[ho:ho + 64, hp, qb * 512:(qb + 1) * 512], pso[ho:ho + 64, :], reads=[pko], writes=[('ocat', hp, qb, ho)], eng='act')

            def gen():
                for n in range(len(steps) + SK1 + SK2):
                    if n < len(steps):
                        stageA(n)
                    if 0 <= n - SK1 < len(steps):
                        stageB1(n - SK1)
                    if 0 <= n - SK1 - SK2 < len(steps):
                        stageB2(n - SK1 - SK2)
                    yield n
            return gen(), len(steps) + SK1 + SK2

        def moe(layer, lnidx, final=False):
            with ExitStack() as s0:
                xT = sb("xTm", [128, KT, L], BF16, s0)
                wr = sb("wr", [128, KT, 16], F32, s0)
                br = sb("br", [128, 16], F32, s0)
                if not os.environ.get('DBG_NOWR'):
                    C.dma('sp', wr[:, :, :].rearrange("p a b -> p (a b)"), w_router_d[:, :], writes=['wr'])
                if not os.environ.get('DBG_NOBR'):
                    C.dma('sp', br[:, :], b_router_d[0:1, :].broadcast_to([128, 16]), writes=['br'])
                xf = [sb("xf%d" % i, [128, 4, 128], F32, s0) for i in range(4)]
                logit = sb("logit", [128, NT, 16], F32, s0)
                psl = PS[7]
                rr = [0]

                def router(t, half, ps, pk):
                    b = rr[0] % 4
                    rr[0] += 1
                    C.op('dve', lambda e: e.tensor_copy(out=xf[b][:, :, :], in_=ps[:, :].rearrange("p (k n) -> p k n", k=4)), reads=[], writes=[pk, 'xf%d' % b])

                    def mm():
                        for kk in range(4):
                            k = half * 4 + kk
                            C.op('pe', lambda e: e.matmul(psl[:, t * 16:(t + 1) * 16], lhsT=xf[b][:, kk, :], rhs=wr[:, k, :], start=(t == 0 and k == 0), stop=(k == 7), skip_group_check=True),
                                 reads=['xf%d' % b, 'wr'], writes=['psl'], chain=True)
                    return mm

                w1b = [sb("w1b%d" % i, [128, KT, 512], BF16, s0) for i in range(2)]
                w3b = [sb("w3b%d" % i, [128, KT, 512], BF16, s0) for i in range(2)]
                w2b = [sb("w2b%d" % i, [128, 4, D], BF16, s0) for i in range(2)]
                hT = sb("hTm", [128, 4, L], BF16, s0)
                sl = [sb("sl%d" % i, [128, 512], F32, s0) for i in range(2)]

                def load_expert(e_):
                    b = e_ % 2
                    C.dma('pool', w1b[b][:, :, :], w1_d[layer][e_].rearrange("(kt p) n -> p kt n", p=128), writes=['w1b%d' % b])
                    C.dma('pool', w3b[b][:, :, :], w3_d[layer][e_].rearrange("(kt p) n -> p kt n", p=128), writes=['w3b%d' % b])
                    C.dma('pool', w2b[b][:, :, :], w2_d[layer][e_].rearrange("(kt p) n -> p kt n", p=128), writes=['w2b%d' % b])

                lnp = sb("lnp", [128, 2, D], F32, s0)
                load_ln(lnp, lnidx)
                load_expert(0)
                bank_range[:] = [0, 7]
                to_feature_major(xT, 'xTm', router=(None if os.environ.get('DBG_NOROUTER') else router))
                V = lambda fn, r, w: C.op('dve', fn, reads=r, writes=w)
                if MSTOP <= 1:
                    C.barrier()
                    return
                aff = sb("aff", [128, NT, 16], F32, s0)
                sel = sb("sel", [128, NT, 16], F32, s0)
                C.op('act', lambda e: e.activation(out=aff[:, :, :].rearrange("p a b -> p (a b)"), in_=psl[:, 0:NT * 16], func=AF.Sigmoid), reads=['psl'], writes=['aff'])
                V(lambda e: e.tensor_tensor(out=sel[:, :, :], in0=aff[:, :, :], in1=br[:, :].unsqueeze(1).to_broadcast([128, NT, 16]), op=ALU.add), ['aff', 'br'], ['sel'])
                sel4 = sel[:, :, :].rearrange("p t (g e) -> p (t g) e", g=4)
                mx1 = sb("mx1", [128, NT * 4], F32, s0)
                mx2 = sb("mx2", [128, NT * 4], F32, s0)
                eq = sb("eq", [128, NT * 4, 4], F32, s0)
                V(lambda e: e.tensor_reduce(out=mx1[:, :], in_=sel4, axis=AX.X, op=ALU.max), ['sel'], ['mx1'])
                V(lambda e: e.tensor_tensor(out=eq[:, :, :], in0=sel4, in1=mx1[:, :].unsqueeze(2).to_broadcast([128, NT * 4, 4]), op=ALU.is_ge), ['sel', 'mx1'], ['eq'])
                V(lambda e: e.scalar_tensor_tensor(out=eq[:, :, :], in0=eq[:, :, :], scalar=-1000.0, in1=sel4, op0=ALU.mult, op1=ALU.add), ['eq', 'sel'], ['eq'])
                V(lambda e: e.tensor_reduce(out=mx2[:, :], in_=eq[:, :, :], axis=AX.X, op=ALU.max), ['eq'], ['mx2'])
                gs = sb("gs", [128, NT, 4], F32, s0)
                V(lambda e: e.tensor_tensor(out=gs[:, :, :].rearrange("p a b -> p (a b)"), in0=mx1[:, :], in1=mx2[:, :], op=ALU.add), ['mx1', 'mx2'], ['gs'])
                gmax = sb("gmax", [128, NT], F32, s0)
                V(lambda e: e.tensor_reduce(out=gmax[:, :], in_=gs[:, :, :], axis=AX.X, op=ALU.max), ['gs'], ['gmax'])
                gsel = sb("gsel", [128, NT, 4], F32, s0)
                V(lambda e: e.tensor_tensor(out=gsel[:, :, :], in0=gs[:, :, :], in1=gmax[:, :].unsqueeze(2).to_broadcast([128, NT, 4]), op=ALU.is_ge), ['gs', 'gmax'], ['gsel'])
                m2 = sb("m2", [128, NT * 4, 4], F32, s0)
                V(lambda e: e.tensor_tensor(out=m2[:, :, :], in0=sel4, in1=mx2[:, :].unsqueeze(2).to_broadcast([128, NT * 4, 4]), op=ALU.is_ge), ['sel', 'mx2'], ['m2'])
                V(lambda e: e.tensor_tensor(out=m2[:, :, :], in0=m2[:, :, :], in1=gsel[:, :, :].rearrange("p a b -> p (a b)").unsqueeze(2).to_broadcast([128, NT * 4, 4]), op=ALU.mult),
                  ['m2', 'gsel'], ['m2'])
                gates = sb("gates", [128, NT, 16], F32, s0)
                V(lambda e: e.tensor_tensor(out=gates[:, :, :].rearrange("p a b -> p (a b)"), in0=aff[:, :, :].rearrange("p a b -> p (a b)"),
                                            in1=m2[:, :, :].rearrange("p a b -> p (a b)"), op=ALU.mult), ['aff', 'm2'], ['gates'])
                gsum = sb("gsum", [128, NT], F32, s0)
                V(lambda e: e.tensor_reduce(out=gsum[:, :], in_=gates[:, :, :], axis=AX.X, op=ALU.add), ['gates'], ['gsum'])
                V(lambda e: e.reciprocal(out=gsum[:, :], in_=gsum[:, :]), ['gsum'], ['gsum'])
                V(lambda e: e.tensor_tensor(out=gates[:, :, :], in0=gates[:, :, :], in1=gsum[:, :].unsqueeze(2).to_broadcast([128, NT, 16]), op=ALU.mult), ['gates', 'gsum'], ['gates'])
                dump("dbg_gates%d" % layer, gates[:, :, :], ['gates'])
                if MSTOP <= 2:
                    C.barrier()
                    return
                for t in range(NT):
                    C.op('act', lambda e: e.activation(out=h[:, t, :], in_=h[:, t, :], func=AF.Copy, scale=ALPHA),
                         reads=[('h', t)], writes=[('h', t)])
                si = 0
                xk = [('xTm', t) for t in range(NT)]
                def up(e_, tb):
                    nonlocal si
                    b = e_ % 2
                    for f in range(4):
                        ps1, pk1 = bank()
                        ps3, pk3 = bank()
                        for k in range(KT):
                            _mm(C, ps1[:, :], w1b[b][:, k, f * 128:(f + 1) * 128], xT[:, k, tb * 512:(tb + 1) * 512], k == 0, k == KT - 1,
                                reads=['w1b%d' % b] + xk[tb * 4:tb * 4 + 4], writes=[pk1])
                        for k in range(KT):
                            _mm(C, ps3[:, :], w3b[b][:, k, f * 128:(f + 1) * 128], xT[:, k, tb * 512:(tb + 1) * 512], k == 0, k == KT - 1,
                                reads=['w3b%d' % b] + xk[tb * 4:tb * 4 + 4], writes=[pk3])
                        sbi = si % 2
                        si += 1
                        C.op('act', lambda e: e.activation(out=sl[sbi][:, :], in_=ps1[:, :], func=AF.Silu), reads=[pk1], writes=['sl%d' % sbi])
                        V(lambda e: e.tensor_tensor(out=hT[:, f, tb * 512:(tb + 1) * 512], in0=sl[sbi][:, :], in1=ps3[:, :], op=ALU.mult),
                          ['sl%d' % sbi, pk3], [('hTm', f, tb)])

                def down(e_, tb):
                    b = e_ % 2
                    for tt in range(4):
                        t = tb * 4 + tt
                        for half in range(2):
                            ps, pk = bank()
                            for f in range(4):
                                _mm(C, ps[:, :], hT[:, f, t * 128:(t + 1) * 128], w2b[b][:, f, half * 512:(half + 1) * 512], f == 0, f == 3,
                                    reads=['w2b%d' % b, ('hTm', f, tb)], writes=[pk])
                            V(lambda e: e.scalar_tensor_tensor(out=h[:, t, half * 512:(half + 1) * 512], in0=ps[:, :], scalar=gates[:, t, e_:e_ + 1],
                                                               in1=h[:, t, half * 512:(half + 1) * 512], op0=ALU.mult, op1=ALU.add),
                              [pk, 'gates', ('h', t)], [('h', t)])

                pairs = [(e_, tb) for e_ in range(NEXP) for tb in range(4)]
                for idx, (e_, tb) in enumerate(pairs):
                    if idx == 0 and NEXP > 1:
                        load_expert(1)
                    up(e_, tb)
                    if idx >= 1:
                        down(*pairs[idx - 1])
                    if tb == 0 and e_ >= 1 and e_ + 1 < NEXP:
                        load_expert(e_ + 1)
                down(*pairs[-1])
                for t in range(NT):
                    layer_norm_tile(t, lnidx, lnp)
                    if final:
                        C.dma('sp', out_d[t * 128:(t + 1) * 128, :], h[:, t, :], reads=[('h', t)])
                C.barrier()
            bank_range[:] = [0, 8]


        def nsa_mixer():
            V = lambda fn, r, w: C.op('dve', fn, reads=r, writes=w)
            G = lambda fn, r, w: C.op('pool', fn, reads=r, writes=w)
            A = lambda fn, r, w: C.op('act', fn, reads=r, writes=w)
            with ExitStack() as s0:
                xo = sb("xo", [128, NT * D], BF16, s0)
                obuf = xo[:, :].rearrange("p (t d) -> p t d", t=NT)
                wo = sb("wo1", [128, KT, D], BF16, s0)
                C.dma('pool', wo[:, :, 0:512], w_out_1[:, 0:512].rearrange("(kt p) n -> p kt n", p=128), writes=['wo'])
                C.dma('pool', wo[:, :, 512:1024], w_out_1[:, 512:1024].rearrange("(kt p) n -> p kt n", p=128), writes=['wo2'])
                lnp = sb("lnp", [128, 2, D], F32, s0)
                load_ln(lnp, 2)
                with ExitStack() as sp_:
                    qT = hb[:, 0:16384].rearrange("p (a n) -> p a n", a=8)
                    ksd = hb[:, 16384:24576].rearrange("p (a n) -> p a n", a=4)
                    kwd = hb[:, 24576:32768].rearrange("p (a n) -> p a n", a=4)
                    kvTz = hb[:, 0:16384].rearrange("p (a n) -> p a n", a=8)
                    vsa = sb("vsa", [128, NT, 4, 65], BF16, sp_)
                    vwa = sb("vwa", [128, NT, 4, 65], BF16, sp_)
                    gate = sb("gate", [128, NT, 48], F32, sp_)
                    kcmp = sb("kcmp", [128, 4, 128], BF16, sp_)
                    V(lambda e: e.memset(kcmp[:, :, :], 0.0), [], ['kcmp0'])
                    vca = sb("vca", [128, 4, 97], BF16, sp_)
                    cmask = sb("cmask", [128, L], BF16, sp_)
                    esel = sb("esel", [128, NT, 128], BF16, sp_)
                    caus = sb("caus", [128, 128], BF16, sp_)
                    anti = sb("anti", [128, 128], BF16, sp_)
                    forceb = sb("forceb", [128, NT, 32], F32, sp_)
                    for c in range(2):
                        C.dma('pool', cmask[:, c * 1024:(c + 1) * 1024], cmask_d[:, c * 1024:(c + 1) * 1024], writes=[('cmask', c)])
                        C.dma('pool', esel[:, c * 8:(c + 1) * 8, :], esel_d[:, c * 8:(c + 1) * 8, :], writes=[('esel', c)])
                    C.dma('pool', caus[:, :], caus_d[:, :], writes=['caus'])
                    C.dma('pool', anti[:, :], anti_d[:, :], writes=['anti'])
                    C.dma('sp', forceb[:, :, :], forceb_d[:, :, :], writes=['forceb'])
                    rowmask = sb("rowmask", [128, 4], F32, sp_)
                    C.dma('sp', rowmask[:, :], rowmask_d[:, :], writes=['rowmask'])
                    ovaug = sb("ovaug", [128, 33], F32, sp_)
                    C.dma('sp', ovaug[:, :], ovaug_d[:, :], writes=['ovaug'])
                    for g in range(4):
                        V(lambda e: e.tensor_copy(out=vca[:, g, 64:97], in_=ovaug[:, :]), ['ovaug'], [('vca1', g)])
                    V(lambda e: e.memset(vsa[:, :, :, 64:65], 1.0), [], ['vsa1'])
                    V(lambda e: e.memset(vwa[:, :, :, 64:65], 1.0), [], ['vwa1'])
                    with ExitStack() as sx:
                        xT = xo[:, :].rearrange("p (k n) -> p k n", k=KT)
                        to_feature_major(xT, 'xT')
                        for t in range(NT):
                            C.dma('sp', hspill_d[t * 128:(t + 1) * 128, :], h[:, t, :], reads=[('h', t)])
                        C.barrier()
                        xk = lambda tb: [('xT', t) for t in range(tb * 4, tb * 4 + 4)]

                        def loadw(wb, key, col, ncols):
                            C.dma('pool', wb[:, :, 0:ncols], w1all_d[:, col:col + ncols].rearrange("(kt p) n -> p kt n", p=128), writes=[key])

                        with ExitStack() as sc:
                            with ExitStack() as swb:
                                wb = sb("nwb", [128, KT, 512], BF16, swb)
                                loadw(wb, 'nwb', OFF["kc"], 512)
                                for c4 in range(4):
                                    C.op('pool', lambda e: e.memset(hb[:, c4 * 4096:(c4 + 1) * 4096], 0.0), writes=[('kvz0', c4)])

                                def kv_evac(f, tb, ps, pk):
                                    cols = slice(tb * 512, (tb + 1) * 512)
                                    zk = [('kvz0', c4) for c4 in range(4)]
                                    evac(kvTz[0:64, 2 * f, cols], ps[0:64, :], reads=[pk] + zk, writes=[('kvTz', 2 * f, tb)], eng='act')
                                    evac(kvTz[64:128, 2 * f + 1, cols], ps[64:128, :], reads=[pk] + zk, writes=[('kvTz', 2 * f + 1, tb)], eng='dve')
                                proj_fm(xT, wb, 'nwb', 512, kvTz, 0, 'kvTz', evac_fn=kv_evac)
                                C.barrier()
                            w1b_single = sb("cw1b", [128, 32, 256], BF16, sc)
                            w1b = [w1b_single, w1b_single]
                            posb = [sb("cposb%d" % i, [128, 32], BF16, sc) for i in range(2)]
                            w2k = sb("cw2k", [128, 2, 128], BF16, sc)
                            w2ks = sb("cw2ks", [128, 2, 128], BF16, sc)
                            w2v = sb("cw2v", [128, 2, 64], BF16, sc)
                            cosC = sb("cosC", [128, 128], F32, sc)
                            sinC = sb("sinC", [128, 128], F32, sc)
                            for i in range(2):
                                C.dma('pool', posb[i][:, :], cpos_d[i][:, :], writes=[('cposb', i)])
                            C.dma('pool', w2k[:, :, :], cw2k_d.rearrange("(kt p) n -> p kt n", p=128), writes=['cw2k'])
                            C.dma('pool', w2ks[:, :, :], cw2ksw_d.rearrange("(kt p) n -> p kt n", p=128), writes=['cw2ks'])
                            C.dma('pool', w2v[:, :, :], cw2v_d.rearrange("(kt p) n -> p kt n", p=128), writes=['cw2v'])
                            C.dma('sp', cosC[:, :], cosC_d[:, :], writes=['cosC'])
                            C.dma('sp', sinC[:, :], sinC_d[:, :], writes=['sinC'])
                            bias = sb("cbias", [128, 2, 2], F32, sc)
                            gh = sb("cgh", [128, 2, 4, 2, 128], BF16, sc)
                            t1 = sb("ct1", [128, 128], F32, sc)
                            t2 = sb("ct2", [128, 128], F32, sc)
                            w1keys = lambda i: [('cw1b', lh) for lh in range(4)]
                            for i in range(2):
                                for lh in range(4):
                                    C.dma('pool', w1b[i][:, lh * 8:(lh + 1) * 8, :], cw1_d[i][:, lh * 8:(lh + 1) * 8, :], writes=[('cw1b', lh)])
                                for ht in range(2):
                                    ps, pk = bank()
                                    for l in range(32):
                                        _mm(C, ps[:, 0:1], w1b[i][0:64, l, ht * 128:(ht + 1) * 128], posb[i][0:64, l:l + 1], l == 0, l == 31,
                                            reads=w1keys(i) + [('cposb', i)], writes=[pk])
                                    V(lambda e: e.tensor_copy(out=bias[:, i, ht:ht + 1], in_=ps[:, 0:1]), [pk], [('cbias', i, ht)])
                                for g in range(4):
                                    for ht in range(2):
                                        ps, pk = bank()
                                        for l in range(32):
                                            _mm(C, ps[:, 0:127], w1b[i][:, l, ht * 128:(ht + 1) * 128],
                                                kvTz[:, 4 * i + g, l:l + 16 * 126 + 1:16], l == 0, l == 31,
                                                reads=w1keys(i) + [('kvTz', 4 * i + g, tb) for tb in range(4)], writes=[pk])
                                        A(lambda e: e.activation(out=gh[:, i, g, ht, 0:127], in_=ps[:, 0:127], func=AF.Gelu_apprx_tanh, bias=bias[:, i, ht:ht + 1]),
                                          [pk, ('cbias', i, ht)], [('cgh', i, g, ht)])
                            for g in range(4):
                                ps, pk = bank()
                                ps2, pk2 = bank()
                                for ht in range(2):
                                    _mm(C, ps[:, 0:127], w2k[:, ht, :], gh[:, 0, g, ht, 0:127], ht == 0, ht == 1, reads=['cw2k', ('cgh', 0, g, ht)], writes=[pk])
                                for ht in range(2):
                                    _mm(C, ps2[:, 0:127], w2ks[:, ht, :], gh[:, 0, g, ht, 0:127], ht == 0, ht == 1, reads=['cw2ks', ('cgh', 0, g, ht)], writes=[pk2])
                                V(lambda e: e.tensor_tensor(out=t1[:, 0:127], in0=ps[:, 0:127], in1=cosC[:, 0:127], op=ALU.mult), [pk, 'cosC', 'ct1'], ['ct1'])
                                V(lambda e: e.tensor_tensor(out=t2[:, 0:127], in0=ps2[:, 0:127], in1=sinC[:, 0:127], op=ALU.mult), [pk2, 'sinC', 'ct2'], ['ct2'])
                                b0 = (g % 2) * 64
                                V(lambda e: e.tensor_tensor(out=kcmp[b0:b0 + 64, g, 0:127], in0=t1[b0:b0 + 64, 0:127], in1=t2[b0:b0 + 64, 0:127], op=ALU.add),
                                  ['ct1', 'ct2', 'kcmp0'], [('kcmp', g)])
                                ps3, pk3 = bank()
                                for ht in range(2):
                                    _mm(C, ps3[0:127, 0:64], gh[:, 1, g, ht, 0:127], w2v[:, ht, :], ht == 0, ht == 1, reads=['cw2v', ('cgh', 1, g, ht)], writes=[pk3])
                                evac(vca[0:127, g, 0:64], ps3[0:127, 0:64], reads=[pk3], writes=[('vca0', g)])
                            C.barrier()
                        with ExitStack() as sr:
                            cosF = sb("cosF", [128, L], F32, sr)
                            sinF = sb("sinF", [128, L], F32, sr)
                            for c in range(4):
                                C.dma('sp', cosF[:, c * 512:(c + 1) * 512], cosF_d[:, c * 512:(c + 1) * 512], writes=[('cosF', c)])
                                C.dma('sp', sinF[:, c * 512:(c + 1) * 512], sinF_d[:, c * 512:(c + 1) * 512], writes=[('sinF', c)])
                            NWB = 2
                            wA = [sb("nwA%d" % i, [128, KT, 128], BF16, sr) for i in range(NWB)]
                            wS = [sb("nwS%d" % i, [128, KT, 128], BF16, sr) for i in range(NWB)]
                            r1 = [sb("r1_%d" % i, [128, 512], F32, sr) for i in range(2)]
                            r2 = [sb("r2_%d" % i, [128, 512], F32, sr) for i in range(2)]
                            ri = [0]
                            for c4 in range(4):
                                C.op('pool', lambda e: e.memset(hb[:, 16384 + c4 * 4096:16384 + (c4 + 1) * 4096], 0.0), writes=[('kz0', c4)])
                            jobs = [('rope', 'q', ft, qT, 'nqT') for ft in range(8)] + [('rope', 'ks', ft, ksd, 'ksd') for ft in range(2)] + \
                                   [('rope', 'kw', ft, kwd, 'kwd') for ft in range(2)] + \
                                   [('tm', 'vs', hf, vsa, 'vs') for hf in range(2)] + [('tm', 'vw', hf, vwa, 'vw') for hf in range(2)] + [('gate', 'gate', 0, gate, 'gate')]

                            def load_job(ji):
                                kind, name, idx, _, _ = jobs[ji]
                                b_ = ji % NWB
                                ncols = 48 if kind == 'gate' else 128
                                loadw(wA[b_], 'nwA%d' % b_, OFF[name] + idx * 128, ncols)
                                if kind == 'rope':
                                    loadw(wS[b_], 'nwS%d' % b_, OFF[name + "_sw"] + idx * 128, 128)

                            load_job(0)
                            for ji, (kind, name, idx, dst, dkey) in enumerate(jobs):
                                b_ = ji % NWB
                                if ji + 1 < len(jobs):
                                    load_job(ji + 1)
                                wa, ws = wA[b_], wS[b_]
                                ka, ks_ = 'nwA%d' % b_, 'nwS%d' % b_
                                if kind == 'rope':
                                    for tb in range(4):
                                        ps, pk = bank()
                                        ps2, pk2 = bank()
                                        for k in range(KT):
                                            _mm(C, ps[:, :], wa[:, k, :], xT[:, k, tb * 512:(tb + 1) * 512], k == 0, k == KT - 1, reads=[ka] + xk(tb), writes=[pk])
                                        for k in range(KT):
                                            _mm(C, ps2[:, :], ws[:, k, :], xT[:, k, tb * 512:(tb + 1) * 512], k == 0, k == KT - 1, reads=[ks_] + xk(tb), writes=[pk2])
                                        rb = ri[0] % 2
                                        ri[0] += 1
                                        V(lambda e: e.tensor_tensor(out=r1[rb][:, :], in0=ps[:, :], in1=cosF[:, tb * 512:(tb + 1) * 512], op=ALU.mult),
                                          [pk, ('cosF', tb)], ['r1_%d' % rb])
                                        V(lambda e: e.tensor_tensor(out=r2[rb][:, :], in0=ps2[:, :], in1=sinF[:, tb * 512:(tb + 1) * 512], op=ALU.mult),
                                          [pk2, ('sinF', tb)], ['r2_%d' % rb])
                                        if name == 'q':
                                            V(lambda e: e.tensor_tensor(out=dst[:, idx, tb * 512:(tb + 1) * 512], in0=r1[rb][:, :], in1=r2[rb][:, :], op=ALU.add),
                                              ['r1_%d' % rb, 'r2_%d' % rb], [(dkey, idx, tb)])
                                        else:
                                            zk = [('kz0', c4) for c4 in range(4)]
                                            for hf in range(2):
                                                V(lambda e: e.tensor_tensor(out=dst[hf * 64:(hf + 1) * 64, 2 * idx + hf, tb * 512:(tb + 1) * 512], in0=r1[rb][hf * 64:(hf + 1) * 64, :],
                                                                            in1=r2[rb][hf * 64:(hf + 1) * 64, :], op=ALU.add),
                                                  ['r1_%d' % rb, 'r2_%d' % rb] + zk, [(dkey, 2 * idx + hf, tb)])
                                elif kind == 'tm':
                                    for t in range(NT):
                                        ps, pk = bank()
                                        for k in range(KT):
                                            _mm(C, ps[:, 0:128], xT[:, k, t * 128:(t + 1) * 128], wa[:, k, 0:128], k == 0, k == KT - 1, reads=[ka, ('xT', t)], writes=[pk])
                                        evac(dst[:, t, 2 * idx:2 * idx + 2, 0:64], ps[:, 0:128].rearrange("p (g d) -> p g d", g=2), reads=[pk], writes=[(dkey, t, idx)])
                                else:
                                    for t in range(NT):
                                        ps, pk = bank()
                                        for k in range(KT):
                                            _mm(C, ps[:, 0:48], xT[:, k, t * 128:(t + 1) * 128], wa[:, k, 0:48], k == 0, k == KT - 1, reads=[ka, ('xT', t)], writes=[pk])
                                        A(lambda e: e.activation(out=gate[:, t, :], in_=ps[:, 0:48], func=AF.Sigmoid), [pk], [('gate', t)])
                            C.barrier()
                        C.barrier()
                    with ExitStack() as sa:
                        imp = sb("imp", [128, NT, 128], F32, sa)
                        selbT = sb("selbTz", [128, 4, L], BF16, sa)
                        NE = 6
                        E = [sb("E%d" % i, [128, 512], BF16, sa) for i in range(NE)]
                        ei = [0]
                        rden2 = [sb("rden%d" % i, [128, 4], F32, sa) for i in range(2)]
                        coef2 = [sb("coef%d" % i, [128, 4], F32, sa) for i in range(2)]
                        tmpO = [sb("tmpO%d" % i, [128, 4, 64], F32, sa) for i in range(2)]
                        tmpI = [sb("tmpI%d" % i, [128, 4, 32], F32, sa) for i in range(2)]
                        C.barrier()

                        cmb_i = [0]

                        def combine(psO, pkO, W, h_, tb, bi, first_branch):
                            ci = cmb_i[0] % 2
                            cmb_i[0] += 1
                            rd, cf, tO, tI = rden2[ci], coef2[ci], tmpO[ci], tmpI[ci]
                            kr, kc_, ko, ki = 'rden%d' % ci, 'coef%d' % ci, 'tmpO%d' % ci, 'tmpI%d' % ci
                            view = psO[:, 0:4 * W].rearrange("p (t w) -> p t w", w=W)
                            V(lambda e: e.tensor_scalar(out=rd[:, :], in0=view[:, :, 64], scalar1=1e-30, scalar2=None, op0=ALU.max), [pkO, kr], [kr])
                            V(lambda e: e.reciprocal(out=rd[:, :], in_=rd[:, :]), [kr], [kr])
                            V(lambda e: e.tensor_tensor(out=cf[:, :], in0=rd[:, :], in1=gate[:, 4 * tb:4 * tb + 4, 3 * h_ + bi], op=ALU.mult),
                              [kr, kc_] + [('gate', t) for t in range(4 * tb, 4 * tb + 4)], [kc_])
                            okeys = [('obuf', t, h_) for t in range(4 * tb, 4 * tb + 4)]
                            odst = obuf[:, 4 * tb:4 * tb + 4, h_ * 64:(h_ + 1) * 64]
                            cfb = cf[:, :].unsqueeze(2).to_broadcast([128, 4, 64])
                            if first_branch:
                                V(lambda e: e.tensor_tensor(out=odst, in0=view[:, :, 0:64], in1=cfb, op=ALU.mult), [pkO, kc_], okeys)
                            else:
                                V(lambda e: e.tensor_tensor(out=tO[:, :, :], in0=view[:, :, 0:64], in1=cfb, op=ALU.mult), [pkO, kc_, ko], [ko])
                                V(lambda e: e.tensor_tensor(out=odst, in0=odst, in1=tO[:, :, :], op=ALU.add), [ko] + okeys, okeys)
                            if W == 97:
                                g_ = h_ // 4
                                ikeys = [('imp', t, g_) for t in range(4 * tb, 4 * tb + 4)]
                                idst = imp[:, 4 * tb:4 * tb + 4, g_ * 32:(g_ + 1) * 32]
                                rdb = rd[:, :].unsqueeze(2).to_broadcast([128, 4, 32])
                                if h_ % 4 == 0:
                                    V(lambda e: e.tensor_tensor(out=idst, in0=view[:, :, 65:97], in1=rdb, op=ALU.mult), [pkO, kr], ikeys)
                                else:
                                    V(lambda e: e.tensor_tensor(out=tI[:, :, :], in0=view[:, :, 65:97], in1=rdb, op=ALU.mult), [pkO, kr, ki], [ki])
                                    V(lambda e: e.tensor_tensor(out=idst, in0=idst, in1=tI[:, :, :], op=ALU.add), [ki] + ikeys, ikeys)

                        SK = 4
                        psO_of = {}
                        psO_cnt = [0]

                        def run_pipeline(steps):
                            def stA(n):
                                st_ = steps[n]
                                psS, pkS = PS[n % 5], 'ps%d' % (n % 5)
                                st_['psS'], st_['pkS'] = psS, pkS
                                mms = st_['mms']
                                R = st_['rows']
                                for i_, (lt, rh, (a0, a1), rd) in enumerate(mms):
                                    C.op('pe', lambda e: e.matmul(psS[0:R, a0:a1], lhsT=lt, rhs=rh, start=(i_ == 0), stop=(i_ == len(mms) - 1), skip_group_check=True),
                                         reads=rd, writes=[pkS], chain=True)
                                eb = n % NE
                                c0, c1 = st_['cols']
                                A(lambda e: e.activation(out=E[eb][0:R, c0:c1], in_=psS[0:R, c0:c1], func=AF.Exp, scale=0.125), [pkS], ['E%d' % eb])

                            def stB(n):
                                st_ = steps[n]
                                eb = n % NE
                                R = st_['rows']
                                W = st_['W']
                                if st_['first']:
                                    bo = 5 + psO_cnt[0] % 3
                                    psO_cnt[0] += 1
                                    psO_of[st_['blk']] = (PS[bo], 'ps%d' % bo)
                                psO, pkO = psO_of[st_['blk']]
                                first = st_['first']
                                for tt in range(st_['tt_lo'], st_['tt_hi'] + 1):
                                    C.op('pe', lambda e: e.matmul(psO[:, tt * W:(tt + 1) * W], lhsT=E[eb][0:R, tt * 128:(tt + 1) * 128], rhs=st_['vrhs'],
                                                                  start=first, stop=False, skip_group_check=True),
                                         reads=['E%d' % eb] + st_['vreads'], writes=[pkO], chain=True)
                                    first = False
                                if st_['last']:
                                    combine(psO, pkO, W, st_['h'], st_['tb'], st_['which'], st_['which'] == 0)

                            for n in range(len(steps) + SK):
                                if n < len(steps):
                                    stA(n)
                                if n >= SK:
                                    stB(n - SK)

                        def compressed_steps(h_, tb, blkid):
                            g_ = h_ // 4
                            qt_ = (g_ // 2) * 4 + h_ % 4
                            mms = [(kcmp[:, g_, 0:127], qT[:, qt_, tb * 512:(tb + 1) * 512], (0, 512), [('kcmp', g_), 'kcmp0', ('nqT', qt_, tb)]),
                                   (identb[0:127, 0:127], cmask[0:127, tb * 512:(tb + 1) * 512], (0, 512), ['identb', ('cmask', tb // 2)])]
                            return [dict(blk=blkid, which=0, h=h_, tb=tb, rows=127, W=97, cols=(0, 512), mms=mms, tt_lo=0, tt_hi=3,
                                         first=True, last=True, vrhs=vca[0:127, g_, :], vreads=[('vca0', g_), ('vca1', g_)])]

                        def branch_steps(h_, tb, which, blkid):
                            g_ = h_ // 4
                            qt_ = (g_ // 2) * 4 + h_ % 4
                            kT_ = ksd if which == 1 else kwd
                            va = vsa if which == 1 else vwa
                            kkey = 'ksd' if which == 1 else 'kwd'
                            vkey = 'vs' if which == 1 else 'vw'
                            v1key = 'vsa1' if which == 1 else 'vwa1'
                            kts = list(range(0, 4 * tb + 4)) if which == 1 else list(range(max(0, 4 * tb - 4), 4 * tb + 4))
                            out = []
                            for ki, kt in enumerate(kts):
                                rel = kt - 4 * tb
                                tt_lo = max(0, rel)
                                tt_hi = 3 if which == 1 else min(3, rel + 4)
                                c0, c1 = 128 * tt_lo, 128 * (tt_hi + 1)
                                mms = [(kT_[:, g_, kt * 128:(kt + 1) * 128], qT[:, qt_, tb * 512 + c0:tb * 512 + c1], (c0, c1),
                                        [(kkey, g_, kt // 4), ('nqT', qt_, tb)])]
                                if which == 1:
                                    mms.append((esel[:, kt, :], selbT[:, g_, tb * 512 + c0:tb * 512 + c1], (c0, c1),
                                                [('esel', kt // 8)] + [('selbT', t, g_) for t in range(4 * tb, 4 * tb + 4)]))
                                if 0 <= rel <= 3:
                                    mms.append((identb[:, :], caus[:, :], (128 * rel, 128 * rel + 128), ['identb', 'caus']))
                                if which == 2 and 0 <= rel + 4 <= 3:
                                    mms.append((identb[:, :], anti[:, :], (128 * (rel + 4), 128 * (rel + 4) + 128), ['identb', 'anti']))
                                out.append(dict(blk=blkid, which=which, h=h_, tb=tb, rows=128, W=65, cols=(c0, c1), mms=mms, tt_lo=tt_lo, tt_hi=tt_hi,
                                                first=(ki == 0), last=(ki == len(kts) - 1), vrhs=va[:, kt, g_, :], vreads=[(vkey, kt, g_ // 2), v1key]))
                            return out

                        score = [sb("score%d" % i, [128, 128], F32, sa) for i in range(2)]
                        mx8 = [sb("mx8_%d" % i, [128, 4, 8], F32, sa) for i in range(2)]
                        selb = [sb("selb%d" % i, [128, 128], F32, sa) for i in range(2)]

                        selb8 = [sb("selb8_%d" % i, [128, 128], F32, sa) for i in range(8)]

                        def sel_pre(t):
                            i = t % 2
                            ks_, km_, kb_ = 'score%d' % i, 'mx8_%d' % i, 'selb8_%d' % (t % 8)
                            sbt = selb8[t % 8]
                            for g_ in range(4):
                                V(lambda e: e.tensor_tensor(out=score[i][:, g_ * 32:(g_ + 1) * 32], in0=imp[:, t, g_ * 32:(g_ + 1) * 32], in1=forceb[:, t, :], op=ALU.add),
                                  [('imp', t, g_), 'forceb', ks_], [ks_])
                            for g_ in range(4):
                                V(lambda e: e.max(out=mx8[i][:, g_, :], in_=score[i][:, g_ * 32:(g_ + 1) * 32]), [ks_, km_], [km_])
                            for g_ in range(4):
                                V(lambda e: e.tensor_scalar(out=sbt[:, g_ * 32:(g_ + 1) * 32], in0=score[i][:, g_ * 32:(g_ + 1) * 32], scalar1=mx8[i][:, g_, 7:8], scalar2=-1.0,
                                                            op0=ALU.is_ge, op1=ALU.add), [ks_, km_, kb_], [kb_])

                        def sel_post(t):
                            kb_ = 'selb8_%d' % (t % 8)
                            ps, pk = PS[t % 5], 'ps%d' % (t % 5)
                            C.op('pe', lambda e: e.transpose(out=ps[:, 0:128], in_=selb8[t % 8][:, :], identity=ident[:]), reads=[kb_, 'ident'], writes=[pk], chain=True)
                            for g_ in range(4):
                                V(lambda e: e.tensor_scalar(out=selbT[:, g_, t * 128:(t + 1) * 128], in0=ps[:, 0:128], scalar1=rowmask[:, g_:g_ + 1], scalar2=None, op0=ALU.mult),
                                  [pk, 'rowmask'], [('selbT', t, g_)])

                        def cw_steps(tb):
                            out = []
                            for h_ in range(16):
                                out += compressed_steps(h_, tb, ('c', h_, tb))
                                out += branch_steps(h_, tb, 2, ('w', h_, tb))
                            return out

                        def s_steps(tb):
                            out = []
                            for h_ in range(16):
                                out += branch_steps(h_, tb, 1, ('s', h_, tb))
                            return out

                        run_pipeline(cw_steps(0))
                        for t in range(0, 4):
                            sel_pre(t)
                        run_pipeline(cw_steps(1))
                        for t in range(0, 4):
                            sel_post(t)
                        for t in range(4, 8):
                            sel_pre(t)
                        run_pipeline(cw_steps(2))
                        for t in range(4, 8):
                            sel_post(t)
                        for t in range(8, 12):
                            sel_pre(t)
                        run_pipeline(cw_steps(3))
                        for t in range(8, 12):
                            sel_post(t)
                        for t in range(12, 16):
                            sel_pre(t)
                        run_pipeline(s_steps(0))
                        for t in range(12, 16):
                            sel_post(t)
                        for tb in range(1, 4):
                            run_pipeline(s_steps(tb))
                        C.barrier()
                    C.barrier()
                with ExitStack() as s3:
                    oT = sb("oT", [128, KT, L], BF16, s3)
                    reload_h(hspill_d)
                    for t in range(NT):
                        for half in range(2):
                            ps, pk = bank()
                            psb_ = ps[:, :].bitcast(BF16)
                            for kk in range(4):
                                k = half * 4 + kk
                                C.op('pe', lambda e: e.transpose(out=psb_[:, kk * 128:(kk + 1) * 128], in_=obuf[:, t, k * 128:(k + 1) * 128], identity=identb[:]),
                                     reads=['identb'], writes=[pk], chain=True)
                            evac(oT[:, half * 4:half * 4 + 4, t * 128:(t + 1) * 128], psb_[:, 0:512].rearrange("p (k n) -> p k n", k=4), reads=[pk], writes=[('oT', t)])
                    if "dbg_obuf" in dbg:
                        for t in range(NT):
                            C.dma('pool', dbg["dbg_obuf"][:, t, :], obuf[:, t, :], reads=[])
                    out_proj_ln(oT, wo, ['wo', 'wo2'] + [('oT', t) for t in range(NT)], 2, lnp)
                    C.barrier()

        stages = ["ssm", "attn", "mix0", "moe0", "all"]
        if upto.startswith("nsa"):
            nsa_mixer()
        else:
            layer0_mixer()
            if upto not in ("mix0", "ssm", "attn"):
                moe(0, 1)
                if upto == "all":
                    nsa_mixer()
                    moe(1, 3, final=True)
        C.barrier()
        if upto != "all":
            for t in range(NT):
                C.dma('sp' if t % 2 == 0 else 'act', out_d[t * 128:(t + 1) * 128, :], h[:, t, :], reads=[('h', t)])
        C.barrier()
        print("instructions", C.n_ins, "waits", C.n_wait)
    return nc


def host_layouts(inp):
    f32 = np.float32
    o = {}
    lam_re = np.asarray(inp["ssm_lam_re"], f32)
    lam_im = np.asarray(inp["ssm_lam_im"], f32)
    log_dt = np.asarray(inp["ssm_log_dt"], f32)

    def state_layout(a):
        return np.ascontiguousarray(a.reshape(16, 2, 64).transpose(1, 2, 0).reshape(128, 16))
    o["lam_re_l"] = state_layout(lam_re)
    o["lam_im_l"] = state_layout(lam_im)
    o["logdt_l"] = state_layout(np.repeat(log_dt[:, None], 64, axis=1))
    b_re = np.asarray(inp["ssm_b_re"], f32)
    b_im = np.asarray(inp["ssm_b_im"], f32)
    c_re = np.asarray(inp["ssm_c_re"], f32)
    c_im = np.asarray(inp["ssm_c_im"], f32)
    bre = np.zeros((128, 16, 128), f32)
    bim = np.zeros((128, 16, 128), f32)
    cre = np.zeros((128, 16, 128), f32)
    cim = np.zeros((128, 16, 128), f32)
    for g in range(32):
        j, two = g // 2, g % 2
        r0 = 32 * (j % 4) + 16 * two
        bre[r0:r0 + 16, j, two * 64:(two + 1) * 64] = b_re[g].T
        bim[r0:r0 + 16, j, two * 64:(two + 1) * 64] = b_im[g].T
        cre[two * 64:(two + 1) * 64, j, r0:r0 + 16] = c_re[g].T
        cim[two * 64:(two + 1) * 64, j, r0:r0 + 16] = c_im[g].T
    o["bre_l"], o["bim_l"], o["cre_l"], o["cim_l"] = bre, bim, cre, cim
    o["ssmd_l"] = np.ascontiguousarray(np.asarray(inp["ssm_d"], f32).reshape(4, 128).T)
    o["ln_all"] = np.stack([np.asarray(inp[k], f32) for k in
                            ["ln_mix_g_0", "ln_mix_b_0", "ln_ffn_g_0", "ln_ffn_b_0", "ln_mix_g_1", "ln_mix_b_1", "ln_ffn_g_1", "ln_ffn_b_1"]])
    o["b_router"] = np.asarray(inp["b_router"], f32).reshape(1, 16)
    for k in ["w_in_0", "w_glu", "w_out_0", "w1_0", "w3_0", "w2_0", "w1_1", "w3_1", "w2_1"]:
        o[k] = np.ascontiguousarray(np.asarray(inp[k], f32))
    o["w_router_l"] = np.ascontiguousarray(np.asarray(inp["w_router"], f32).reshape(8, 128, 16).transpose(1, 0, 2).reshape(128, 128))
    o["ident"] = np.eye(128, dtype=f32)
    jj, ss = np.meshgrid(np.arange(128), np.arange(128), indexing="ij")
    o["triu_neg"] = -(jj >= ss).astype(f32)

    w1 = np.asarray(inp["w_in_1"], f32)
    perm64 = (np.arange(64) + 32) % 64

    def swap_cols(w):
        nh = w.shape[1] // 64
        idx = (np.arange(nh)[:, None] * 64 + perm64[None, :]).reshape(-1)
        return w[:, idx]

    def dup_heads(w):
        return np.concatenate([np.concatenate([w[:, g * 64:(g + 1) * 64]] * 2, axis=1) for g in range(4)], axis=1)
    q_w = w1[:, 0:1024]
    kc_w, vc_w, ks_w, vs_w, kw_w, vw_w = [w1[:, 1024 + i * 256:1024 + (i + 1) * 256] for i in range(6)]
    gate_w = w1[:, 2560:2608]
    hq = []
    for T in range(8):
        G2, r = T // 4, T % 4
        hq += [(2 * G2) * 4 + r, (2 * G2 + 1) * 4 + r]
    qidx = (np.asarray(hq)[:, None] * 64 + np.arange(64)[None, :]).reshape(-1)
    q_sw_full = swap_cols(q_w)
    pieces = {"q": q_w[:, qidx], "q_sw": q_sw_full[:, qidx], "ks": ks_w, "ks_sw": swap_cols(ks_w),
              "kw": kw_w, "kw_sw": swap_cols(kw_w), "kc": kc_w, "vc": vc_w, "vs": vs_w, "vw": vw_w, "gate": gate_w}
    w1all = np.zeros((1024, W1ALL_COLS), f32)
    for n_, a_ in pieces.items():
        w1all[:, OFF[n_]:OFF[n_] + a_.shape[1]] = a_
    o["w1all"] = w1all
    for nm, key in (("k", "cmp_w1_k"), ("v", "cmp_w1_v")):
        cw1 = np.asarray(inp[key], f32).reshape(32, 64, 256).transpose(1, 0, 2)
        o["cw1%s_l" % nm] = np.ascontiguousarray(np.concatenate([cw1, cw1], axis=0))
    for nm, key in (("k", "cmp_pos_k"), ("v", "cmp_pos_v")):
        pt = np.asarray(inp[key], f32).T
        o["cpos%s_T" % nm] = np.ascontiguousarray(np.concatenate([pt, pt], axis=0))
    w2k = np.asarray(inp["cmp_w2_k"], f32)
    o["cw2k_dup"] = np.ascontiguousarray(np.concatenate([w2k, w2k], axis=1))
    o["cw2k_dup_sw"] = np.ascontiguousarray(np.concatenate([w2k[:, perm64], w2k[:, perm64]], axis=1))
    o["cw2v"] = np.ascontiguousarray(np.asarray(inp["cmp_w2_v"], f32))
    o["w_out_1"] = np.ascontiguousarray(np.asarray(inp["w_out_1"], f32))
    inv = (10000.0 ** (-np.arange(0, 64, 2, dtype=f32) / 64)).astype(f32)

    def rope_tabs(pos):
        ang = pos.astype(f32)[None, :] * inv[:, None]
        c, s_ = np.cos(ang).astype(f32), np.sin(ang).astype(f32)
        cosT = np.concatenate([c, c, c, c], axis=0)
        sinT = np.concatenate([-s_, s_, -s_, s_], axis=0)
        return cosT, sinT
    o["cosF"], o["sinF"] = rope_tabs(np.arange(2048))
    cc, sc_ = rope_tabs(np.arange(127) * 16 + 31)
    o["cosC"] = np.zeros((128, 128), f32); o["cosC"][:, :127] = cc
    o["sinC"] = np.zeros((128, 128), f32); o["sinC"][:, :127] = sc_
    m_ = np.arange(128)[:, None]; t_ = np.arange(2048)[None, :]
    o["cmask"] = np.where(16 * m_ + 31 <= t_, 0.0, -BIG).astype(f32)
    mm_ = np.arange(128)[:, None]; nn_ = np.arange(32)[None, :]
    ov = ((16 * mm_ < 64 * nn_ + 64) & (16 * mm_ + 32 > 64 * nn_)).astype(f32)
    o["ovaug"] = np.concatenate([np.ones((128, 1), f32), ov], axis=1)
    tt_ = np.arange(2048)[:, None]; cur = tt_ // 64
    valid = nn_ * 64 <= tt_
    forced = (nn_ == 0) | (nn_ == cur) | (nn_ == cur - 1)
    fb = np.where(valid, np.where(forced, 1e4, 0.0), -1e30).astype(f32)
    o["forceb"] = np.ascontiguousarray(fb.reshape(16, 128, 32).transpose(1, 0, 2))
    es = np.zeros((128, 16, 128), f32)
    for g in range(4):
        for kt in range(16):
            for j in range(128):
                es[32 * g + 2 * kt + j // 64, kt, j] = BIG
    o["esel"] = es
    ji, ii = np.meshgrid(np.arange(128), np.arange(128), indexing="ij")
    o["caus"] = np.where(ji > ii, -BIG, 0.0).astype(f32)
    o["anti"] = np.where(ji <= ii, -BIG, 0.0).astype(f32)
    o["caus_s"] = np.where(ji >= ii, -BIG, 0.0).astype(f32)
    o["rowmask"] = (np.arange(128)[:, None] // 32 == np.arange(4)[None, :]).astype(f32)
    return o


_NC_CACHE = {}


def kernel(**inputs):
    n = 8
    x = np.asarray(inputs["x"], np.float32)
    shared = host_layouts(inputs)
    if "nc" not in _NC_CACHE:
        _NC_CACHE["nc"] = build("all")
    nc = _NC_CACHE["nc"]
    in_maps = []
    for c in range(n):
        m = dict(shared)
        m["x"] = np.ascontiguousarray(x[c])
        in_maps.append(m)
    res = run_bass_kernel_spmd(nc, in_maps, core_ids=list(range(n)))
    return np.stack([np.asarray(r["out"], np.float32) for r in res.results], axis=0)
```
